# Optimizing a Trainium2 kernel written in Bass

```python
import math
import jax, jax.numpy as jnp
from jax import lax
import numpy as np

D_MODEL = 1024
BATCH = 8
SEQ = 4096
DEPTH = 1

D_MIX = D_MODEL
HEAD_SIZE = 64
RWKV_DIM = D_MIX // 2
RWKV_HEADS = RWKV_DIM // HEAD_SIZE
POOL_DIM = D_MIX - RWKV_DIM
POOL_WINDOWS = (2, 4, 8, 16)
POOL_GROUPS = len(POOL_WINDOWS)
POOL_GROUP_DIM = POOL_DIM // POOL_GROUPS

DECAY_LORA = max(32, int(round(1.8 * D_MODEL ** 0.5 / 32)) * 32)
AAA_LORA = max(32, int(round(1.8 * D_MODEL ** 0.5 / 32)) * 32)
GATE_LORA = max(32, int(round(0.6 * D_MODEL ** 0.8 / 32)) * 32)
N_DIR = 2
RWKV_IN = 3 * RWKV_DIM + N_DIR * DECAY_LORA + N_DIR * AAA_LORA + GATE_LORA
D_IN = RWKV_IN + POOL_DIM
RWKV_SPLITS = (RWKV_DIM, 2 * RWKV_DIM, 3 * RWKV_DIM,
               3 * RWKV_DIM + N_DIR * DECAY_LORA,
               3 * RWKV_DIM + N_DIR * DECAY_LORA + N_DIR * AAA_LORA)
GN_EPS = 64e-5
RMS_EPS = 1e-5

N_KEYS = 128
N_EXPERTS = N_KEYS * N_KEYS
PEER_HEADS = 8
PEER_TOPK = 16
PEER_QDIM = 256
PEER_KHALF = PEER_QDIM // 2
PEER_BLOCK = 128

kernel_name = "hybrid_rwkv7_pool_peer_encoder"


def rmsnorm(x, w):
    xf = x.astype(jnp.float32)
    y = xf * lax.rsqrt(jnp.mean(xf * xf, axis=-1, keepdims=True) + RMS_EPS)
    return (y * w.astype(jnp.float32)).astype(x.dtype)


def bi_token_shift(p, mu):
    prev = jnp.pad(p[:, :-1], ((0, 0), (1, 0), (0, 0)))
    nxt = jnp.pad(p[:, 1:], ((0, 0), (0, 1), (0, 0)))
    return p + mu[0] * (prev - p) + mu[1] * (nxt - p)


def wkv7_scan(r, w, k, v, kk, a, reverse):
    B, S, H, N = r.shape

    def step(state, inp):
        r_t, w_t, k_t, v_t, kk_t, a_t = inp
        sa = jnp.einsum('bhvk,bhk->bhv', state, -kk_t)
        state = (state * w_t[:, :, None, :]
                 + sa[..., :, None] * (kk_t * a_t)[..., None, :]
                 + v_t[..., :, None] * k_t[..., None, :])
        out = jnp.einsum('bhvk,bhk->bhv', state, r_t)
        return state, out

    xs = tuple(jnp.moveaxis(t, 1, 0) for t in (r, w, k, v, kk, a))
    state0 = jnp.zeros((B, H, N, N), jnp.float32)
    _, out = lax.scan(step, state0, xs, reverse=reverse)
    return jnp.moveaxis(out, 0, 1)


def multiscale_pool(p, pool_w, pool_scale):
    B, S, _ = p.shape
    pg = p.reshape(B, S, POOL_GROUPS, POOL_GROUP_DIM).astype(jnp.float32)
    cs = jnp.concatenate([jnp.zeros((B, 1, POOL_GROUPS, POOL_GROUP_DIM), jnp.float32),
                          jnp.cumsum(pg, axis=1)], axis=1)
    t = jnp.arange(S)
    outs = []
    for gi, win in enumerate(POOL_WINDOWS):
        half = win // 2
        lo = jnp.clip(t - half, 0, S)
        hi = jnp.clip(t + half, 0, S)
        cnt = (hi - lo).astype(jnp.float32)[None, :, None]
        cs_g = cs[:, :, gi]
        mean = (jnp.take(cs_g, hi, axis=1) - jnp.take(cs_g, lo, axis=1)) / cnt
        outs.append(mean - pg[:, :, gi])
    pooled = jnp.stack(outs, axis=2)
    mixed = jnp.einsum('bsgc,gcd->bsgd', pooled, pool_w.astype(jnp.float32))
    return (mixed.reshape(B, S, POOL_DIM) * pool_scale.astype(jnp.float32)).astype(p.dtype)


def hybrid_mixer(h, w_in, shift_mu, w0, w_up, a0, a_up, g_up, k_k, k_a, r_k,
                 ln_x_w, ln_x_b, pool_w, pool_scale, w_out):
    B, S, _ = h.shape
    f32 = jnp.float32
    p = h @ w_in
    p_rw = bi_token_shift(p[..., :RWKV_IN], shift_mu)
    p_pool = p[..., RWKV_IN:]

    r, k, v, wd, ad, gd = jnp.split(p_rw, RWKV_SPLITS, axis=-1)
    wd = wd.reshape(B, S, N_DIR, DECAY_LORA)
    ad = ad.reshape(B, S, N_DIR, AAA_LORA)

    def heads(t):
        return t.reshape(B, S, RWKV_HEADS, HEAD_SIZE).astype(f32)

    rh = heads(r)
    vh = heads(v)
    kk = heads(k * k_k)
    kk = kk / jnp.maximum(jnp.sqrt(jnp.sum(kk * kk, axis=-1, keepdims=True)), 1e-12)
    g = jax.nn.sigmoid(gd) @ g_up

    o = jnp.zeros((B, S, RWKV_HEADS, HEAD_SIZE), f32)
    bonus = jnp.zeros((B, S, RWKV_HEADS, HEAD_SIZE), f32)
    for d in range(N_DIR):
        wlog = -jax.nn.softplus(-(w0[d] + jnp.tanh(wd[:, :, d]) @ w_up[d])) - 0.5
        decay = jnp.exp(-jnp.exp(wlog.astype(f32)))
        a = jax.nn.sigmoid(a0[d] + ad[:, :, d] @ a_up[d])
        kd = heads(k * (1 + (a - 1) * k_a))
        o = o + wkv7_scan(rh, heads(decay), kd, vh, kk, heads(a), reverse=(d == 1))
        bonus = bonus + jnp.sum(rh * kd * r_k.astype(f32), axis=-1, keepdims=True) * vh

    mu = jnp.mean(o, axis=-1, keepdims=True)
    var = jnp.mean(jnp.square(o - mu), axis=-1, keepdims=True)
    o = ((o - mu) * lax.rsqrt(var + GN_EPS)).reshape(B, S, RWKV_DIM)
    o = o * ln_x_w.astype(f32) + ln_x_b.astype(f32) + bonus.reshape(B, S, RWKV_DIM)
    y_rwkv = (o * g.astype(f32)).astype(h.dtype)

    y_pool = multiscale_pool(p_pool, pool_w, pool_scale)
    return jnp.concatenate([y_rwkv, y_pool], axis=-1) @ w_out


def peer_ffn(h, wq, keys, u_tab, v_tab):
    B, S, D = h.shape
    hb = h.reshape(-1, PEER_BLOCK, D)

    def block_fn(xb):
        T = xb.shape[0]
        q = (xb @ wq).reshape(T, PEER_HEADS, 2, PEER_KHALF)
        s = jnp.einsum('thpc,hpnc->thpn', q.astype(jnp.float32), keys.astype(jnp.float32))
        v1, i1 = lax.top_k(s[:, :, 0], PEER_TOPK)
        v2, i2 = lax.top_k(s[:, :, 1], PEER_TOPK)
        cand = (v1[..., :, None] + v2[..., None, :]).reshape(T, PEER_HEADS, PEER_TOPK * PEER_TOPK)
        cidx = (i1[..., :, None] * N_KEYS + i2[..., None, :]).reshape(T, PEER_HEADS, PEER_TOPK * PEER_TOPK)
        sc, pos = lax.top_k(cand, PEER_TOPK)
        idx = jnp.take_along_axis(cidx, pos, axis=-1)
        gate = jax.nn.softmax(sc, axis=-1)
        ug = jnp.take(u_tab, idx, axis=0)
        act = jax.nn.gelu(jnp.einsum('td,thkd->thk', xb, ug).astype(jnp.float32), approximate=False)
        vg = jnp.take(v_tab, idx, axis=0)
        return jnp.einsum('thk,thkd->td', (gate * act).astype(xb.dtype), vg)

    return lax.map(block_fn, hb).reshape(B, S, D)


def setup_inputs(seed: int = 0) -> dict:
    key = jax.random.key(seed)
    ks = jax.random.split(key, 24)
    L = DEPTH
    nrm = lambda k, shape, s: jax.random.normal(k, shape, jnp.float32) * s
    return {
        "x": nrm(ks[0], (BATCH, SEQ, D_MODEL), 1.0),
        "norm1_w": 1.0 + nrm(ks[1], (L, D_MODEL), 0.02),
        "w_in": nrm(ks[2], (L, D_MODEL, D_IN), D_MODEL ** -0.5),
        "shift_mu": jax.random.uniform(ks[3], (L, 2, RWKV_IN), jnp.float32, 0.0, 0.5),
        "w0": nrm(ks[4], (L, N_DIR, RWKV_DIM), 0.5) - 1.0,
        "w_up": nrm(ks[5], (L, N_DIR, DECAY_LORA, RWKV_DIM), 0.1),
        "a0": nrm(ks[6], (L, N_DIR, RWKV_DIM), 0.5),
        "a_up": nrm(ks[7], (L, N_DIR, AAA_LORA, RWKV_DIM), 0.1),
        "g_up": nrm(ks[8], (L, GATE_LORA, RWKV_DIM), GATE_LORA ** -0.5),
        "k_k": 0.85 + nrm(ks[9], (L, RWKV_DIM), 0.05),
        "k_a": 1.0 + nrm(ks[10], (L, RWKV_DIM), 0.05),
        "r_k": nrm(ks[11], (L, RWKV_HEADS, HEAD_SIZE), 0.1),
        "ln_x_w": 1.0 + nrm(ks[12], (L, RWKV_DIM), 0.02),
        "ln_x_b": nrm(ks[13], (L, RWKV_DIM), 0.02),
        "pool_w": nrm(ks[14], (L, POOL_GROUPS, POOL_GROUP_DIM, POOL_GROUP_DIM), POOL_GROUP_DIM ** -0.5),
        "pool_scale": 1.0 + nrm(ks[15], (L, POOL_DIM), 0.1),
        "w_out": nrm(ks[16], (L, D_MIX, D_MODEL), D_MIX ** -0.5),
        "norm2_w": 1.0 + nrm(ks[17], (L, D_MODEL), 0.02),
        "peer_wq": nrm(ks[18], (L, D_MODEL, PEER_HEADS * PEER_QDIM), D_MODEL ** -0.5),
        "peer_keys": nrm(ks[19], (L, PEER_HEADS, 2, N_KEYS, PEER_KHALF), PEER_KHALF ** -0.5),
        "peer_u": nrm(ks[20], (L, N_EXPERTS, D_MODEL), D_MODEL ** -0.5),
        "peer_v": nrm(ks[21], (L, N_EXPERTS, D_MODEL), PEER_HEADS ** -0.5),
        "norm_f_w": 1.0 + nrm(ks[22], (D_MODEL,), 0.02),
    }


def reference(x, norm1_w, w_in, shift_mu, w0, w_up, a0, a_up, g_up, k_k, k_a, r_k,
              ln_x_w, ln_x_b, pool_w, pool_scale, w_out, norm2_w, peer_wq, peer_keys,
              peer_u, peer_v, norm_f_w):
    for l in range(DEPTH):
        h = rmsnorm(x, norm1_w[l])
        x = x + hybrid_mixer(h, w_in[l], shift_mu[l], w0[l], w_up[l], a0[l], a_up[l], g_up[l],
                             k_k[l], k_a[l], r_k[l], ln_x_w[l], ln_x_b[l], pool_w[l],
                             pool_scale[l], w_out[l])
        h = rmsnorm(x, norm2_w[l])
        x = x + peer_ffn(h, peer_wq[l], peer_keys[l], peer_u[l], peer_v[l])
    return rmsnorm(x, norm_f_w)
```

```python
import numpy as np
import ml_dtypes
import concourse.bass as bass
import concourse.mybir as mybir
from concourse.bass_utils import run_bass_kernel_spmd

F32 = mybir.dt.float32
BF16 = mybir.dt.bfloat16
I32 = mybir.dt.int32
U32 = mybir.dt.uint32
AF = mybir.ActivationFunctionType
ALU = mybir.AluOpType
AX = mybir.AxisListType

S_LEN = 4096
D = 1024
NT = S_LEN // 128
RWKV_IN = 1952
D_IN = 2464
C0 = float(np.exp(-0.5))
RMS_EPS = 1e-5
GN_EPS = 64e-5
PAD = 8


class Buf:
    __slots__ = ("name", "w", "r", "excl")

    def __init__(self, name=""):
        self.name = name
        self.w = None
        self.r = {}
        self.excl = False


class T:
    def __init__(self, t, name=""):
        self.t = t
        self.b = Buf(name)

    def __getitem__(self, k):
        return self.t[k]


def _b(x):
    return x.b if isinstance(x, T) else x


class Sched:
    def __init__(self, nc, ndma=32):
        self.nc = nc
        self.es = {"pe": nc.tensor, "act": nc.scalar, "dve": nc.vector, "pool": nc.gpsimd, "sp": nc.sync}
        self.sem = {}
        self.tick = {k: 0 for k in self.es}
        self.seen = {k: {} for k in self.es}
        self._ctx = []
        for k in self.es:
            cm = nc.semaphore("s_" + k)
            self.sem[k] = cm.__enter__()
            self._ctx.append(cm)
        self.ndma = ndma
        self.dsem = []
        for i in range(ndma):
            cm = nc.semaphore("d_%d" % i)
            self.dsem.append(cm.__enter__())
            self._ctx.append(cm)
        self.dcount = 0
        self.rec = False
        self.pending = []
        self.nwait = 0
        self.nops = 0

    def _need(self, e, deps):
        best = {}
        for d in deps:
            if d is None:
                continue
            key, val = d
            if key == e and e == "pe":
                continue
            if best.get(key, 0) < val:
                best[key] = val
        for key, val in best.items():
            if self.seen[e].get(key, 0) >= val:
                continue
            sem = self.sem[key] if isinstance(key, str) else self.dsem[key]
            self.es[e].wait_ge(sem, val)
            self.nwait += 1
            self.seen[e][key] = val

    def _deps(self, reads, writes):
        deps = []
        for b in reads:
            deps.append(b.w)
        for b in writes:
            deps.append(b.w)
            for k, v in b.r.items():
                deps.append((k, v))
        return deps

    def flush(self, n=None):
        pend = self.pending
        k = len(pend) if n is None else min(n, len(pend))
        todo, self.pending = pend[:k], pend[k:]
        rec, self.rec = self.rec, False
        for kind, a, kw in todo:
            if kind == "op":
                self.op(*a, **kw)
            else:
                self.dma(*a, **kw)
        self.rec = rec

    def op(self, e, name, reads=(), writes=(), **kw):
        if self.rec:
            self.pending.append(("op", (e, name, reads, writes), kw))
            return None
        reads = [_b(x) for x in reads]
        writes = [_b(x) for x in writes]
        writes = writes + [b for b in reads if b.excl and b not in writes]
        reads = [b for b in reads if not b.excl]
        self._need(e, self._deps(reads, writes))
        ins = getattr(self.es[e], name)(**kw)
        self.tick[e] += 1
        self.nops += 1
        ins.then_inc(self.sem[e], 1)
        tk = self.tick[e]
        for b in reads:
            b.r[e] = tk
        for b in writes:
            b.w = (e, tk)
            b.r = {}
        return ins

    def dma(self, e, out, in_, reads=(), writes=(), indirect=None, **kw):
        if self.rec:
            self.pending.append(("dma", (e, out, in_, reads, writes, indirect), kw))
            return None
        reads = [_b(x) for x in reads]
        writes = [_b(x) for x in writes]
        j = self.dcount
        self.dcount += 1
        slot = j % self.ndma
        rnd = j // self.ndma
        deps = self._deps(reads, writes)
        if rnd > 0:
            deps.append((slot, 16 * rnd))
        self._need(e, deps)
        if indirect is None:
            ins = self.es[e].dma_start(out=out, in_=in_, **kw)
        else:
            ins = self.es[e].indirect_dma_start(out=out, out_offset=None, in_=in_, in_offset=indirect, **kw)
        ins.then_inc(self.dsem[slot], 16)
        self.nops += 1
        val = 16 * (rnd + 1)
        for b in reads:
            b.r[slot] = val
        for b in writes:
            b.w = (slot, val)
            b.r = {}
        return ins

    def barrier(self):
        deps = [(k, self.tick[k]) for k in self.es if self.tick[k] > 0]
        for j in range(min(self.dcount, self.ndma)):
            cnt = (self.dcount - 1 - j) // self.ndma + 1
            deps.append((j, 16 * cnt))
        for e in self.es:
            self._need(e, deps)


def build(debug=(), peer=True, cut=99, nblk=99):
    nc = bass.Bass("TRN2", target_bir_lowering=False)
    S = Sched(nc)
    guards = []

    def din(name, shape, dt=F32):
        return nc.dram_tensor(name, list(shape), dt, kind="ExternalInput").ap()

    def dscr(name, shape, dt=F32):
        kind = "ExternalOutput" if name in debug else "Internal"
        return nc.dram_tensor(name, list(shape), dt, kind=kind).ap()

    def sb(name, shape, dt=F32):
        g = nc.sbuf_tensor(name, list(shape), dt)
        t = g.__enter__()
        guards.append(g)
        return T(t, name)

    def ps(name, shape, dt=F32):
        g = nc.psum_tensor(name, list(shape), dt)
        t = g.__enter__()
        guards.append(g)
        return T(t, name)

    def release(n):
        for _ in range(n):
            g = guards.pop()
            g.__exit__(None, None, None)

    x = din("x", [S_LEN, D])
    w_in = din("w_in", [128, 8, D_IN])
    shift_mu = din("shift_mu", [2, RWKV_IN])
    norm1_w = din("norm1_w", [D])
    w0 = din("w0", [2, 512]); a0 = din("a0", [2, 512])
    w_up = din("w_up", [128, 512]); a_up = din("a_up", [128, 512])
    g_up = din("g_up", [160, 512])
    k_k = din("k_k", [512]); k_a = din("k_a", [512]); r_k = din("r_k", [512])
    ln_x_w = din("ln_x_w", [512]); ln_x_b = din("ln_x_b", [512])
    pool_w = din("pool_w", [128, 4, 128]); pool_scale = din("pool_scale", [512])
    w_out = din("w_out", [128, 8, D])
    norm2_w = din("norm2_w", [D]); norm_f_w = din("norm_f_w", [D])
    wq = din("wq", [128, 8, 2048])
    keysT = din("keysT", [128, 16, 128])
    if peer:
        peer_u = din("peer_u", [16384, D]); peer_v = din("peer_v", [16384, D])
    c_ident = din("c_ident", [128, 128])
    c_masks = din("c_masks", [128, 4, 128])
    c_bones = din("c_bones", [128, 128])
    c_sel = din("c_sel", [128, 2])
    c_reset = din("c_reset", [128, 1024])
    c_invcnt = din("c_invcnt", [128, 4, 16])
    out = nc.dram_tensor("out", [S_LEN, D], F32, kind="ExternalOutput").ap()

    RT = dscr("RT", [512, S_LEN]); KT = dscr("KT", [512, S_LEN])
    VTOK = dscr("VTOK", [S_LEN, 512])
    YP = dscr("YP", [512, S_LEN], BF16)

    identf = sb("identf", [128, 128]); identb = sb("identb", [128, 128], BF16)
    S.dma("sp", identf[:], c_ident[:, :], writes=[identf])
    S.op("dve", "tensor_copy", reads=[identf], writes=[identb], out=identb[:], in_=identf[:])
    epsc = sb("epsc", [128, 2])
    S.op("pool", "memset", writes=[epsc], ap=epsc[:, 0:1], constant=RMS_EPS)
    S.op("pool", "memset", writes=[epsc], ap=epsc[:, 1:2], constant=GN_EPS)
    NPC = 16 + 16 + 16 + 8 + 8 + 4 + 4 + 4 + 4 + 4
    pc = sb("pc", [128, NPC])
    S.op("pool", "memset", writes=[pc], ap=pc[:], constant=0.0)
    col = {}
    o = 0

    def ldcol(name, vec, n):
        nonlocal o
        col[name] = o
        nfull = n // 128
        if nfull:
            S.dma("sp", pc[:, o:o + nfull], vec[0:nfull * 128].rearrange("(c p) -> p c", p=128), writes=[pc],
                  allow_slow_non_contiguous=True)
        rem = n - nfull * 128
        if rem:
            S.dma("sp", pc[0:rem, o + nfull:o + nfull + 1], vec[nfull * 128:n].rearrange("(c p) -> p c", p=rem),
                  writes=[pc], allow_slow_non_contiguous=True)
        o += (n + 127) // 128

    ldcol("mu0", shift_mu[0, :], RWKV_IN); ldcol("mu1", shift_mu[1, :], RWKV_IN)
    col["muc"] = o; o += 16
    ldcol("w0_0", w0[0, :], 512); ldcol("w0_1", w0[1, :], 512)
    ldcol("a0_0", a0[0, :], 512); ldcol("a0_1", a0[1, :], 512)
    ldcol("k_k", k_k, 512); ldcol("k_a", k_a, 512); ldcol("r_k", r_k, 512); ldcol("pscale", pool_scale, 512)
    col["omka"] = o; o += 4
    assert o == NPC, (o, NPC)
    S.op("dve", "tensor_tensor", reads=[pc], writes=[pc], out=pc[:, col["muc"]:col["muc"] + 16],
         in0=pc[:, col["mu0"]:col["mu0"] + 16], in1=pc[:, col["mu1"]:col["mu1"] + 16], op=ALU.add)
    S.op("dve", "tensor_scalar", reads=[pc], writes=[pc], out=pc[:, col["muc"]:col["muc"] + 16],
         in0=pc[:, col["muc"]:col["muc"] + 16], scalar1=-1.0, scalar2=1.0, op0=ALU.mult, op1=ALU.add)
    S.op("dve", "tensor_scalar", reads=[pc], writes=[pc], out=pc[:, col["omka"]:col["omka"] + 4],
         in0=pc[:, col["k_a"]:col["k_a"] + 4], scalar1=-1.0, scalar2=1.0, op0=ALU.mult, op1=ALU.add)

    twd = sb("twd", [128, S_LEN], BF16)
    adT = sb("adT", [128, S_LEN], BF16)
    sgA = sb("sgA", [128, S_LEN], BF16)
    sgB = sb("sgB", [32, S_LEN], BF16)

    pall = ps("pall", [128, 8, 512])
    banks = [T(pall.t[:, i, :], "bank%d" % i) for i in range(8)]
    for bk_ in banks:
        bk_.b.excl = True

    n_ph = len(guards)
    hT = sb("hT", [128, 8, S_LEN], BF16)
    pbuf = [sb("pbuf%d" % i, [128, S_LEN + 2 * PAD]) for i in range(2)]
    for pb in pbuf:
        S.op("pool", "memset", writes=[pb], ap=pb[:, 0:PAD], constant=0.0)
        S.op("pool", "memset", writes=[pb], ap=pb[:, PAD + S_LEN:], constant=0.0)
    tA = sb("tA", [128, S_LEN + 2 * PAD]); tB = sb("tB", [128, S_LEN + 2 * PAD])
    wb1 = sb("nw1b", [128, D])
    S.dma("sp", wb1[:], norm1_w.partition_broadcast(128), writes=[wb1])
    ssq = sb("ssq", [128, 4])
    hn = [sb("hn%d" % i, [128, D], BF16) for i in range(2)]
    wf = [sb("wf%d" % i, [128, 8, 128]) for i in range(2)]
    wb = [sb("wb%d" % i, [128, 8, 128], BF16) for i in range(2)]

    xts = [tA, tB]
    ptb = [T(banks[i].t[:].bitcast(BF16), "ptb%d" % i) for i in range(2)]
    for pt_, bk in zip(ptb, banks[:2]):
        pt_.b = bk.b
    for i in range(NT):
        xt = xts[i % 2]
        S.dma("sp" if i % 2 == 0 else "act", xt[:, 0:D], x[i * 128:(i + 1) * 128, :], writes=[xt])
        h_ = hn[i % 2]
        S.op("act", "activation", reads=[xt], writes=[h_, ssq], out=h_[:], in_=xt[:, 0:D], func=AF.Square,
             accum_out=ssq[:, 0:1])
        S.op("act", "activation", reads=[ssq], writes=[ssq], out=ssq[:, 1:2], in_=ssq[:, 0:1], func=AF.Sqrt,
             scale=1.0 / D, bias=epsc[:, 0:1])
        S.op("dve", "reciprocal", reads=[ssq], writes=[ssq], out=ssq[:, 2:3], in_=ssq[:, 1:2])
        S.op("dve", "scalar_tensor_tensor", reads=[xt, ssq, wb1], writes=[h_], out=h_[:], in0=xt[:, 0:D],
             scalar=ssq[:, 2:3], in1=wb1[:], op0=ALU.mult, op1=ALU.mult)
        pt_ = ptb[i % 2]
        for kc in range(8):
            S.op("pe", "transpose", reads=[h_, identb], writes=[pt_], out=pt_.t[:, kc * 128:(kc + 1) * 128],
                 in_=h_[:, kc * 128:(kc + 1) * 128], identity=identb[:])
        S.op("act" if i % 2 == 0 else "dve", "activation" if i % 2 == 0 else "tensor_copy", reads=[pt_], writes=[hT],
             out=hT[:, :, i * 128:(i + 1) * 128], in_=pt_.t.rearrange("p (k t) -> p k t", k=8),
             **({"func": AF.Copy} if i % 2 == 0 else {}))

    chunks = []
    for i in range(4):
        chunks.append((i * 128, 128, "r", i))
    for i in range(4):
        chunks.append((512 + i * 128, 128, "k", i))
    for i in range(4):
        chunks.append((1024 + i * 128, 128, "v", i))
    chunks.append((1536, 128, "wd", 0))
    chunks.append((1664, 128, "ad", 0))
    chunks.append((1792, 128, "gdA", 0))
    chunks.append((1920, 32, "gdB", 0))
    for i in range(4):
        chunks.append((RWKV_IN + i * 128, 128, "pool", i))

    poolw_f = sb("poolw_f", [128, 4, 128]); poolw_b = sb("poolw_b", [128, 4, 128], BF16)
    S.dma("sp", poolw_f[:], pool_w[:, :, :], writes=[poolw_f])
    S.op("pool", "tensor_copy", reads=[poolw_f], writes=[poolw_b], out=poolw_b[:], in_=poolw_f[:])
    invcnt = sb("invcnt", [128, 4, 16])
    S.dma("sp", invcnt[:], c_invcnt[:, :, :], writes=[invcnt])
    plb = sb("plb", [128, S_LEN], BF16)
    ypb = sb("ypb", [128, S_LEN], BF16)
    vtk = [sb("vtk%d" % i, [128, 4, 128]) for i in range(2)]

    evac_i = 0
    for ci, (c0, w, kind, idx) in enumerate(chunks):
        wf_, wb_, pb = wf[ci % 2], wb[ci % 2], pbuf[ci % 2]
        S.dma("sp", wf_[:, :, 0:w], w_in[:, :, c0:c0 + w], writes=[wf_])
        S.op("pool", "tensor_copy", reads=[wf_], writes=[wb_], out=wb_[:, :, 0:w], in_=wf_[:, :, 0:w])
        for j in range(8):
            bk = banks[2 + (evac_i % 4)]
            for kc in range(8):
                S.op("pe", "matmul", reads=[wb_, hT], writes=[bk], out=bk[0:w, :], lhsT=wb_[:, kc, 0:w],
                     rhs=hT[:, kc, j * 512:(j + 1) * 512], start=(kc == 0), stop=(kc == 7))
            if evac_i % 2 == 0:
                S.op("act", "activation", reads=[bk], writes=[pb], out=pb[0:w, PAD + j * 512:PAD + (j + 1) * 512],
                     in_=bk[0:w, :], func=AF.Copy)
            else:
                S.op("dve", "tensor_copy", reads=[bk], writes=[pb], out=pb[0:w, PAD + j * 512:PAD + (j + 1) * 512],
                     in_=bk[0:w, :])
            evac_i += 1
        if kind != "pool":
            cc = c0 // 128
            m0 = pc[0:w, col["mu0"] + cc:col["mu0"] + cc + 1]
            m1 = pc[0:w, col["mu1"] + cc:col["mu1"] + cc + 1]
            mc = pc[0:w, col["muc"] + cc:col["muc"] + cc + 1]
            HS = S_LEN // 2
            for hf in range(2):
                lo = PAD + hf * HS
                S.op("act", "activation", reads=[pb, pc], writes=[tA], out=tA[0:w, lo:lo + HS],
                     in_=pb[0:w, lo - 1:lo - 1 + HS], func=AF.Copy, scale=m0)
                S.op("dve", "scalar_tensor_tensor", reads=[pb, pc, tA], writes=[tA], out=tA[0:w, lo:lo + HS],
                     in0=pb[0:w, lo + 1:lo + 1 + HS], scalar=m1, in1=tA[0:w, lo:lo + HS], op0=ALU.mult, op1=ALU.add)
                S.op("dve", "scalar_tensor_tensor", reads=[pb, pc, tA], writes=[tB], out=tB[0:w, lo:lo + HS],
                     in0=pb[0:w, lo:lo + HS], scalar=mc, in1=tA[0:w, lo:lo + HS], op0=ALU.mult, op1=ALU.add)
            res = tB
            R_ = res[0:w, PAD:PAD + S_LEN]
            if kind == "r":
                S.dma("sp", RT[idx * 128:(idx + 1) * 128, :], R_, reads=[res])
            elif kind == "k":
                S.dma("sp", KT[idx * 128:(idx + 1) * 128, :], R_, reads=[res])
            elif kind == "v":
                for g4 in range(8):
                    bk = banks[6 + g4 % 2]
                    for q in range(4):
                        tt = g4 * 4 + q
                        S.op("pe", "transpose", reads=[res, identf], writes=[bk], out=bk[:, q * 128:(q + 1) * 128],
                             in_=res[:, PAD + tt * 128:PAD + (tt + 1) * 128], identity=identf[:])
                    vt = vtk[g4 % 2]
                    S.op("act", "activation", reads=[bk], writes=[vt], out=vt[:],
                         in_=bk.t.rearrange("p (n c) -> p n c", n=4), func=AF.Copy)
                    S.dma("act", VTOK[g4 * 512:(g4 + 1) * 512, idx * 128:(idx + 1) * 128].rearrange("(n p) c -> p n c", p=128),
                          vt[:], reads=[vt])
            elif kind == "wd":
                S.op("act", "activation", reads=[res], writes=[twd], out=twd[:], in_=R_, func=AF.Tanh)
            elif kind == "ad":
                S.op("act", "activation", reads=[res], writes=[adT], out=adT[:], in_=R_, func=AF.Copy)
            elif kind == "gdA":
                S.op("act", "activation", reads=[res], writes=[sgA], out=sgA[:], in_=R_, func=AF.Sigmoid)
            elif kind == "gdB":
                S.op("act", "activation", reads=[res], writes=[sgB], out=sgB[:], in_=R_, func=AF.Sigmoid)
        else:
            gi = idx
            win = (2, 4, 8, 16)[gi]
            half = win // 2
            W_ = S_LEN + 2 * PAD
            S.op("dve", "tensor_tensor", reads=[pb], writes=[tA], out=tA[:, 1:W_], in0=pb[:, 0:W_ - 1], in1=pb[:, 1:W_],
                 op=ALU.add)
            cur, oth = tA, tB
            lo_, hi_ = 1, W_
            sh = 1
            for lev in range(gi):
                nlo, nhi = lo_ + sh, hi_ - sh
                S.op("dve" if lev % 2 else "pool", "tensor_tensor", reads=[cur], writes=[oth], out=oth[:, nlo:nhi],
                     in0=cur[:, nlo - sh:nhi - sh], in1=cur[:, nlo + sh:nhi + sh], op=ALU.add)
                cur, oth = oth, cur
                lo_, hi_ = nlo, nhi
                sh *= 2
            S.op("dve", "scalar_tensor_tensor", reads=[cur, pb], writes=[plb], out=plb[:], in0=cur[:, PAD:PAD + S_LEN],
                 scalar=1.0 / win, in1=pb[:, PAD:PAD + S_LEN], op0=ALU.mult, op1=ALU.subtract)
            S.op("dve", "tensor_tensor", reads=[cur, invcnt], writes=[oth], out=oth[:, 0:half], in0=cur[:, PAD:PAD + half],
                 in1=invcnt[:, gi, 0:half], op=ALU.mult)
            S.op("dve", "tensor_tensor", reads=[oth, pb], writes=[plb], out=plb[:, 0:half], in0=oth[:, 0:half],
                 in1=pb[:, PAD:PAD + half], op=ALU.subtract)
            if half > 1:
                nr = half - 1
                S.op("dve", "tensor_tensor", reads=[cur, invcnt], writes=[oth], out=oth[:, 8:8 + nr],
                     in0=cur[:, PAD + S_LEN - nr:PAD + S_LEN], in1=invcnt[:, gi, 8:8 + nr], op=ALU.mult)
                S.op("dve", "tensor_tensor", reads=[oth, pb], writes=[plb], out=plb[:, S_LEN - nr:S_LEN],
                     in0=oth[:, 8:8 + nr], in1=pb[:, PAD + S_LEN - nr:PAD + S_LEN], op=ALU.subtract)
            for j in range(8):
                bk = banks[2 + (evac_i % 4)]
                evac_i += 1
                S.op("pe", "matmul", reads=[poolw_b, plb], writes=[bk], out=bk[:, :], lhsT=poolw_b[:, gi, :],
                     rhs=plb[:, j * 512:(j + 1) * 512], start=True, stop=True)
                S.op("act", "activation", reads=[bk, pc], writes=[ypb], out=ypb[:, j * 512:(j + 1) * 512], in_=bk[:, :],
                     func=AF.Copy, scale=pc[:, col["pscale"] + gi:col["pscale"] + gi + 1])
            S.dma("sp", YP[gi * 128:(gi + 1) * 128, :], ypb[:], reads=[ypb])

    S.barrier()
    release(len(guards) - n_ph)

    OB = dscr("OB", [S_LEN, 512])
    n_ph3 = len(guards)
    obuf = sb("obuf", [128, NT, 512])
    coef = sb("coef", [128, NT, 8])
    S.op("pool", "memset", writes=[coef], ap=coef[:], constant=0.0)
    ctmp = sb("ctmp", [128, 4, 128])
    S.dma("sp", ctmp[:], c_masks[:, :, :], writes=[ctmp])
    mT2 = [sb("mT2_%d" % d, [128, 2, 256], BF16) for d in range(2)]
    mN2 = [sb("mN2_%d" % d, [128, 2, 128], BF16) for d in range(2)]
    for d in range(2):
        src = ctmp[:, 0:2, :] if d == 0 else ctmp[:, 2:4, :]
        nsrc = ctmp[:, 2, :] if d == 0 else ctmp[:, 0, :]
        for e in range(2):
            S.op("dve", "tensor_copy", reads=[ctmp], writes=[mT2[d]], out=mT2[d][:, e, :].rearrange("p (a t) -> p a t", a=2), in_=src)
            S.op("dve", "tensor_copy", reads=[ctmp], writes=[mN2[d]], out=mN2[d][:, e, :], in_=nsrc)
    ident2 = sb("ident2", [128, 2, 128], BF16)
    for e in range(2):
        S.op("dve", "tensor_copy", reads=[identf], writes=[ident2], out=ident2[:, e, :], in_=identf[:])
    ident2s = sb("ident2s", [128, 64])
    S.op("dve", "tensor_tensor", reads=[identf], writes=[ident2s], out=ident2s[:], in0=identf[:, 0:64], in1=identf[:, 64:128], op=ALU.add)
    ctmp2 = sb("ctmp2", [128, 130])
    S.dma("sp", ctmp2[:, 0:128], c_bones[:, :], writes=[ctmp2])
    S.dma("sp", ctmp2[:, 128:130], c_sel[:, :], writes=[ctmp2])
    bones = sb("bones", [128, 128], BF16); selb = sb("selb", [128, 2], BF16)
    S.op("dve", "tensor_copy", reads=[ctmp2], writes=[bones], out=bones[:], in_=ctmp2[:, 0:128])
    S.op("dve", "tensor_copy", reads=[ctmp2], writes=[selb], out=selb[:], in_=ctmp2[:, 128:130])
    BLK = 512
    NB = S_LEN // BLK
    CPB = BLK // 128
    reset = sb("reset", [128, BLK])
    S.dma("sp", reset[:], c_reset[:, 0:BLK], writes=[reset])
    upf = sb("upf", [128, 1024]); wupb = sb("wupb", [128, 512], BF16); aupb = sb("aupb", [128, 512], BF16)
    S.dma("sp", upf[:, 0:512], w_up[:, :], writes=[upf])
    S.dma("sp", upf[:, 512:1024], a_up[:, :], writes=[upf])
    S.op("dve", "tensor_copy", reads=[upf], writes=[wupb], out=wupb[:], in_=upf[:, 0:512])
    S.op("dve", "tensor_copy", reads=[upf], writes=[aupb], out=aupb[:], in_=upf[:, 512:1024])

    def f32t(n):
        return sb(n, [128, BLK])
    rF, kF, alpha, sg, cs, cum, t1, t2, t3, t4, t5 = [f32t("p3_%d" % i) for i in range(11)]
    vtF = sb("vtF", [128, CPB, 128])
    sqb = sb("sqb", [128, BLK], BF16); xbb = sb("xbb", [128, BLK], BF16)
    etot = sb("etot", [128, CPB])
    NPB = 2
    prep = []
    for i in range(NPB):
        prep.append(dict(
            ar=sb("ar%d" % i, [128, CPB, 2, 128], BF16), kt=sb("kt%d" % i, [128, BLK], BF16), bt=sb("bt%d" % i, [128, BLK], BF16),
            kh=sb("kh%d" % i, [128, BLK], BF16), bh=sb("bh%d" % i, [128, BLK], BF16),
            atk=sb("atk%d" % i, [128, CPB, 128], BF16), bhk=sb("bhk%d" % i, [128, CPB, 128], BF16),
            khk=sb("khk%d" % i, [128, CPB, 128], BF16), vbf=sb("vbf%d" % i, [128, CPB, 128], BF16),
            etot=sb("etot%d" % i, [128, CPB])))
    NSLOT = 4
    slots = []
    for i in range(NSLOT):
        sl = dict(
            Z=None,
            arb=sb("arb%d" % i, [128, 2, 128], BF16), s2b=sb("s2b%d" % i, [128, 2, 256], BF16),
            P=[sb("P%d_%d" % (i, j), [128, 2, 128], BF16) for j in range(2)],
            PTT=[sb("PTT%d_%d" % (i, j), [128, 2, 256], BF16) for j in range(2)],
            ysb=sb("ysb%d" % i, [128, 2, 64], BF16), w1m=sb("w1m%d" % i, [128, 2, 128], BF16),
            phiT=sb("phiT%d" % i, [128, 64]), xT=sb("xT%d" % i, [128, 128]))
        sl["Z"] = T(pall.t[:, 2 * i:2 * i + 2, :], "Z%d" % i)
        sl["Z"].b.excl = True
        slots.append(sl)
    Hs = [sb("H%d" % i, [128, 64]) for i in range(2)]
    pbk = []
    for k_ in range(2 * NSLOT):
        t_ = T(pall.t[:, k_, :], "pbk%d" % k_)
        t_.b = slots[k_ // 2]["Z"].b
        pbk.append(t_)
    NPBK = len(pbk)
    pbi = 0
    evc = [0]

    def evac_copy(out, in_, reads, writes):
        evc[0] += 1
        if evc[0] % 2 == 0:
            S.op("act", "activation", reads=reads, writes=writes, out=out, in_=in_, func=AF.Copy)
        else:
            S.op("dve", "tensor_copy", reads=reads, writes=writes, out=out, in_=in_)

    def evac2(out3, in3, reads, writes, eng=None):
        if eng is None:
            evc[0] += 1
            eng = "act" if evc[0] % 2 == 0 else "dve"
        if eng == "act":
            for e in range(2):
                S.op("act", "activation", reads=reads, writes=writes, out=out3[:, e, :], in_=in3[:, e, :], func=AF.Copy)
        else:
            S.op("dve", "tensor_copy", reads=reads, writes=writes, out=out3, in_=in3)

    def v3(ap_, c=CPB):
        return ap_.rearrange("p (c t) -> p c t", c=c)

    if peer:
        UVB = dscr("UVB", [16384, 2 * D], BF16)
        cvf = [sb("cvf%d" % i, [128, 2 * D]) for i in range(2)]
        cvb = [sb("cvb%d" % i, [128, 2 * D], BF16) for i in range(2)]
        S.rec = True
        ci_ = 0
        for tbl, dst in ((peer_u, UVB[:, 0:D]), (peer_v, UVB[:, D:2 * D])):
            for c in range(64):
                f_, b_ = cvf[ci_ % 2], cvb[ci_ % 2]
                rows = slice(c * 256, (c + 1) * 256)
                S.dma("sp", f_[:], tbl[rows, :].rearrange("(p r) d -> p (r d)", r=2), writes=[f_])
                S.op("pool", "tensor_copy", reads=[f_], writes=[b_], out=b_[:], in_=f_[:])
                S.dma("sp", dst[rows, :].rearrange("(p r) d -> p r d", r=2), b_[:].rearrange("p (r d) -> p r d", r=2), reads=[b_])
                ci_ += 1
        S.rec = False
        cv_pending = S.pending
        S.pending = []
    else:
        cv_pending = []

    def cv_flush(n):
        nonlocal cv_pending
        keep = S.pending
        S.pending = cv_pending
        S.flush(n)
        cv_pending = S.pending
        S.pending = keep

    for hp in range(4):
        hsl = slice(hp * 128, (hp + 1) * 128)
        for d in range(2):
            hcur = 0
            S.op("pool", "memset", writes=[Hs[0]], ap=Hs[0][:], constant=0.0)
            blks = range(NB) if d == 0 else range(NB - 1, -1, -1)
            for bi, blk in enumerate(blks):
                if hp * 16 + d * 8 + bi >= nblk:
                    continue
                cv_flush(6)
                tsl = slice(blk * BLK, (blk + 1) * BLK)
                pr = prep[bi % NPB]
                S.dma("sp", rF[:], RT[hsl, tsl], writes=[rF])
                S.dma("act", kF[:], KT[hsl, tsl], writes=[kF])
                S.dma("sp", vtF[:], VTOK[tsl, hsl].rearrange("(n p) c -> p n c", p=128), writes=[vtF])
                S.op("pool", "tensor_copy", reads=[vtF], writes=[pr["vbf"]], out=pr["vbf"][:], in_=vtF[:])
                bk = pbk[pbi % NPBK]; pbi += 1
                S.op("pe", "matmul", reads=[aupb, adT], writes=[bk], out=bk[:, :], lhsT=aupb[64 * d:64 * d + 64, hsl],
                     rhs=adT[64 * d:64 * d + 64, tsl], start=True, stop=True)
                S.op("act", "activation", reads=[bk, pc], writes=[alpha], out=alpha[:], in_=bk[:, :], func=AF.Sigmoid,
                     bias=pc[:, col["a0_%d" % d] + hp:col["a0_%d" % d] + hp + 1])
                bk = pbk[pbi % NPBK]; pbi += 1
                S.op("pe", "matmul", reads=[wupb, twd], writes=[bk], out=bk[:, :], lhsT=wupb[64 * d:64 * d + 64, hsl],
                     rhs=twd[64 * d:64 * d + 64, tsl], start=True, stop=True)
                S.op("act", "activation", reads=[bk, pc], writes=[sg], out=sg[:], in_=bk[:, :], func=AF.Sigmoid,
                     bias=pc[:, col["w0_%d" % d] + hp:col["w0_%d" % d] + hp + 1])
                S.op("dve", "tensor_tensor_scan", reads=[reset, sg], writes=[cs], out=cs[:], data0=reset[:], data1=sg[:],
                     initial=0.0, op0=ALU.mult, op1=ALU.add)
                S.op("act", "activation", reads=[cs], writes=[pr["etot"]], out=pr["etot"][:], in_=v3(cs[:])[:, :, 127], func=AF.Exp,
                     scale=-C0)
                if d == 0:
                    cm = cs
                else:
                    S.op("dve", "tensor_tensor", reads=[sg, cs], writes=[t1], out=t1[:], in0=sg[:], in1=cs[:], op=ALU.subtract)
                    S.op("dve", "tensor_tensor", reads=[t1, cs], writes=[cum], out=v3(cum[:]), in0=v3(t1[:]),
                         in1=v3(cs[:])[:, :, 127:128].to_broadcast([128, CPB, 128]), op=ALU.add)
                    cm = cum
                S.op("pool", "tensor_tensor", reads=[cm, sg], writes=[t1], out=t1[:], in0=cm[:], in1=sg[:], op=ALU.subtract)
                S.op("act", "activation", reads=[t1], writes=[t1], out=t1[:], in_=t1[:], func=AF.Exp, scale=-C0)
                S.op("act", "activation", reads=[cm], writes=[t2], out=t2[:], in_=cm[:], func=AF.Exp, scale=-C0)
                S.op("act", "activation", reads=[cm], writes=[t3], out=t3[:], in_=cm[:], func=AF.Exp, scale=C0)
                S.op("dve", "tensor_scalar", reads=[kF, pc], writes=[t4], out=t4[:], in0=kF[:],
                     scalar1=pc[:, col["k_k"] + hp:col["k_k"] + hp + 1], scalar2=None, op0=ALU.mult)
                S.op("pool", "tensor_tensor", reads=[t4], writes=[sqb], out=sqb[:], in0=t4[:], in1=t4[:], op=ALU.mult)
                bk = pbk[pbi % NPBK]; pbi += 1
                S.op("pe", "matmul", reads=[bones, sqb], writes=[bk], out=bk[:, :], lhsT=bones[:], rhs=sqb[:], start=True, stop=True)
                S.op("act", "activation", reads=[bk], writes=[t5], out=t5[:], in_=bk[:, :], func=AF.Sqrt)
                S.op("dve", "tensor_scalar", reads=[t5], writes=[t5], out=t5[:], in0=t5[:], scalar1=1e-12, scalar2=None, op0=ALU.max)
                S.op("dve", "reciprocal", reads=[t5], writes=[t5], out=t5[:], in_=t5[:])
                S.op("dve", "tensor_tensor", reads=[t4, t5], writes=[t4], out=t4[:], in0=t4[:], in1=t5[:], op=ALU.mult)
                S.op("act", "activation", reads=[alpha, pc], writes=[t5], out=t5[:], in_=alpha[:], func=AF.Identity,
                     scale=pc[:, col["k_a"] + hp:col["k_a"] + hp + 1], bias=pc[:, col["omka"] + hp:col["omka"] + hp + 1])
                S.op("dve", "tensor_tensor", reads=[t5, kF], writes=[t5], out=t5[:], in0=t5[:], in1=kF[:], op=ALU.mult)
                ar = pr["ar"]
                S.op("dve", "tensor_tensor", reads=[rF, t2], writes=[ar], out=ar[:, :, 1, :], in0=v3(rF[:]), in1=v3(t2[:]), op=ALU.mult)
                S.op("dve", "scalar_tensor_tensor", reads=[t4, t1], writes=[ar], out=ar[:, :, 0, :], in0=v3(t4[:]), scalar=-1.0,
                     in1=v3(t1[:]), op0=ALU.mult, op1=ALU.mult)
                S.op("pool", "tensor_tensor", reads=[t5, t3], writes=[pr["kt"]], out=pr["kt"][:], in0=t5[:], in1=t3[:], op=ALU.mult)
                S.op("pool", "tensor_tensor", reads=[t4, alpha], writes=[t2], out=t2[:], in0=t4[:], in1=alpha[:], op=ALU.mult)
                S.op("pool", "tensor_tensor", reads=[t2, t3], writes=[pr["bt"]], out=pr["bt"][:], in0=t2[:], in1=t3[:], op=ALU.mult)
                etb = pr["etot"][:, 0:CPB].unsqueeze(2).to_broadcast([128, CPB, 128])
                S.op("dve", "tensor_tensor", reads=[pr["kt"], pr["etot"]], writes=[pr["kh"]], out=v3(pr["kh"][:]), in0=v3(pr["kt"][:]),
                     in1=etb, op=ALU.mult)
                S.op("dve", "tensor_tensor", reads=[pr["bt"], pr["etot"]], writes=[pr["bh"]], out=v3(pr["bh"][:]), in0=v3(pr["bt"][:]),
                     in1=etb, op=ALU.mult)
                S.op("dve", "scalar_tensor_tensor", reads=[rF, pc, t5], writes=[xbb], out=xbb[:], in0=rF[:],
                     scalar=pc[:, col["r_k"] + hp:col["r_k"] + hp + 1], in1=t5[:], op0=ALU.mult, op1=ALU.mult)
                bk = pbk[pbi % NPBK]; pbi += 1
                for c in range(CPB):
                    S.op("pe", "matmul", reads=[xbb, selb], writes=[bk], out=bk[:, 2 * c:2 * c + 2], lhsT=xbb[:, c * 128:(c + 1) * 128],
                         rhs=selb[:], start=True, stop=True)
                cf = coef[:, blk * CPB:(blk + 1) * CPB, 2 * hp:2 * hp + 2]
                S.op("dve", "tensor_tensor", reads=[bk, coef], writes=[coef], out=cf, in0=bk[:, 0:2 * CPB].rearrange("p (c e) -> p c e", e=2),
                     in1=cf, op=ALU.add)
                for nm, srcT in (("atk", None), ("bhk", pr["bh"]), ("khk", pr["kh"])):
                    bk = pbk[pbi % NPBK]; pbi += 1
                    bkb = bk.t[:].bitcast(BF16)
                    for c in range(CPB):
                        in_ = ar[:, c, 0, :] if srcT is None else srcT[:, c * 128:(c + 1) * 128]
                        S.op("pe", "transpose", reads=[ar if srcT is None else srcT, identb], writes=[bk],
                             out=bkb[:, c * 128:(c + 1) * 128], in_=in_, identity=identb[:])
                    evac_copy(pr[nm][:], bkb[:, 0:CPB * 128].rearrange("p (c t) -> p c t", c=CPB), [bk], [pr[nm]])

                if cut <= 1:
                    continue
                corder = list(range(CPB)) if d == 0 else list(range(CPB - 1, -1, -1))
                for g0 in range(0, CPB, NSLOT):
                    grp = corder[g0:g0 + NSLOT]
                    for si, c in enumerate(grp):
                        sl = slots[si]
                        Z = sl["Z"]
                        csl = slice(c * 128, (c + 1) * 128)
                        for e in range(2):
                            ps_ = slice(64 * e, 64 * e + 64)
                            S.op("pe", "matmul", reads=[pr["bt"], ar], writes=[Z], out=Z[:, e, 0:256], lhsT=pr["bt"][ps_, csl],
                                 rhs=ar[ps_, c, :, :], start=True, stop=True)
                            S.op("pe", "matmul", reads=[pr["kt"], ar], writes=[Z], out=Z[:, e, 256:512], lhsT=pr["kt"][ps_, csl],
                                 rhs=ar[ps_, c, :, :], start=True, stop=True)
                        PTT0 = sl["PTT"][0]; PTT1 = sl["PTT"][1]
                        S.op("dve", "tensor_tensor", reads=[Z, mT2[d]], writes=[PTT0], out=PTT0[:, :, 0:128], in0=Z[:, :, 0:128],
                             in1=mT2[d][:, :, 0:128], op=ALU.mult)
                        S.op("dve", "tensor_tensor", reads=[Z, mT2[d]], writes=[sl["arb"]], out=sl["arb"][:], in0=Z[:, :, 128:256],
                             in1=mT2[d][:, :, 128:256], op=ALU.mult)
                        S.op("dve", "tensor_tensor", reads=[Z, mT2[d]], writes=[sl["s2b"]], out=sl["s2b"][:], in0=Z[:, :, 256:512], in1=mT2[d][:],
                             op=ALU.mult)
                        S.op("pool", "tensor_tensor", reads=[PTT0, ident2], writes=[PTT1], out=PTT1[:, :, 128:256], in0=PTT0[:, :, 0:128],
                             in1=ident2[:], op=ALU.add)
                    for si, c in enumerate(grp):
                        sl = slots[si]
                        Z = sl["Z"]
                        for e in range(2):
                            ps_ = slice(64 * e, 64 * e + 64)
                            S.op("pe", "matmul", reads=[ar, pr["bt"]], writes=[Z], out=Z[:, e, 0:128], lhsT=ar[ps_, c, 0, :],
                                 rhs=pr["bt"][ps_, c * 128:(c + 1) * 128], start=True, stop=True)
                        S.op("dve", "tensor_tensor", reads=[Z, mN2[d]], writes=[sl["P"][0]], out=sl["P"][0][:], in0=Z[:, :, 0:128], in1=mN2[d][:],
                             op=ALU.mult)
                    if cut <= 2:
                        continue
                    for k in range(7):
                        for si, c in enumerate(grp):
                            sl = slots[si]
                            Z = sl["Z"]
                            Px, Py = sl["P"][k % 2], sl["P"][(k + 1) % 2]
                            Tx, Ty = sl["PTT"][k % 2], sl["PTT"][(k + 1) % 2]
                            for e in range(2):
                                if k == 0:
                                    S.op("pe", "matmul", reads=[Px, Tx], writes=[Z], out=Z[:, e, 0:128], lhsT=Px[:, e, :],
                                         rhs=Tx[:, e, 0:128], start=True, stop=True)
                                elif k <= 4:
                                    S.op("pe", "matmul", reads=[Px, Tx], writes=[Z], out=Z[:, e, 0:256], lhsT=Px[:, e, :],
                                         rhs=Tx[:, e, :], start=True, stop=True)
                                else:
                                    S.op("pe", "matmul", reads=[Px, Tx], writes=[Z], out=Z[:, e, 128:256], lhsT=Px[:, e, :],
                                         rhs=Tx[:, e, 128:256], start=True, stop=True)
                                if k <= 5:
                                    S.op("pe", "matmul", reads=[Px, Tx], writes=[Z], out=Z[:, e, 256:384], lhsT=Tx[:, e, 0:128],
                                         rhs=Px[:, e, :], start=True, stop=True)
                            if k <= 5:
                                evac2(Py[:], Z[:, :, 256:384], [Z], [Py], eng="act")
                            if k <= 4:
                                S.op("dve", "tensor_copy", reads=[Z], writes=[Ty], out=Ty[:, :, 0:128], in_=Z[:, :, 0:128])
                            if k >= 1:
                                S.op("dve", "tensor_tensor", reads=[Z, Tx], writes=[Ty], out=Ty[:, :, 128:256], in0=Z[:, :, 128:256],
                                     in1=Tx[:, :, 128:256], op=ALU.add)
                    if cut <= 3:
                        continue
                    TTF = 1
                    for si, c in enumerate(grp):
                        sl = slots[si]
                        Z = sl["Z"]
                        for e in range(2):
                            S.op("pe", "matmul", reads=[sl["s2b"], pr["vbf"]], writes=[Z], out=Z[:, e, 384:448], lhsT=sl["s2b"][:, e, 0:128],
                                 rhs=pr["vbf"][:, c, 64 * e:64 * e + 64], start=True, stop=True)
                        evac2(sl["ysb"][:], Z[:, :, 384:448], [Z], [sl["ysb"]], eng="act")
                    if cut <= 3.1:
                        continue
                    for si, c in enumerate(grp):
                        sl = slots[si]
                        Z = sl["Z"]
                        TT = sl["PTT"][TTF]
                        for e in range(2):
                            S.op("pe", "matmul", reads=[TT, sl["ysb"]], writes=[Z], out=Z[:, e, 0:64], lhsT=TT[:, e, 128:256],
                                 rhs=sl["ysb"][:, e, :], start=True, stop=True)
                            S.op("pe", "matmul", reads=[TT, pr["atk"]], writes=[Z], out=Z[:, e, 64:128], lhsT=TT[:, e, 128:256],
                                 rhs=pr["atk"][:, c, 64 * e:64 * e + 64], start=True, stop=True)
                        evac2(sl["w1m"][:], Z[:, :, 0:128], [Z], [sl["w1m"]], eng="act")
                    if cut <= 3.2:
                        continue
                    for si, c in enumerate(grp):
                        sl = slots[si]
                        Z = sl["Z"]
                        w1m = sl["w1m"]
                        for e in range(2):
                            ps_ = slice(64 * e, 64 * e + 64)
                            S.op("pe", "matmul", reads=[w1m, pr["bhk"]], writes=[Z], out=Z[ps_, e, 448:512], lhsT=w1m[:, e, 64:128],
                                 rhs=pr["bhk"][:, c, 64 * e:64 * e + 64], start=True, stop=True)
                            S.op("pe", "matmul", reads=[w1m, sl["arb"]], writes=[Z], out=Z[ps_, e, 128:256], lhsT=w1m[:, e, 64:128],
                                 rhs=sl["arb"][:, e, :], start=True, stop=True)
                        if cut <= 3.3:
                            continue
                        for e in range(2):
                            ps_ = slice(64 * e, 64 * e + 64)
                            S.op("dve", "scalar_tensor_tensor", reads=[ident2s, pr["etot"], Z], writes=[sl["phiT"]], out=sl["phiT"][ps_, :],
                                 in0=ident2s[ps_, :], scalar=pr["etot"][ps_, c:c + 1], in1=Z[ps_, e, 448:512], op0=ALU.mult, op1=ALU.add)
                            S.op("dve", "tensor_tensor", reads=[Z, ar], writes=[sl["xT"]], out=sl["xT"][ps_, :], in0=Z[ps_, e, 128:256],
                                 in1=ar[ps_, c, 1, :], op=ALU.add)
                    if cut <= 4:
                        continue
                    for si, c in enumerate(grp):
                        sl = slots[si]
                        Z = sl["Z"]
                        w1m = sl["w1m"]
                        Hc, Hn = Hs[hcur], Hs[1 - hcur]
                        for e in range(2):
                            ps_ = slice(64 * e, 64 * e + 64)
                            vv = pr["vbf"][:, c, 64 * e:64 * e + 64]
                            S.op("pe", "matmul", reads=[sl["arb"], w1m], writes=[Z], out=Z[:, e, 256:320], lhsT=sl["arb"][:, e, :],
                                 rhs=w1m[:, e, 0:64], start=True, stop=False)
                            S.op("pe", "matmul", reads=[sl["s2b"], pr["vbf"]], writes=[Z], out=Z[:, e, 256:320],
                                 lhsT=sl["s2b"][:, e, 128:256], rhs=vv, start=False, stop=False)
                            S.op("pe", "matmul", reads=[sl["xT"], Hc], writes=[Z], out=Z[:, e, 256:320], lhsT=sl["xT"][ps_, :],
                                 rhs=Hc[ps_, :], start=False, stop=True)
                            S.op("pe", "matmul", reads=[pr["bhk"], w1m], writes=[Z], out=Z[ps_, e, 320:384], lhsT=pr["bhk"][:, c, 64 * e:64 * e + 64],
                                 rhs=w1m[:, e, 0:64], start=True, stop=False)
                            S.op("pe", "matmul", reads=[pr["khk"], pr["vbf"]], writes=[Z], out=Z[ps_, e, 320:384],
                                 lhsT=pr["khk"][:, c, 64 * e:64 * e + 64], rhs=vv, start=False, stop=False)
                            S.op("pe", "matmul", reads=[sl["phiT"], Hc], writes=[Z], out=Z[ps_, e, 320:384], lhsT=sl["phiT"][ps_, :],
                                 rhs=Hc[ps_, :], start=False, stop=True)
                        for e in range(2):
                            ps_ = slice(64 * e, 64 * e + 64)
                            S.op("act", "activation", reads=[Z], writes=[Hn], out=Hn[ps_, :], in_=Z[ps_, e, 320:384], func=AF.Copy)
                        hcur = 1 - hcur
                        tile_i = blk * CPB + c
                        osl = obuf[:, tile_i, hp * 128:(hp + 1) * 128].rearrange("p (e n) -> p e n", e=2)
                        if d == 0:
                            S.op("dve", "tensor_copy", reads=[Z], writes=[obuf], out=osl, in_=Z[:, :, 256:320])
                        else:
                            S.op("dve", "tensor_tensor", reads=[Z, obuf], writes=[obuf], out=osl, in0=Z[:, :, 256:320], in1=osl, op=ALU.add)

    cv_flush(None)
    for q in range(8):
        S.dma("sp" if q % 2 == 0 else "act", OB[q * 512:(q + 1) * 512, :].rearrange("(n p) c -> p n c", p=128), obuf[:, q * 4:(q + 1) * 4, :], reads=[obuf])
    CF = dscr("CF", [128, NT * 8])
    S.dma("sp", CF[:, :], coef[:].rearrange("p n e -> p (n e)"), reads=[coef])
    S.barrier()
    release(len(guards) - n_ph3)

    X1 = dscr("X1", [S_LEN, D])
    coef = sb("coef5", [128, NT * 8])
    S.dma("sp", coef[:], CF[:, :], writes=[coef])
    lnw_b = sb("lnw_b", [128, 512]); lnb_b = sb("lnb_b", [128, 512]); wb2 = sb("nw2b", [128, D]); wfb = sb("nwfb", [128, D])
    S.dma("sp", lnw_b[:], ln_x_w.partition_broadcast(128), writes=[lnw_b])
    S.dma("sp", lnb_b[:], ln_x_b.partition_broadcast(128), writes=[lnb_b])
    S.dma("sp", wb2[:], norm2_w.partition_broadcast(128), writes=[wb2])
    S.dma("sp", wfb[:], norm_f_w.partition_broadcast(128), writes=[wfb])
    stg = sb("stg", [128, 2048])
    gupA = sb("gupA", [128, 512], BF16); gupB = sb("gupB", [32, 512], BF16)
    S.dma("sp", stg[:, 0:512], g_up[0:128, :], writes=[stg])
    S.dma("sp", stg[0:32, 512:1024], g_up[128:160, :], writes=[stg])
    S.op("dve", "tensor_copy", reads=[stg], writes=[gupA], out=gupA[:], in_=stg[:, 0:512])
    S.op("dve", "tensor_copy", reads=[stg], writes=[gupB], out=gupB[:], in_=stg[0:32, 512:1024])
    woutb = sb("woutb", [128, 8, D], BF16)
    for kc in range(0, 8, 2):
        S.dma("sp", stg[:].rearrange("p (k n) -> p k n", k=2), w_out[:, kc:kc + 2, :], writes=[stg])
        S.op("dve", "tensor_copy", reads=[stg], writes=[woutb], out=woutb[:, kc:kc + 2, :], in_=stg[:].rearrange("p (k n) -> p k n", k=2))
    if peer:
        wqb = sb("wqb", [128, 8, 2048], BF16)
        for kc in range(8):
            S.dma("sp", stg[:], wq[:, kc, :], writes=[stg])
            S.op("dve", "tensor_copy", reads=[stg], writes=[wqb], out=wqb[:, kc, :], in_=stg[:])
        keyb = sb("keyb", [128, 16, 128], BF16)
        S.dma("sp", stg[:].rearrange("p (k n) -> p k n", k=16), keysT[:, :, :], writes=[stg])
        S.op("dve", "tensor_copy", reads=[stg], writes=[keyb], out=keyb[:], in_=stg[:].rearrange("p (k n) -> p k n", k=16))
        iota_i = sb("iota_i", [128, 256], I32); iota_f = sb("iota_f", [128, 256])
        S.op("pool", "iota", writes=[iota_i], out=iota_i[:], pattern=[[1, 256]], base=0, channel_multiplier=0)
        S.op("dve", "tensor_copy", reads=[iota_i], writes=[iota_f], out=iota_f[:], in_=iota_i[:])
        sc2 = sb("sc2", [128, 2048])
        m1 = sb("m1", [128, 256]); i1 = sb("i1", [128, 256], U32); idxf = sb("idxf", [128, 256]); idx128 = sb("idx128", [128, 256])
        cand = sb("cand", [128, 2048])
        r1u = sb("r1u", [128, 128], U32); r2u = sb("r2u", [128, 128], U32); r2f = sb("r2f", [128, 128])
        e1v = sb("e1v", [128, 128]); e2v = sb("e2v", [128, 128])
        sc16 = sb("sc16", [128, 128]); pos = sb("pos", [128, 128], U32); posf = sb("posf", [128, 128])
        eidf = sb("eidf", [128, 128]); eid = sb("eid", [128, 128], U32)
        gsm = sb("gsm", [128, 16]); gate = sb("gate", [128, 128]); aact = sb("aact", [128, 128]); wgt = sb("wgt", [128, 128])
        junk = sb("junk", [128, D], BF16)
        NG = 6
        m1c = [Buf() for _ in range(16)]; i1c = [Buf() for _ in range(16)]; sc2c = [Buf() for _ in range(16)]
        s16c = [Buf() for _ in range(8)]; posc = [Buf() for _ in range(8)]
        eidc = [Buf() for _ in range(128)]; aactc = [Buf() for _ in range(128)]
        accb = Buf("accb")
        uvg = [sb("uvg%d" % i, [128, 2 * D], BF16) for i in range(NG)]
        dg = [sb("dg%d" % i, [128, 128], BF16) for i in range(NG)]
        gel = sb("gel", [128, 128]); gelc = [Buf() for _ in range(128)]
        gateb = sb("gateb", [128, 128]); h2bb = sb("h2bb", [128, D])
        qT = sb("qT", [128, 16, 128], BF16)
    ot = sb("ot", [128, 512]); tm = sb("tm5", [128, 512]); vt5 = sb("vt5", [128, 512])
    st8 = sb("st8", [128, 64])
    ybf = sb("ybf", [128, 512], BF16); yT = sb("yT5", [128, 4, 128], BF16); ypT = sb("ypT", [128, 4, 128], BF16)
    xt5 = sb("xt5", [128, D]); x1 = sb("x1", [128, D]); h2 = sb("h2", [128, D]); h2b = sb("h2b", [128, D], BF16)
    h2T = sb("h2T", [128, 8, 128], BF16)
    ssq5 = sb("ssq5", [128, 8])
    junk2 = sb("junk2", [128, D], BF16)
    P2 = T(pall.t[:, 0:2, :], "P2"); P2.b.excl = True
    P4 = T(pall.t[:, 4:6, :], "P4"); P4.b.excl = True
    PV = T(pall.t[:, 6:8, :], "PV"); PV.b.excl = True
    b2, b3 = banks[2], banks[3]

    def rms(src, dst_f32, wbt, k0):
        S.op("act", "activation", reads=[src], writes=[junk2, ssq5], out=junk2[:], in_=src[:], func=AF.Square,
             accum_out=ssq5[:, k0:k0 + 1])
        S.op("act", "activation", reads=[ssq5], writes=[ssq5], out=ssq5[:, k0 + 1:k0 + 2], in_=ssq5[:, k0:k0 + 1], func=AF.Sqrt,
             scale=1.0 / D, bias=epsc[:, 0:1])
        S.op("dve", "reciprocal", reads=[ssq5], writes=[ssq5], out=ssq5[:, k0 + 2:k0 + 3], in_=ssq5[:, k0 + 1:k0 + 2])
        S.op("dve", "scalar_tensor_tensor", reads=[src, ssq5, wbt], writes=[dst_f32], out=dst_f32[:], in0=src[:],
             scalar=ssq5[:, k0 + 2:k0 + 3], in1=wbt[:], op0=ALU.mult, op1=ALU.mult)

    x1s = [x1, sb("x1b", [128, D])]
    h2s = [h2, h2bb] if peer else [h2, h2]
    gates = [gate, gateb] if peer else None
    eids = [eid, sb("eidb", [128, 128], U32)] if peer else None

    def partA(i):
        tsl = slice(i * 128, (i + 1) * 128)
        x1 = x1s[i % 2]
        eid = eids[i % 2] if peer else None
        h2 = h2s[i % 2]
        gate = gates[i % 2] if peer else None
        S.dma("sp", ot[:], OB[tsl, :], writes=[ot])
        S.dma("act", vt5[:], VTOK[tsl, :], writes=[vt5])
        S.dma("sp", ypT[:], YP[:, tsl].rearrange("(g p) t -> p g t", p=128), writes=[ypT])
        S.dma("act", xt5[:], x[tsl, :], writes=[xt5])
        o3 = ot[:].rearrange("p (h n) -> p h n", h=8)
        t3 = tm[:].rearrange("p (h n) -> p h n", h=8)
        S.op("dve", "tensor_reduce", reads=[ot], writes=[st8], out=st8[:, 0:8], in_=o3, axis=AX.X, op=ALU.add)
        S.op("pool", "tensor_tensor", reads=[ot], writes=[tm], out=tm[:], in0=ot[:], in1=ot[:], op=ALU.mult)
        S.op("dve", "tensor_reduce", reads=[tm], writes=[st8], out=st8[:, 8:16], in_=t3, axis=AX.X, op=ALU.add)
        S.op("dve", "tensor_scalar", reads=[st8], writes=[st8], out=st8[:, 16:24], in0=st8[:, 0:8], scalar1=1.0 / 64, scalar2=None, op0=ALU.mult)
        S.op("dve", "tensor_tensor", reads=[st8], writes=[st8], out=st8[:, 24:32], in0=st8[:, 16:24], in1=st8[:, 16:24], op=ALU.mult)
        S.op("dve", "scalar_tensor_tensor", reads=[st8], writes=[st8], out=st8[:, 32:40], in0=st8[:, 8:16], scalar=1.0 / 64, in1=st8[:, 24:32],
             op0=ALU.mult, op1=ALU.subtract)
        S.op("act", "activation", reads=[st8], writes=[st8], out=st8[:, 40:48], in_=st8[:, 32:40], func=AF.Sqrt, bias=epsc[:, 1:2])
        S.op("dve", "reciprocal", reads=[st8], writes=[st8], out=st8[:, 48:56], in_=st8[:, 40:48])
        S.op("dve", "tensor_tensor", reads=[ot, st8], writes=[tm], out=t3, in0=o3, in1=st8[:, 16:24].unsqueeze(2).to_broadcast([128, 8, 64]),
             op=ALU.subtract)
        S.op("dve", "tensor_tensor", reads=[tm, st8], writes=[tm], out=t3, in0=t3, in1=st8[:, 48:56].unsqueeze(2).to_broadcast([128, 8, 64]),
             op=ALU.mult)
        S.op("pool", "tensor_tensor", reads=[tm, lnw_b], writes=[tm], out=tm[:], in0=tm[:], in1=lnw_b[:], op=ALU.mult)
        S.op("pool", "tensor_tensor", reads=[tm, lnb_b], writes=[tm], out=tm[:], in0=tm[:], in1=lnb_b[:], op=ALU.add)
        S.op("dve", "tensor_tensor", reads=[vt5, coef], writes=[vt5], out=vt5[:].rearrange("p (h n) -> p h n", h=8),
             in0=vt5[:].rearrange("p (h n) -> p h n", h=8), in1=coef[:, i * 8:(i + 1) * 8].unsqueeze(2).to_broadcast([128, 8, 64]), op=ALU.mult)
        S.op("dve", "tensor_tensor", reads=[tm, vt5], writes=[tm], out=tm[:], in0=tm[:], in1=vt5[:], op=ALU.add)
        S.op("pe", "matmul", reads=[sgA, gupA], writes=[b2], out=b2[:, :], lhsT=sgA[:, tsl], rhs=gupA[:], start=True, stop=False)
        S.op("pe", "matmul", reads=[sgB, gupB], writes=[b2], out=b2[:, :], lhsT=sgB[:, tsl], rhs=gupB[:], start=False, stop=True)
        S.op("dve", "tensor_tensor", reads=[tm, b2], writes=[ybf], out=ybf[:], in0=tm[:], in1=b2[:, :], op=ALU.mult)
        b3b = b3.t.bitcast(BF16)
        for q in range(4):
            S.op("pe", "transpose", reads=[ybf, identb], writes=[b3], out=b3b[:, q * 128:(q + 1) * 128], in_=ybf[:, q * 128:(q + 1) * 128],
                 identity=identb[:])
        S.op("act", "activation", reads=[b3], writes=[yT], out=yT[:], in_=b3b[:, 0:512].rearrange("p (q t) -> p q t", q=4), func=AF.Copy)
        for hf in range(2):
            for kc in range(8):
                lt = yT[:, kc, :] if kc < 4 else ypT[:, kc - 4, :]
                S.op("pe", "matmul", reads=[yT, ypT, woutb], writes=[P2], out=P2[:, hf, :], lhsT=lt, rhs=woutb[:, kc, hf * 512:(hf + 1) * 512],
                     start=(kc == 0), stop=(kc == 7))
        S.op("dve", "tensor_tensor", reads=[P2, xt5], writes=[x1], out=x1[:].rearrange("p (a n) -> p a n", a=2), in0=P2[:, :, :],
             in1=xt5[:].rearrange("p (a n) -> p a n", a=2), op=ALU.add)
        if "X1" in debug:
            S.dma("sp", X1[tsl, :], x1[:], reads=[x1])
        if not peer:
            rms(x1, h2, wfb, 0)
            S.dma("sp", out[tsl, :], h2[:], reads=[h2])
            return
        rms(x1, h2, wb2, 0)
        S.op("pool", "tensor_copy", reads=[h2], writes=[h2b], out=h2b[:], in_=h2[:])
        for kc in range(8):
            S.op("pe", "transpose", reads=[h2b, identb], writes=[b3], out=b3b[:, kc * 128:(kc + 1) * 128], in_=h2b[:, kc * 128:(kc + 1) * 128],
                 identity=identb[:])
        S.op("act", "activation", reads=[b3], writes=[h2T], out=h2T[:], in_=b3b[:, :].rearrange("p (k t) -> p k t", k=8), func=AF.Copy)
        for c4 in range(4):
            bk = b2 if c4 % 2 == 0 else b3
            for cq in range(4):
                ch = c4 * 4 + cq
                for kc in range(8):
                    S.op("pe", "matmul", reads=[wqb, h2T], writes=[bk], out=bk[:, cq * 128:(cq + 1) * 128], lhsT=wqb[:, kc, ch * 128:(ch + 1) * 128],
                         rhs=h2T[:, kc, :], start=(kc == 0), stop=(kc == 7))
            S.op("act" if c4 % 2 == 0 else "dve", "activation" if c4 % 2 == 0 else "tensor_copy", reads=[bk], writes=[qT],
                 out=qT[:, c4 * 4:(c4 + 1) * 4, :], in_=bk[:, :].rearrange("p (c t) -> p c t", c=4), **({"func": AF.Copy} if c4 % 2 == 0 else {}))
        for half in range(2):
            for c8 in range(8):
                ch = half * 8 + c8
                S.op("pe", "matmul", reads=[qT, keyb], writes=[P4], out=P4[:, c8 // 4, (c8 % 4) * 128:(c8 % 4 + 1) * 128], lhsT=qT[:, ch, :],
                     rhs=keyb[:, ch, :], start=True, stop=True)
            S.op("dve", "tensor_copy", reads=[P4], writes=[stg], out=stg[:, half * 1024:(half + 1) * 1024].rearrange("p (a n) -> p a n", a=2),
                 in_=P4[:, :, :])
        scv = stg[:].rearrange("p (c n) -> p c n", c=16)
        sc2v = sc2[:].rearrange("p (c n) -> p c n", c=16)
        m1v = m1[:].rearrange("p (c n) -> p c n", c=16)
        i1v = i1[:].rearrange("p (c n) -> p c n", c=16)
        for ch in range(16):
            S.op("dve", "max", reads=[stg], writes=[m1c[ch]], out=m1v[:, ch, 0:8], in_=scv[:, ch, :])
        for ch in range(16):
            S.op("dve", "max_index", reads=[stg, m1c[ch]], writes=[i1c[ch]], out=i1v[:, ch, 0:8], in_max=m1v[:, ch, 0:8], in_values=scv[:, ch, :])
        for ch in range(16):
            S.op("dve", "match_replace", reads=[stg, m1c[ch]], writes=[sc2c[ch]], out=sc2v[:, ch, :], in_to_replace=m1v[:, ch, 0:8],
                 in_values=scv[:, ch, :], imm_value=-1e30)
        for ch in range(16):
            S.op("dve", "max", reads=[sc2c[ch]], writes=[m1c[ch]], out=m1v[:, ch, 8:16], in_=sc2v[:, ch, :])
        for ch in range(16):
            S.op("dve", "max_index", reads=[sc2c[ch], m1c[ch]], writes=[i1c[ch]], out=i1v[:, ch, 8:16], in_max=m1v[:, ch, 8:16],
                 in_values=sc2v[:, ch, :])
        S.op("dve", "tensor_copy", reads=i1c, writes=[idxf], out=idxf[:], in_=i1[:])
        S.op("dve", "tensor_scalar", reads=[idxf], writes=[idx128], out=idx128[:], in0=idxf[:], scalar1=128.0, scalar2=None, op0=ALU.mult)
        m4 = m1[:].rearrange("p (h a n) -> p h a n", h=8, a=2)
        x4 = idxf[:].rearrange("p (h a n) -> p h a n", h=8, a=2)
        y4 = idx128[:].rearrange("p (h a n) -> p h a n", h=8, a=2)
        c4v = cand[:].rearrange("p (h i j) -> p h i j", h=8, i=16)
        S.op("dve", "tensor_tensor", reads=m1c, writes=[cand], out=c4v, in0=m4[:, :, 0, :].unsqueeze(3).to_broadcast([128, 8, 16, 16]),
             in1=m4[:, :, 1, :].unsqueeze(2).to_broadcast([128, 8, 16, 16]), op=ALU.add)
        cv = cand[:].rearrange("p (h n) -> p h n", h=8)
        c2v = sc2[:].rearrange("p (h n) -> p h n", h=8)
        s16 = sc16[:].rearrange("p (h n) -> p h n", h=8)
        p16 = pos[:].rearrange("p (h n) -> p h n", h=8)
        for hh in range(8):
            S.op("dve", "max", reads=[cand], writes=[s16c[hh]], out=s16[:, hh, 0:8], in_=cv[:, hh, :])
        for hh in range(8):
            S.op("dve", "max_index", reads=[cand, s16c[hh]], writes=[posc[hh]], out=p16[:, hh, 0:8], in_max=s16[:, hh, 0:8], in_values=cv[:, hh, :])
        for hh in range(8):
            S.op("dve", "match_replace", reads=[cand, s16c[hh]], writes=[sc2c[2 * hh], sc2c[2 * hh + 1]], out=c2v[:, hh, :],
                 in_to_replace=s16[:, hh, 0:8], in_values=cv[:, hh, :], imm_value=-1e30)
        for hh in range(8):
            S.op("dve", "max", reads=[sc2c[2 * hh], sc2c[2 * hh + 1]], writes=[s16c[hh]], out=s16[:, hh, 8:16], in_=c2v[:, hh, :])
        for hh in range(8):
            S.op("dve", "max_index", reads=[sc2c[2 * hh], sc2c[2 * hh + 1], s16c[hh]], writes=[posc[hh]], out=p16[:, hh, 8:16],
                 in_max=s16[:, hh, 8:16], in_values=c2v[:, hh, :])
        S.op("dve", "tensor_scalar", reads=posc, writes=[r1u], out=r1u[:], in0=pos[:], scalar1=4, scalar2=None, op0=ALU.logical_shift_right)
        S.op("dve", "tensor_scalar", reads=posc, writes=[r2u], out=r2u[:], in0=pos[:], scalar1=15, scalar2=None, op0=ALU.bitwise_and)
        S.op("dve", "tensor_copy", reads=[r1u], writes=[posf], out=posf[:], in_=r1u[:])
        S.op("dve", "tensor_copy", reads=[r2u], writes=[r2f], out=r2f[:], in_=r2u[:])
        io4 = iota_f[:, 0:16].unsqueeze(1).unsqueeze(1).to_broadcast([128, 8, 16, 16])
        for (rf_, src4, dstv) in ((posf, y4[:, :, 0, :], e1v), (r2f, x4[:, :, 1, :], e2v)):
            S.op("dve", "tensor_tensor", reads=[iota_f, rf_], writes=[cand], out=c4v, in0=io4,
                 in1=rf_[:].rearrange("p (h j) -> p h j", h=8).unsqueeze(3).to_broadcast([128, 8, 16, 16]), op=ALU.is_equal)
            S.op("dve", "tensor_tensor", reads=[cand, idxf, idx128], writes=[cand], out=c4v, in0=c4v,
                 in1=src4.unsqueeze(2).to_broadcast([128, 8, 16, 16]), op=ALU.mult)
            S.op("dve", "tensor_reduce", reads=[cand], writes=[dstv], out=dstv[:], in_=cand[:].rearrange("p (q i) -> p q i", i=16), axis=AX.X, op=ALU.add)
        S.op("dve", "tensor_tensor", reads=[e1v, e2v], writes=eidc, out=eidf[:], in0=e1v[:], in1=e2v[:], op=ALU.add)
        S.op("dve", "tensor_scalar", reads=eidc, writes=eidc, out=eidf[:], in0=eidf[:], scalar1=0.0, scalar2=16383.0, op0=ALU.max, op1=ALU.min)
        S.op("dve", "tensor_copy", reads=eidc, writes=[eid], out=eid[:], in_=eidf[:])
        g3 = gate[:].rearrange("p (h n) -> p h n", h=8)
        S.op("dve", "tensor_tensor", reads=s16c, writes=[gate], out=g3, in0=s16, in1=s16[:, :, 0:1].to_broadcast([128, 8, 16]), op=ALU.subtract)
        S.op("act", "activation", reads=[gate], writes=[gate], out=gate[:], in_=gate[:], func=AF.Exp)
        S.op("dve", "tensor_reduce", reads=[gate], writes=[gsm], out=gsm[:, 0:8], in_=g3, axis=AX.X, op=ALU.add)
        S.op("dve", "reciprocal", reads=[gsm], writes=[gsm], out=gsm[:, 8:16], in_=gsm[:, 0:8])
        S.op("dve", "tensor_tensor", reads=[gate, gsm], writes=[gate], out=g3, in0=g3, in1=gsm[:, 8:16].unsqueeze(2).to_broadcast([128, 8, 16]),
             op=ALU.mult)

    LAG = 3

    def partUV(i, interleave):
        tsl = slice(i * 128, (i + 1) * 128)
        eid = eids[i % 2]
        h2 = h2s[i % 2]
        gate = gates[i % 2]
        per = (len(S.pending) + 127) // 128 if interleave else 0

        def tail(hj):
            uv_ = uvg[hj % NG]
            d_ = dg[hj % NG]
            S.op("dve", "scalar_tensor_tensor", reads=[identb, gelc[hj], gate], writes=[d_], out=d_[:], in0=identb[:], scalar=gel[:, hj:hj + 1],
                 in1=gate[:, hj:hj + 1].to_broadcast([128, 128]), op0=ALU.mult, op1=ALU.mult)
            for hf in range(2):
                S.op("pe", "matmul", reads=[d_, uv_], writes=[PV], out=PV[:, hf, :], lhsT=d_[:], rhs=uv_[:, D + hf * 512:D + (hf + 1) * 512],
                     start=(hj == 0), stop=(hj == 127))

        for hj in range(128):
            uv_ = uvg[hj % NG]
            S.dma("pool", uv_[:], UVB[:, :], reads=[eid], writes=[uv_], indirect=bass.IndirectOffsetOnAxis(ap=eid[:, hj:hj + 1], axis=0))
            S.op("dve", "scalar_tensor_tensor", reads=[uv_, h2], writes=[aactc[hj], accb], out=junk[:], in0=uv_[:, 0:D], scalar=1.0, in1=h2[:],
                 op0=ALU.mult, op1=ALU.mult, accum_out=aact[:, hj:hj + 1])
            S.op("act", "activation", reads=[aactc[hj]], writes=[gelc[hj]], out=gel[:, hj:hj + 1], in_=aact[:, hj:hj + 1], func=AF.Gelu)
            if hj >= LAG:
                tail(hj - LAG)
            if per:
                S.flush(per)
        for hj in range(128 - LAG, 128):
            tail(hj)
        S.flush()

    def partF(i):
        tsl = slice(i * 128, (i + 1) * 128)
        x1 = x1s[i % 2]
        S.op("dve", "tensor_tensor", reads=[PV, x1], writes=[x1], out=x1[:].rearrange("p (a n) -> p a n", a=2), in0=PV[:, :, :],
             in1=x1[:].rearrange("p (a n) -> p a n", a=2), op=ALU.add)
        rms(x1, x1, wfb, 4)
        S.dma("sp", out[tsl, :], x1[:], reads=[x1])

    if not peer:
        for i in range(NT):
            partA(i)
    else:
        partA(0)
        for i in range(NT):
            if i + 1 < NT:
                S.rec = True
                partA(i + 1)
                S.rec = False
            partUV(i, True)
            partF(i)

    S.barrier()
    print("ops", S.nops, "waits", S.nwait, "dmas", S.dcount)
    return nc


def make_consts():
    idx = np.arange(128)
    ident = np.eye(128, dtype=np.float32)
    mus = (idx[None, :] > idx[:, None]).astype(np.float32)
    mui = (idx[None, :] >= idx[:, None]).astype(np.float32)
    mls = (idx[None, :] < idx[:, None]).astype(np.float32)
    mli = (idx[None, :] <= idx[:, None]).astype(np.float32)
    masks = np.stack([mus, mui, mls, mli], axis=1)
    bones = (idx[None, :] // 64 == idx[:, None] // 64).astype(np.float32)
    sel = (idx[:, None] // 64 == np.arange(2)[None, :]).astype(np.float32)
    reset = np.ones((128, 1024), np.float32)
    reset[:, ::128] = 0.0
    invcnt = np.zeros((128, 4, 16), np.float32)
    for gi, win in enumerate((2, 4, 8, 16)):
        half = win // 2
        for t in range(half):
            invcnt[:, gi, t] = 1.0 / (t + half)
        for q in range(half - 1):
            t = S_LEN - (half - 1) + q
            invcnt[:, gi, 8 + q] = 1.0 / (S_LEN - t + half)
    return dict(c_ident=ident, c_masks=masks, c_bones=bones, c_sel=sel, c_reset=reset, c_invcnt=invcnt)


def make_in_maps(inp, peer=True):
    f = lambda a: np.ascontiguousarray(np.asarray(a, dtype=np.float32))
    shared = dict(
        w_in=f(inp["w_in"][0].reshape(8, 128, D_IN).transpose(1, 0, 2)),
        shift_mu=f(inp["shift_mu"][0]), norm1_w=f(inp["norm1_w"][0]),
        w0=f(inp["w0"][0]), a0=f(inp["a0"][0]),
        w_up=f(inp["w_up"][0].reshape(128, 512)), a_up=f(inp["a_up"][0].reshape(128, 512)),
        g_up=f(inp["g_up"][0]), k_k=f(inp["k_k"][0]), k_a=f(inp["k_a"][0]), r_k=f(inp["r_k"][0].reshape(512)),
        ln_x_w=f(inp["ln_x_w"][0]), ln_x_b=f(inp["ln_x_b"][0]),
        pool_w=f(inp["pool_w"][0].transpose(1, 0, 2)), pool_scale=f(inp["pool_scale"][0]),
        w_out=f(inp["w_out"][0].reshape(8, 128, D).transpose(1, 0, 2)),
        norm2_w=f(inp["norm2_w"][0]), norm_f_w=f(inp["norm_f_w"]),
        wq=f(inp["peer_wq"][0].reshape(8, 128, 2048).transpose(1, 0, 2)),
        keysT=f(inp["peer_keys"][0].transpose(3, 0, 1, 2).reshape(128, 16, 128)),
        peer_u=f(inp["peer_u"][0]), peer_v=f(inp["peer_v"][0]),
    )
    shared.update(make_consts())
    if not peer:
        del shared["peer_u"], shared["peer_v"]
    xs = np.asarray(inp["x"], dtype=np.float32)
    return [dict(shared, x=np.ascontiguousarray(xs[c])) for c in range(8)]


def kernel(**inputs):
    nc = build()
    in_maps = make_in_maps(inputs)
    res = run_bass_kernel_spmd(nc, in_maps, core_ids=list(range(8)))
    return np.stack([np.asarray(r["out"], dtype=np.float32) for r in res.results], axis=0)
```

```python
import numpy as np
import ml_dtypes
import concourse.bass as bass
import concourse.mybir as mybir
from concourse.bass_utils import run_bass_kernel_spmd

F32 = mybir.dt.float32
BF16 = mybir.dt.bfloat16
I32 = mybir.dt.int32
U32 = mybir.dt.uint32
AF = mybir.ActivationFunctionType
ALU = mybir.AluOpType
AX = mybir.AxisListType

S_LEN = 4096
D = 1024
NT = S_LEN // 128
RWKV_IN = 1952
D_IN = 2464
C0 = float(np.exp(-0.5))
RMS_EPS = 1e-5
GN_EPS = 64e-5
PAD = 8


class Buf:
    __slots__ = ("name", "w", "r", "excl")

    def __init__(self, name=""):
        self.name = name
        self.w = None
        self.r = {}
        self.excl = False


class T:
    def __init__(self, t, name=""):
        self.t = t
        self.b = Buf(name)

    def __getitem__(self, k):
        return self.t[k]


def _b(x):
    return x.b if isinstance(x, T) else x


class Sched:
    def __init__(self, nc, ndma=32):
        self.nc = nc
        self.es = {"pe": nc.tensor, "act": nc.scalar, "dve": nc.vector, "pool": nc.gpsimd, "sp": nc.sync}
        self.sem = {}
        self.tick = {k: 0 for k in self.es}
        self.seen = {k: {} for k in self.es}
        self._ctx = []
        for k in self.es:
            cm = nc.semaphore("s_" + k)
            self.sem[k] = cm.__enter__()
            self._ctx.append(cm)
        self.ndma = ndma
        self.dsem = []
        for i in range(ndma):
            cm = nc.semaphore("d_%d" % i)
            self.dsem.append(cm.__enter__())
            self._ctx.append(cm)
        self.dcount = 0
        self.rec = False
        self.pending = []
        self.nwait = 0
        self.nops = 0

    def _need(self, e, deps):
        best = {}
        for d in deps:
            if d is None:
                continue
            key, val = d
            if key == e and e == "pe":
                continue
            if best.get(key, 0) < val:
                best[key] = val
        for key, val in best.items():
            if self.seen[e].get(key, 0) >= val:
                continue
            sem = self.sem[key] if isinstance(key, str) else self.dsem[key]
            self.es[e].wait_ge(sem, val)
            self.nwait += 1
            self.seen[e][key] = val

    def _deps(self, reads, writes):
        deps = []
        for b in reads:
            deps.append(b.w)
        for b in writes:
            deps.append(b.w)
            for k, v in b.r.items():
                deps.append((k, v))
        return deps

    def flush(self, n=None):
        pend = self.pending
        k = len(pend) if n is None else min(n, len(pend))
        todo, self.pending = pend[:k], pend[k:]
        rec, self.rec = self.rec, False
        for kind, a, kw in todo:
            if kind == "op":
                self.op(*a, **kw)
            else:
                self.dma(*a, **kw)
        self.rec = rec

    def op(self, e, name, reads=(), writes=(), **kw):
        if self.rec:
            self.pending.append(("op", (e, name, reads, writes), kw))
            return None
        reads = [_b(x) for x in reads]
        writes = [_b(x) for x in writes]
        writes = writes + [b for b in reads if b.excl and b not in writes]
        reads = [b for b in reads if not b.excl]
        self._need(e, self._deps(reads, writes))
        ins = getattr(self.es[e], name)(**kw)
        self.tick[e] += 1
        self.nops += 1
        ins.then_inc(self.sem[e], 1)
        tk = self.tick[e]
        for b in reads:
            b.r[e] = tk
        for b in writes:
            b.w = (e, tk)
            b.r = {}
        return ins

    def dma(self, e, out, in_, reads=(), writes=(), indirect=None, **kw):
        if self.rec:
            self.pending.append(("dma", (e, out, in_, reads, writes, indirect), kw))
            return None
        reads = [_b(x) for x in reads]
        writes = [_b(x) for x in writes]
        j = self.dcount
        self.dcount += 1
        slot = j % self.ndma
        rnd = j // self.ndma
        deps = self._deps(reads, writes)
        if rnd > 0:
            deps.append((slot, 16 * rnd))
        self._need(e, deps)
        if indirect is None:
            ins = self.es[e].dma_start(out=out, in_=in_, **kw)
        else:
            ins = self.es[e].indirect_dma_start(out=out, out_offset=None, in_=in_, in_offset=indirect, **kw)
        ins.then_inc(self.dsem[slot], 16)
        self.nops += 1
        val = 16 * (rnd + 1)
        for b in reads:
            b.r[slot] = val
        for b in writes:
            b.w = (slot, val)
            b.r = {}
        return ins

    def barrier(self):
        deps = [(k, self.tick[k]) for k in self.es if self.tick[k] > 0]
        for j in range(min(self.dcount, self.ndma)):
            cnt = (self.dcount - 1 - j) // self.ndma + 1
            deps.append((j, 16 * cnt))
        for e in self.es:
            self._need(e, deps)


def build(debug=(), peer=True, cut=99, nblk=99):
    nc = bass.Bass("TRN2", target_bir_lowering=False)
    S = Sched(nc)
    guards = []

    def din(name, shape, dt=F32):
        return nc.dram_tensor(name, list(shape), dt, kind="ExternalInput").ap()

    def dscr(name, shape, dt=F32):
        kind = "ExternalOutput" if name in debug else "Internal"
        return nc.dram_tensor(name, list(shape), dt, kind=kind).ap()

    def sb(name, shape, dt=F32):
        g = nc.sbuf_tensor(name, list(shape), dt)
        t = g.__enter__()
        guards.append(g)
        return T(t, name)

    def ps(name, shape, dt=F32):
        g = nc.psum_tensor(name, list(shape), dt)
        t = g.__enter__()
        guards.append(g)
        return T(t, name)

    def release(n):
        for _ in range(n):
            g = guards.pop()
            g.__exit__(None, None, None)

    x = din("x", [S_LEN, D])
    w_in = din("w_in", [128, 8, D_IN])
    shift_mu = din("shift_mu", [2, RWKV_IN])
    norm1_w = din("norm1_w", [D])
    w0 = din("w0", [2, 512]); a0 = din("a0", [2, 512])
    w_up = din("w_up", [128, 512]); a_up = din("a_up", [128, 512])
    g_up = din("g_up", [160, 512])
    k_k = din("k_k", [512]); k_a = din("k_a", [512]); r_k = din("r_k", [512])
    ln_x_w = din("ln_x_w", [512]); ln_x_b = din("ln_x_b", [512])
    pool_w = din("pool_w", [128, 4, 128]); pool_scale = din("pool_scale", [512])
    w_out = din("w_out", [128, 8, D])
    norm2_w = din("norm2_w", [D]); norm_f_w = din("norm_f_w", [D])
    wq = din("wq", [128, 8, 2048])
    keysT = din("keysT", [128, 16, 128])
    if peer:
        peer_u = din("peer_u", [16384, D]); peer_v = din("peer_v", [16384, D])
    c_ident = din("c_ident", [128, 128])
    c_masks = din("c_masks", [128, 4, 128])
    c_bones = din("c_bones", [128, 128])
    c_sel = din("c_sel", [128, 2])
    c_reset = din("c_reset", [128, 1024])
    c_invcnt = din("c_invcnt", [128, 4, 16])
    out = nc.dram_tensor("out", [S_LEN, D], F32, kind="ExternalOutput").ap()

    RT = dscr("RT", [512, S_LEN]); KT = dscr("KT", [512, S_LEN])
    VTOK = dscr("VTOK", [S_LEN, 512])
    YP = dscr("YP", [512, S_LEN], BF16)

    identf = sb("identf", [128, 128]); identb = sb("identb", [128, 128], BF16)
    S.dma("sp", identf[:], c_ident[:, :], writes=[identf])
    S.op("dve", "tensor_copy", reads=[identf], writes=[identb], out=identb[:], in_=identf[:])
    epsc = sb("epsc", [128, 2])
    S.op("pool", "memset", writes=[epsc], ap=epsc[:, 0:1], constant=RMS_EPS)
    S.op("pool", "memset", writes=[epsc], ap=epsc[:, 1:2], constant=GN_EPS)
    NPC = 16 + 16 + 16 + 8 + 8 + 4 + 4 + 4 + 4 + 4
    pc = sb("pc", [128, NPC])
    S.op("pool", "memset", writes=[pc], ap=pc[:], constant=0.0)
    col = {}
    o = 0

    def ldcol(name, vec, n):
        nonlocal o
        col[name] = o
        nfull = n // 128
        if nfull:
            S.dma("sp", pc[:, o:o + nfull], vec[0:nfull * 128].rearrange("(c p) -> p c", p=128), writes=[pc],
                  allow_slow_non_contiguous=True)
        rem = n - nfull * 128
        if rem:
            S.dma("sp", pc[0:rem, o + nfull:o + nfull + 1], vec[nfull * 128:n].rearrange("(c p) -> p c", p=rem),
                  writes=[pc], allow_slow_non_contiguous=True)
        o += (n + 127) // 128

    ldcol("mu0", shift_mu[0, :], RWKV_IN); ldcol("mu1", shift_mu[1, :], RWKV_IN)
    col["muc"] = o; o += 16
    ldcol("w0_0", w0[0, :], 512); ldcol("w0_1", w0[1, :], 512)
    ldcol("a0_0", a0[0, :], 512); ldcol("a0_1", a0[1, :], 512)
    ldcol("k_k", k_k, 512); ldcol("k_a", k_a, 512); ldcol("r_k", r_k, 512); ldcol("pscale", pool_scale, 512)
    col["omka"] = o; o += 4
    assert o == NPC, (o, NPC)
    S.op("dve", "tensor_tensor", reads=[pc], writes=[pc], out=pc[:, col["muc"]:col["muc"] + 16],
         in0=pc[:, col["mu0"]:col["mu0"] + 16], in1=pc[:, col["mu1"]:col["mu1"] + 16], op=ALU.add)
    S.op("dve", "tensor_scalar", reads=[pc], writes=[pc], out=pc[:, col["muc"]:col["muc"] + 16],
         in0=pc[:, col["muc"]:col["muc"] + 16], scalar1=-1.0, scalar2=1.0, op0=ALU.mult, op1=ALU.add)
    S.op("dve", "tensor_scalar", reads=[pc], writes=[pc], out=pc[:, col["omka"]:col["omka"] + 4],
         in0=pc[:, col["k_a"]:col["k_a"] + 4], scalar1=-1.0, scalar2=1.0, op0=ALU.mult, op1=ALU.add)

    twd = sb("twd", [128, S_LEN], BF16)
    adT = sb("adT", [128, S_LEN], BF16)
    sgA = sb("sgA", [128, S_LEN], BF16)
    sgB = sb("sgB", [32, S_LEN], BF16)

    pall = ps("pall", [128, 8, 512])
    banks = [T(pall.t[:, i, :], "bank%d" % i) for i in range(8)]
    for bk_ in banks:
        bk_.b.excl = True

    n_ph = len(guards)
    hT = sb("hT", [128, 8, S_LEN], BF16)
    pbuf = [sb("pbuf%d" % i, [128, S_LEN + 2 * PAD]) for i in range(2)]
    for pb in pbuf:
        S.op("pool", "memset", writes=[pb], ap=pb[:, 0:PAD], constant=0.0)
        S.op("pool", "memset", writes=[pb], ap=pb[:, PAD + S_LEN:], constant=0.0)
    tA = sb("tA", [128, S_LEN + 2 * PAD]); tB = sb("tB", [128, S_LEN + 2 * PAD])
    wb1 = sb("nw1b", [128, D])
    S.dma("sp", wb1[:], norm1_w.partition_broadcast(128), writes=[wb1])
    ssq = sb("ssq", [128, 4])
    hn = [sb("hn%d" % i, [128, D], BF16) for i in range(2)]
    wf = [sb("wf%d" % i, [128, 8, 128]) for i in range(2)]
    wb = [sb("wb%d" % i, [128, 8, 128], BF16) for i in range(2)]

    xts = [tA, tB]
    ptb = [T(banks[i].t[:].bitcast(BF16), "ptb%d" % i) for i in range(2)]
    for pt_, bk in zip(ptb, banks[:2]):
        pt_.b = bk.b
    for i in range(NT):
        xt = xts[i % 2]
        S.dma("sp" if i % 2 == 0 else "act", xt[:, 0:D], x[i * 128:(i + 1) * 128, :], writes=[xt])
        h_ = hn[i % 2]
        S.op("act", "activation", reads=[xt], writes=[h_, ssq], out=h_[:], in_=xt[:, 0:D], func=AF.Square,
             accum_out=ssq[:, 0:1])
        S.op("act", "activation", reads=[ssq], writes=[ssq], out=ssq[:, 1:2], in_=ssq[:, 0:1], func=AF.Sqrt,
             scale=1.0 / D, bias=epsc[:, 0:1])
        S.op("dve", "reciprocal", reads=[ssq], writes=[ssq], out=ssq[:, 2:3], in_=ssq[:, 1:2])
        S.op("dve", "scalar_tensor_tensor", reads=[xt, ssq, wb1], writes=[h_], out=h_[:], in0=xt[:, 0:D],
             scalar=ssq[:, 2:3], in1=wb1[:], op0=ALU.mult, op1=ALU.mult)
        pt_ = ptb[i % 2]
        for kc in range(8):
            S.op("pe", "transpose", reads=[h_, identb], writes=[pt_], out=pt_.t[:, kc * 128:(kc + 1) * 128],
                 in_=h_[:, kc * 128:(kc + 1) * 128], identity=identb[:])
        S.op("act" if i % 2 == 0 else "dve", "activation" if i % 2 == 0 else "tensor_copy", reads=[pt_], writes=[hT],
             out=hT[:, :, i * 128:(i + 1) * 128], in_=pt_.t.rearrange("p (k t) -> p k t", k=8),
             **({"func": AF.Copy} if i % 2 == 0 else {}))

    chunks = []
    for i in range(4):
        chunks.append((i * 128, 128, "r", i))
    for i in range(4):
        chunks.append((512 + i * 128, 128, "k", i))
    for i in range(4):
        chunks.append((1024 + i * 128, 128, "v", i))
    chunks.append((1536, 128, "wd", 0))
    chunks.append((1664, 128, "ad", 0))
    chunks.append((1792, 128, "gdA", 0))
    chunks.append((1920, 32, "gdB", 0))
    for i in range(4):
        chunks.append((RWKV_IN + i * 128, 128, "pool", i))

    poolw_f = sb("poolw_f", [128, 4, 128]); poolw_b = sb("poolw_b", [128, 4, 128], BF16)
    S.dma("sp", poolw_f[:], pool_w[:, :, :], writes=[poolw_f])
    S.op("pool", "tensor_copy", reads=[poolw_f], writes=[poolw_b], out=poolw_b[:], in_=poolw_f[:])
    invcnt = sb("invcnt", [128, 4, 16])
    S.dma("sp", invcnt[:], c_invcnt[:, :, :], writes=[invcnt])
    plb = sb("plb", [128, S_LEN], BF16)
    ypb = sb("ypb", [128, S_LEN], BF16)
    vtk = [sb("vtk%d" % i, [128, 4, 128]) for i in range(2)]

    evac_i = 0
    for ci, (c0, w, kind, idx) in enumerate(chunks):
        wf_, wb_, pb = wf[ci % 2], wb[ci % 2], pbuf[ci % 2]
        S.dma("sp", wf_[:, :, 0:w], w_in[:, :, c0:c0 + w], writes=[wf_])
        S.op("pool", "tensor_copy", reads=[wf_], writes=[wb_], out=wb_[:, :, 0:w], in_=wf_[:, :, 0:w])
        for j in range(8):
            bk = banks[2 + (evac_i % 4)]
            for kc in range(8):
                S.op("pe", "matmul", reads=[wb_, hT], writes=[bk], out=bk[0:w, :], lhsT=wb_[:, kc, 0:w],
                     rhs=hT[:, kc, j * 512:(j + 1) * 512], start=(kc == 0), stop=(kc == 7))
            if evac_i % 2 == 0:
                S.op("act", "activation", reads=[bk], writes=[pb], out=pb[0:w, PAD + j * 512:PAD + (j + 1) * 512],
                     in_=bk[0:w, :], func=AF.Copy)
            else:
                S.op("dve", "tensor_copy", reads=[bk], writes=[pb], out=pb[0:w, PAD + j * 512:PAD + (j + 1) * 512],
                     in_=bk[0:w, :])
            evac_i += 1
        if kind != "pool":
            cc = c0 // 128
            m0 = pc[0:w, col["mu0"] + cc:col["mu0"] + cc + 1]
            m1 = pc[0:w, col["mu1"] + cc:col["mu1"] + cc + 1]
            mc = pc[0:w, col["muc"] + cc:col["muc"] + cc + 1]
            HS = S_LEN // 2
            for hf in range(2):
                lo = PAD + hf * HS
                S.op("act", "activation", reads=[pb, pc], writes=[tA], out=tA[0:w, lo:lo + HS],
                     in_=pb[0:w, lo - 1:lo - 1 + HS], func=AF.Copy, scale=m0)
                S.op("dve", "scalar_tensor_tensor", reads=[pb, pc, tA], writes=[tA], out=tA[0:w, lo:lo + HS],
                     in0=pb[0:w, lo + 1:lo + 1 + HS], scalar=m1, in1=tA[0:w, lo:lo + HS], op0=ALU.mult, op1=ALU.add)
                S.op("dve", "scalar_tensor_tensor", reads=[pb, pc, tA], writes=[tB], out=tB[0:w, lo:lo + HS],
                     in0=pb[0:w, lo:lo + HS], scalar=mc, in1=tA[0:w, lo:lo + HS], op0=ALU.mult, op1=ALU.add)
            res = tB
            R_ = res[0:w, PAD:PAD + S_LEN]
            if kind == "r":
                S.dma("sp", RT[idx * 128:(idx + 1) * 128, :], R_, reads=[res])
            elif kind == "k":
                S.dma("sp", KT[idx * 128:(idx + 1) * 128, :], R_, reads=[res])
            elif kind == "v":
                for g4 in range(8):
                    bk = banks[6 + g4 % 2]
                    for q in range(4):
                        tt = g4 * 4 + q
                        S.op("pe", "transpose", reads=[res, identf], writes=[bk], out=bk[:, q * 128:(q + 1) * 128],
                             in_=res[:, PAD + tt * 128:PAD + (tt + 1) * 128], identity=identf[:])
                    vt = vtk[g4 % 2]
                    S.op("act", "activation", reads=[bk], writes=[vt], out=vt[:],
                         in_=bk.t.rearrange("p (n c) -> p n c", n=4), func=AF.Copy)
                    S.dma("act", VTOK[g4 * 512:(g4 + 1) * 512, idx * 128:(idx + 1) * 128].rearrange("(n p) c -> p n c", p=128),
                          vt[:], reads=[vt])
            elif kind == "wd":
                S.op("act", "activation", reads=[res], writes=[twd], out=twd[:], in_=R_, func=AF.Tanh)
            elif kind == "ad":
                S.op("act", "activation", reads=[res], writes=[adT], out=adT[:], in_=R_, func=AF.Copy)
            elif kind == "gdA":
                S.op("act", "activation", reads=[res], writes=[sgA], out=sgA[:], in_=R_, func=AF.Sigmoid)
            elif kind == "gdB":
                S.op("act", "activation", reads=[res], writes=[sgB], out=sgB[:], in_=R_, func=AF.Sigmoid)
        else:
            gi = idx
            win = (2, 4, 8, 16)[gi]
            half = win // 2
            W_ = S_LEN + 2 * PAD
            S.op("dve", "tensor_tensor", reads=[pb], writes=[tA], out=tA[:, 1:W_], in0=pb[:, 0:W_ - 1], in1=pb[:, 1:W_],
                 op=ALU.add)
            cur, oth = tA, tB
            lo_, hi_ = 1, W_
            sh = 1
            for lev in range(gi):
                nlo, nhi = lo_ + sh, hi_ - sh
                S.op("dve" if lev % 2 else "pool", "tensor_tensor", reads=[cur], writes=[oth], out=oth[:, nlo:nhi],
                     in0=cur[:, nlo - sh:nhi - sh], in1=cur[:, nlo + sh:nhi + sh], op=ALU.add)
                cur, oth = oth, cur
                lo_, hi_ = nlo, nhi
                sh *= 2
            S.op("dve", "scalar_tensor_tensor", reads=[cur, pb], writes=[plb], out=plb[:], in0=cur[:, PAD:PAD + S_LEN],
                 scalar=1.0 / win, in1=pb[:, PAD:PAD + S_LEN], op0=ALU.mult, op1=ALU.subtract)
            S.op("dve", "tensor_tensor", reads=[cur, invcnt], writes=[oth], out=oth[:, 0:half], in0=cur[:, PAD:PAD + half],
                 in1=invcnt[:, gi, 0:half], op=ALU.mult)
            S.op("dve", "tensor_tensor", reads=[oth, pb], writes=[plb], out=plb[:, 0:half], in0=oth[:, 0:half],
                 in1=pb[:, PAD:PAD + half], op=ALU.subtract)
            if half > 1:
                nr = half - 1
                S.op("dve", "tensor_tensor", reads=[cur, invcnt], writes=[oth], out=oth[:, 8:8 + nr],
                     in0=cur[:, PAD + S_LEN - nr:PAD + S_LEN], in1=invcnt[:, gi, 8:8 + nr], op=ALU.mult)
                S.op("dve", "tensor_tensor", reads=[oth, pb], writes=[plb], out=plb[:, S_LEN - nr:S_LEN],
                     in0=oth[:, 8:8 + nr], in1=pb[:, PAD + S_LEN - nr:PAD + S_LEN], op=ALU.subtract)
            for j in range(8):
                bk = banks[2 + (evac_i % 4)]
                evac_i += 1
                S.op("pe", "matmul", reads=[poolw_b, plb], writes=[bk], out=bk[:, :], lhsT=poolw_b[:, gi, :],
                     rhs=plb[:, j * 512:(j + 1) * 512], start=True, stop=True)
                S.op("act", "activation", reads=[bk, pc], writes=[ypb], out=ypb[:, j * 512:(j + 1) * 512], in_=bk[:, :],
                     func=AF.Copy, scale=pc[:, col["pscale"] + gi:col["pscale"] + gi + 1])
            S.dma("sp", YP[gi * 128:(gi + 1) * 128, :], ypb[:], reads=[ypb])

    S.barrier()
    release(len(guards) - n_ph)

    OB = dscr("OB", [S_LEN, 512])
    n_ph3 = len(guards)
    obuf = sb("obuf", [128, NT, 512])
    coef = sb("coef", [128, NT, 8])
    S.op("pool", "memset", writes=[coef], ap=coef[:], constant=0.0)
    ctmp = sb("ctmp", [128, 4, 128])
    S.dma("sp", ctmp[:], c_masks[:, :, :], writes=[ctmp])
    mT2 = [sb("mT2_%d" % d, [128, 2, 256], BF16) for d in range(2)]
    mN2 = [sb("mN2_%d" % d, [128, 2, 128], BF16) for d in range(2)]
    for d in range(2):
        src = ctmp[:, 0:2, :] if d == 0 else ctmp[:, 2:4, :]
        nsrc = ctmp[:, 2, :] if d == 0 else ctmp[:, 0, :]
        for e in range(2):
            S.op("dve", "tensor_copy", reads=[ctmp], writes=[mT2[d]], out=mT2[d][:, e, :].rearrange("p (a t) -> p a t", a=2), in_=src)
            S.op("dve", "tensor_copy", reads=[ctmp], writes=[mN2[d]], out=mN2[d][:, e, :], in_=nsrc)
    ident2 = sb("ident2", [128, 2, 128], BF16)
    for e in range(2):
        S.op("dve", "tensor_copy", reads=[identf], writes=[ident2], out=ident2[:, e, :], in_=identf[:])
    ident2s = sb("ident2s", [128, 64])
    S.op("dve", "tensor_tensor", reads=[identf], writes=[ident2s], out=ident2s[:], in0=identf[:, 0:64], in1=identf[:, 64:128], op=ALU.add)
    ctmp2 = sb("ctmp2", [128, 130])
    S.dma("sp", ctmp2[:, 0:128], c_bones[:, :], writes=[ctmp2])
    S.dma("sp", ctmp2[:, 128:130], c_sel[:, :], writes=[ctmp2])
    bones = sb("bones", [128, 128], BF16); selb = sb("selb", [128, 2], BF16)
    S.op("dve", "tensor_copy", reads=[ctmp2], writes=[bones], out=bones[:], in_=ctmp2[:, 0:128])
    S.op("dve", "tensor_copy", reads=[ctmp2], writes=[selb], out=selb[:], in_=ctmp2[:, 128:130])
    BLK = 512
    NB = S_LEN // BLK
    CPB = BLK // 128
    reset = sb("reset", [128, BLK])
    S.dma("sp", reset[:], c_reset[:, 0:BLK], writes=[reset])
    upf = sb("upf", [128, 1024]); wupb = sb("wupb", [128, 512], BF16); aupb = sb("aupb", [128, 512], BF16)
    S.dma("sp", upf[:, 0:512], w_up[:, :], writes=[upf])
    S.dma("sp", upf[:, 512:1024], a_up[:, :], writes=[upf])
    S.op("dve", "tensor_copy", reads=[upf], writes=[wupb], out=wupb[:], in_=upf[:, 0:512])
    S.op("dve", "tensor_copy", reads=[upf], writes=[aupb], out=aupb[:], in_=upf[:, 512:1024])

    def f32t(n):
        return sb(n, [128, BLK])
    rF, kF, alpha, sg, cs, cum, t1, t2, t3, t4, t5 = [f32t("p3_%d" % i) for i in range(11)]
    vtF = sb("vtF", [128, CPB, 128])
    sqb = sb("sqb", [128, BLK], BF16); xbb = sb("xbb", [128, BLK], BF16)
    etot = sb("etot", [128, CPB])
    NPB = 2
    prep = []
    for i in range(NPB):
        prep.append(dict(
            ar=sb("ar%d" % i, [128, CPB, 2, 128], BF16), kt=sb("kt%d" % i, [128, BLK], BF16), bt=sb("bt%d" % i, [128, BLK], BF16),
            kh=sb("kh%d" % i, [128, BLK], BF16), bh=sb("bh%d" % i, [128, BLK], BF16),
            atk=sb("atk%d" % i, [128, CPB, 128], BF16), bhk=sb("bhk%d" % i, [128, CPB, 128], BF16),
            khk=sb("khk%d" % i, [128, CPB, 128], BF16), vbf=sb("vbf%d" % i, [128, CPB, 128], BF16),
            etot=sb("etot%d" % i, [128, CPB])))
    NSLOT = 4
    slots = []
    for i in range(NSLOT):
        sl = dict(
            Z=None,
            arb=sb("arb%d" % i, [128, 2, 128], BF16), s2b=sb("s2b%d" % i, [128, 2, 256], BF16),
            P=[sb("P%d_%d" % (i, j), [128, 2, 128], BF16) for j in range(2)],
            PTT=[sb("PTT%d_%d" % (i, j), [128, 2, 256], BF16) for j in range(2)],
            ysb=sb("ysb%d" % i, [128, 2, 64], BF16), w1m=sb("w1m%d" % i, [128, 2, 128], BF16),
            phiT=sb("phiT%d" % i, [128, 64]), xT=sb("xT%d" % i, [128, 128]))
        sl["Z"] = T(pall.t[:, 2 * i:2 * i + 2, :], "Z%d" % i)
        sl["Z"].b.excl = True
        slots.append(sl)
    Hs = [sb("H%d" % i, [128, 64]) for i in range(2)]
    pbk = []
    for k_ in range(2 * NSLOT):
        t_ = T(pall.t[:, k_, :], "pbk%d" % k_)
        t_.b = slots[k_ // 2]["Z"].b
        pbk.append(t_)
    NPBK = len(pbk)
    pbi = 0
    evc = [0]

    def evac_copy(out, in_, reads, writes):
        evc[0] += 1
        if evc[0] % 2 == 0:
            S.op("act", "activation", reads=reads, writes=writes, out=out, in_=in_, func=AF.Copy)
        else:
            S.op("dve", "tensor_copy", reads=reads, writes=writes, out=out, in_=in_)

    def evac2(out3, in3, reads, writes, eng=None):
        if eng is None:
            evc[0] += 1
            eng = "act" if evc[0] % 2 == 0 else "dve"
        if eng == "act":
            for e in range(2):
                S.op("act", "activation", reads=reads, writes=writes, out=out3[:, e, :], in_=in3[:, e, :], func=AF.Copy)
        else:
            S.op("dve", "tensor_copy", reads=reads, writes=writes, out=out3, in_=in3)

    def v3(ap_, c=CPB):
        return ap_.rearrange("p (c t) -> p c t", c=c)

    if peer:
        UVB = dscr("UVB", [16384, 2 * D], BF16)
        cvf = [sb("cvf%d" % i, [128, 2 * D]) for i in range(2)]
        cvb = [sb("cvb%d" % i, [128, 2 * D], BF16) for i in range(2)]
        S.rec = True
        ci_ = 0
        for tbl, dst in ((peer_u, UVB[:, 0:D]), (peer_v, UVB[:, D:2 * D])):
            for c in range(64):
                f_, b_ = cvf[ci_ % 2], cvb[ci_ % 2]
                rows = slice(c * 256, (c + 1) * 256)
                S.dma("sp", f_[:], tbl[rows, :].rearrange("(p r) d -> p (r d)", r=2), writes=[f_])
                S.op("pool", "tensor_copy", reads=[f_], writes=[b_], out=b_[:], in_=f_[:])
                S.dma("sp", dst[rows, :].rearrange("(p r) d -> p r d", r=2), b_[:].rearrange("p (r d) -> p r d", r=2), reads=[b_])
                ci_ += 1
        S.rec = False
        cv_pending = S.pending
        S.pending = []
    else:
        cv_pending = []

    def cv_flush(n):
        nonlocal cv_pending
        keep = S.pending
        S.pending = cv_pending
        S.flush(n)
        cv_pending = S.pending
        S.pending = keep

    for hp in range(4):
        hsl = slice(hp * 128, (hp + 1) * 128)
        for d in range(2):
            hcur = 0
            S.op("pool", "memset", writes=[Hs[0]], ap=Hs[0][:], constant=0.0)
            blks = range(NB) if d == 0 else range(NB - 1, -1, -1)
            for bi, blk in enumerate(blks):
                if hp * 16 + d * 8 + bi >= nblk:
                    continue
                cv_flush(6)
                tsl = slice(blk * BLK, (blk + 1) * BLK)
                pr = prep[bi % NPB]
                S.dma("sp", rF[:], RT[hsl, tsl], writes=[rF])
                S.dma("act", kF[:], KT[hsl, tsl], writes=[kF])
                S.dma("sp", vtF[:], VTOK[tsl, hsl].rearrange("(n p) c -> p n c", p=128), writes=[vtF])
                S.op("pool", "tensor_copy", reads=[vtF], writes=[pr["vbf"]], out=pr["vbf"][:], in_=vtF[:])
                bk = pbk[pbi % NPBK]; pbi += 1
                S.op("pe", "matmul", reads=[aupb, adT], writes=[bk], out=bk[:, :], lhsT=aupb[64 * d:64 * d + 64, hsl],
                     rhs=adT[64 * d:64 * d + 64, tsl], start=True, stop=True)
                S.op("act", "activation", reads=[bk, pc], writes=[alpha], out=alpha[:], in_=bk[:, :], func=AF.Sigmoid,
                     bias=pc[:, col["a0_%d" % d] + hp:col["a0_%d" % d] + hp + 1])
                bk = pbk[pbi % NPBK]; pbi += 1
                S.op("pe", "matmul", reads=[wupb, twd], writes=[bk], out=bk[:, :], lhsT=wupb[64 * d:64 * d + 64, hsl],
                     rhs=twd[64 * d:64 * d + 64, tsl], start=True, stop=True)
                S.op("act", "activation", reads=[bk, pc], writes=[sg], out=sg[:], in_=bk[:, :], func=AF.Sigmoid,
                     bias=pc[:, col["w0_%d" % d] + hp:col["w0_%d" % d] + hp + 1])
                S.op("dve", "tensor_tensor_scan", reads=[reset, sg], writes=[cs], out=cs[:], data0=reset[:], data1=sg[:],
                     initial=0.0, op0=ALU.mult, op1=ALU.add)
                S.op("act", "activation", reads=[cs], writes=[pr["etot"]], out=pr["etot"][:], in_=v3(cs[:])[:, :, 127], func=AF.Exp,
                     scale=-C0)
                if d == 0:
                    cm = cs
                else:
                    S.op("dve", "tensor_tensor", reads=[sg, cs], writes=[t1], out=t1[:], in0=sg[:], in1=cs[:], op=ALU.subtract)
                    S.op("dve", "tensor_tensor", reads=[t1, cs], writes=[cum], out=v3(cum[:]), in0=v3(t1[:]),
                         in1=v3(cs[:])[:, :, 127:128].to_broadcast([128, CPB, 128]), op=ALU.add)
                    cm = cum
                S.op("pool", "tensor_tensor", reads=[cm, sg], writes=[t1], out=t1[:], in0=cm[:], in1=sg[:], op=ALU.subtract)
                S.op("act", "activation", reads=[t1], writes=[t1], out=t1[:], in_=t1[:], func=AF.Exp, scale=-C0)
                S.op("act", "activation", reads=[cm], writes=[t2], out=t2[:], in_=cm[:], func=AF.Exp, scale=-C0)
                S.op("act", "activation", reads=[cm], writes=[t3], out=t3[:], in_=cm[:], func=AF.Exp, scale=C0)
                S.op("dve", "tensor_scalar", reads=[kF, pc], writes=[t4], out=t4[:], in0=kF[:],
                     scalar1=pc[:, col["k_k"] + hp:col["k_k"] + hp + 1], scalar2=None, op0=ALU.mult)
                S.op("pool", "tensor_tensor", reads=[t4], writes=[sqb], out=sqb[:], in0=t4[:], in1=t4[:], op=ALU.mult)
                bk = pbk[pbi % NPBK]; pbi += 1
                S.op("pe", "matmul", reads=[bones, sqb], writes=[bk], out=bk[:, :], lhsT=bones[:], rhs=sqb[:], start=True, stop=True)
                S.op("act", "activation", reads=[bk], writes=[t5], out=t5[:], in_=bk[:, :], func=AF.Sqrt)
                S.op("dve", "tensor_scalar", reads=[t5], writes=[t5], out=t5[:], in0=t5[:], scalar1=1e-12, scalar2=None, op0=ALU.max)
                S.op("dve", "reciprocal", reads=[t5], writes=[t5], out=t5[:], in_=t5[:])
                S.op("dve", "tensor_tensor", reads=[t4, t5], writes=[t4], out=t4[:], in0=t4[:], in1=t5[:], op=ALU.mult)
                S.op("act", "activation", reads=[alpha, pc], writes=[t5], out=t5[:], in_=alpha[:], func=AF.Identity,
                     scale=pc[:, col["k_a"] + hp:col["k_a"] + hp + 1], bias=pc[:, col["omka"] + hp:col["omka"] + hp + 1])
                S.op("dve", "tensor_tensor", reads=[t5, kF], writes=[t5], out=t5[:], in0=t5[:], in1=kF[:], op=ALU.mult)
                ar = pr["ar"]
                S.op("dve", "tensor_tensor", reads=[rF, t2], writes=[ar], out=ar[:, :, 1, :], in0=v3(rF[:]), in1=v3(t2[:]), op=ALU.mult)
                S.op("dve", "scalar_tensor_tensor", reads=[t4, t1], writes=[ar], out=ar[:, :, 0, :], in0=v3(t4[:]), scalar=-1.0,
                     in1=v3(t1[:]), op0=ALU.mult, op1=ALU.mult)
                S.op("pool", "tensor_tensor", reads=[t5, t3], writes=[pr["kt"]], out=pr["kt"][:], in0=t5[:], in1=t3[:], op=ALU.mult)
                S.op("pool", "tensor_tensor", reads=[t4, alpha], writes=[t2], out=t2[:], in0=t4[:], in1=alpha[:], op=ALU.mult)
                S.op("pool", "tensor_tensor", reads=[t2, t3], writes=[pr["bt"]], out=pr["bt"][:], in0=t2[:], in1=t3[:], op=ALU.mult)
                etb = pr["etot"][:, 0:CPB].unsqueeze(2).to_broadcast([128, CPB, 128])
                S.op("dve", "tensor_tensor", reads=[pr["kt"], pr["etot"]], writes=[pr["kh"]], out=v3(pr["kh"][:]), in0=v3(pr["kt"][:]),
                     in1=etb, op=ALU.mult)
                S.op("dve", "tensor_tensor", reads=[pr["bt"], pr["etot"]], writes=[pr["bh"]], out=v3(pr["bh"][:]), in0=v3(pr["bt"][:]),
                     in1=etb, op=ALU.mult)
                S.op("dve", "scalar_tensor_tensor", reads=[rF, pc, t5], writes=[xbb], out=xbb[:], in0=rF[:],
                     scalar=pc[:, col["r_k"] + hp:col["r_k"] + hp + 1], in1=t5[:], op0=ALU.mult, op1=ALU.mult)
                bk = pbk[pbi % NPBK]; pbi += 1
                for c in range(CPB):
                    S.op("pe", "matmul", reads=[xbb, selb], writes=[bk], out=bk[:, 2 * c:2 * c + 2], lhsT=xbb[:, c * 128:(c + 1) * 128],
                         rhs=selb[:], start=True, stop=True)
                cf = coef[:, blk * CPB:(blk + 1) * CPB, 2 * hp:2 * hp + 2]
                S.op("dve", "tensor_tensor", reads=[bk, coef], writes=[coef], out=cf, in0=bk[:, 0:2 * CPB].rearrange("p (c e) -> p c e", e=2),
                     in1=cf, op=ALU.add)
                for nm, srcT in (("atk", None), ("bhk", pr["bh"]), ("khk", pr["kh"])):
                    bk = pbk[pbi % NPBK]; pbi += 1
                    bkb = bk.t[:].bitcast(BF16)
                    for c in range(CPB):
                        in_ = ar[:, c, 0, :] if srcT is None else srcT[:, c * 128:(c + 1) * 128]
                        S.op("pe", "transpose", reads=[ar if srcT is None else srcT, identb], writes=[bk],
                             out=bkb[:, c * 128:(c + 1) * 128], in_=in_, identity=identb[:])
                    evac_copy(pr[nm][:], bkb[:, 0:CPB * 128].rearrange("p (c t) -> p c t", c=CPB), [bk], [pr[nm]])

                if cut <= 1:
                    continue
                corder = list(range(CPB)) if d == 0 else list(range(CPB - 1, -1, -1))
                for g0 in range(0, CPB, NSLOT):
                    grp = corder[g0:g0 + NSLOT]
                    for si, c in enumerate(grp):
                        sl = slots[si]
                        Z = sl["Z"]
                        csl = slice(c * 128, (c + 1) * 128)
                        for e in range(2):
                            ps_ = slice(64 * e, 64 * e + 64)
                            S.op("pe", "matmul", reads=[pr["bt"], ar], writes=[Z], out=Z[:, e, 0:256], lhsT=pr["bt"][ps_, csl],
                                 rhs=ar[ps_, c, :, :], start=True, stop=True)
                            S.op("pe", "matmul", reads=[pr["kt"], ar], writes=[Z], out=Z[:, e, 256:512], lhsT=pr["kt"][ps_, csl],
                                 rhs=ar[ps_, c, :, :], start=True, stop=True)
                        PTT0 = sl["PTT"][0]; PTT1 = sl["PTT"][1]
                        S.op("dve", "tensor_tensor", reads=[Z, mT2[d]], writes=[PTT0], out=PTT0[:, :, 0:128], in0=Z[:, :, 0:128],
                             in1=mT2[d][:, :, 0:128], op=ALU.mult)
                        S.op("dve", "tensor_tensor", reads=[Z, mT2[d]], writes=[sl["arb"]], out=sl["arb"][:], in0=Z[:, :, 128:256],
                             in1=mT2[d][:, :, 128:256], op=ALU.mult)
                        S.op("dve", "tensor_tensor", reads=[Z, mT2[d]], writes=[sl["s2b"]], out=sl["s2b"][:], in0=Z[:, :, 256:512], in1=mT2[d][:],
                             op=ALU.mult)
                        S.op("pool", "tensor_tensor", reads=[PTT0, ident2], writes=[PTT1], out=PTT1[:, :, 128:256], in0=PTT0[:, :, 0:128],
                             in1=ident2[:], op=ALU.add)
                    for si, c in enumerate(grp):
                        sl = slots[si]
                        Z = sl["Z"]
                        for e in range(2):
                            ps_ = slice(64 * e, 64 * e + 64)
                            S.op("pe", "matmul", reads=[ar, pr["bt"]], writes=[Z], out=Z[:, e, 0:128], lhsT=ar[ps_, c, 0, :],
                                 rhs=pr["bt"][ps_, c * 128:(c + 1) * 128], start=True, stop=True)
                        S.op("dve", "tensor_tensor", reads=[Z, mN2[d]], writes=[sl["P"][0]], out=sl["P"][0][:], in0=Z[:, :, 0:128], in1=mN2[d][:],
                             op=ALU.mult)
                    if cut <= 2:
                        continue
                    for k in range(7):
                        for si, c in enumerate(grp):
                            sl = slots[si]
                            Z = sl["Z"]
                            Px, Py = sl["P"][k % 2], sl["P"][(k + 1) % 2]
                            Tx, Ty = sl["PTT"][k % 2], sl["PTT"][(k + 1) % 2]
                            for e in range(2):
                                if k == 0:
                                    S.op("pe", "matmul", reads=[Px, Tx], writes=[Z], out=Z[:, e, 0:128], lhsT=Px[:, e, :],
                                         rhs=Tx[:, e, 0:128], start=True, stop=True)
                                elif k <= 4:
                                    S.op("pe", "matmul", reads=[Px, Tx], writes=[Z], out=Z[:, e, 0:256], lhsT=Px[:, e, :],
                                         rhs=Tx[:, e, :], start=True, stop=True)
                                else:
                                    S.op("pe", "matmul", reads=[Px, Tx], writes=[Z], out=Z[:, e, 128:256], lhsT=Px[:, e, :],
                                         rhs=Tx[:, e, 128:256], start=True, stop=True)
                                if k <= 5:
                                    S.op("pe", "matmul", reads=[Px, Tx], writes=[Z], out=Z[:, e, 256:384], lhsT=Tx[:, e, 0:128],
                                         rhs=Px[:, e, :], start=True, stop=True)
                            if k <= 5:
                                evac2(Py[:], Z[:, :, 256:384], [Z], [Py], eng="act")
                            if k <= 4:
                                S.op("dve", "tensor_copy", reads=[Z], writes=[Ty], out=Ty[:, :, 0:128], in_=Z[:, :, 0:128])
                            if k >= 1:
                                S.op("dve", "tensor_tensor", reads=[Z, Tx], writes=[Ty], out=Ty[:, :, 128:256], in0=Z[:, :, 128:256],
                                     in1=Tx[:, :, 128:256], op=ALU.add)
                    if cut <= 3:
                        continue
                    TTF = 1
                    for si, c in enumerate(grp):
                        sl = slots[si]
                        Z = sl["Z"]
                        for e in range(2):
                            S.op("pe", "matmul", reads=[sl["s2b"], pr["vbf"]], writes=[Z], out=Z[:, e, 384:448], lhsT=sl["s2b"][:, e, 0:128],
                                 rhs=pr["vbf"][:, c, 64 * e:64 * e + 64], start=True, stop=True)
                        evac2(sl["ysb"][:], Z[:, :, 384:448], [Z], [sl["ysb"]], eng="act")
                    if cut <= 3.1:
                        continue
                    for si, c in enumerate(grp):
                        sl = slots[si]
                        Z = sl["Z"]
                        TT = sl["PTT"][TTF]
                        for e in range(2):
                            S.op("pe", "matmul", reads=[TT, sl["ysb"]], writes=[Z], out=Z[:, e, 0:64], lhsT=TT[:, e, 128:256],
                                 rhs=sl["ysb"][:, e, :], start=True, stop=True)
                            S.op("pe", "matmul", reads=[TT, pr["atk"]], writes=[Z], out=Z[:, e, 64:128], lhsT=TT[:, e, 128:256],
                                 rhs=pr["atk"][:, c, 64 * e:64 * e + 64], start=True, stop=True)
                        evac2(sl["w1m"][:], Z[:, :, 0:128], [Z], [sl["w1m"]], eng="act")
                    if cut <= 3.2:
                        continue
                    for si, c in enumerate(grp):
                        sl = slots[si]
                        Z = sl["Z"]
                        w1m = sl["w1m"]
                        for e in range(2):
                            ps_ = slice(64 * e, 64 * e + 64)
                            S.op("pe", "matmul", reads=[w1m, pr["bhk"]], writes=[Z], out=Z[ps_, e, 448:512], lhsT=w1m[:, e, 64:128],
                                 rhs=pr["bhk"][:, c, 64 * e:64 * e + 64], start=True, stop=True)
                            S.op("pe", "matmul", reads=[w1m, sl["arb"]], writes=[Z], out=Z[ps_, e, 128:256], lhsT=w1m[:, e, 64:128],
                                 rhs=sl["arb"][:, e, :], start=True, stop=True)
                        if cut <= 3.3:
                            continue
                        for e in range(2):
                            ps_ = slice(64 * e, 64 * e + 64)
                            S.op("dve", "scalar_tensor_tensor", reads=[ident2s, pr["etot"], Z], writes=[sl["phiT"]], out=sl["phiT"][ps_, :],
                                 in0=ident2s[ps_, :], scalar=pr["etot"][ps_, c:c + 1], in1=Z[ps_, e, 448:512], op0=ALU.mult, op1=ALU.add)
                            S.op("dve", "tensor_tensor", reads=[Z, ar], writes=[sl["xT"]], out=sl["xT"][ps_, :], in0=Z[ps_, e, 128:256],
                                 in1=ar[ps_, c, 1, :], op=ALU.add)
                    if cut <= 4:
                        continue
                    for si, c in enumerate(grp):
                        sl = slots[si]
                        Z = sl["Z"]
                        w1m = sl["w1m"]
                        Hc, Hn = Hs[hcur], Hs[1 - hcur]
                        for e in range(2):
                            ps_ = slice(64 * e, 64 * e + 64)
                            vv = pr["vbf"][:, c, 64 * e:64 * e + 64]
                            S.op("pe", "matmul", reads=[sl["arb"], w1m], writes=[Z], out=Z[:, e, 256:320], lhsT=sl["arb"][:, e, :],
                                 rhs=w1m[:, e, 0:64], start=True, stop=False)
                            S.op("pe", "matmul", reads=[sl["s2b"], pr["vbf"]], writes=[Z], out=Z[:, e, 256:320],
                                 lhsT=sl["s2b"][:, e, 128:256], rhs=vv, start=False, stop=False)
                            S.op("pe", "matmul", reads=[sl["xT"], Hc], writes=[Z], out=Z[:, e, 256:320], lhsT=sl["xT"][ps_, :],
                                 rhs=Hc[ps_, :], start=False, stop=True)
                            S.op("pe", "matmul", reads=[pr["bhk"], w1m], writes=[Z], out=Z[ps_, e, 320:384], lhsT=pr["bhk"][:, c, 64 * e:64 * e + 64],
                                 rhs=w1m[:, e, 0:64], start=True, stop=False)
                            S.op("pe", "matmul", reads=[pr["khk"], pr["vbf"]], writes=[Z], out=Z[ps_, e, 320:384],
                                 lhsT=pr["khk"][:, c, 64 * e:64 * e + 64], rhs=vv, start=False, stop=False)
                            S.op("pe", "matmul", reads=[sl["phiT"], Hc], writes=[Z], out=Z[ps_, e, 320:384], lhsT=sl["phiT"][ps_, :],
                                 rhs=Hc[ps_, :], start=False, stop=True)
                        for e in range(2):
                            ps_ = slice(64 * e, 64 * e + 64)
                            S.op("act", "activation", reads=[Z], writes=[Hn], out=Hn[ps_, :], in_=Z[ps_, e, 320:384], func=AF.Copy)
                        hcur = 1 - hcur
                        tile_i = blk * CPB + c
                        osl = obuf[:, tile_i, hp * 128:(hp + 1) * 128].rearrange("p (e n) -> p e n", e=2)
                        if d == 0:
                            S.op("dve", "tensor_copy", reads=[Z], writes=[obuf], out=osl, in_=Z[:, :, 256:320])
                        else:
                            S.op("dve", "tensor_tensor", reads=[Z, obuf], writes=[obuf], out=osl, in0=Z[:, :, 256:320], in1=osl, op=ALU.add)

    cv_flush(None)
    for q in range(8):
        S.dma("sp" if q % 2 == 0 else "act", OB[q * 512:(q + 1) * 512, :].rearrange("(n p) c -> p n c", p=128), obuf[:, q * 4:(q + 1) * 4, :], reads=[obuf])
    CF = dscr("CF", [128, NT * 8])
    S.dma("sp", CF[:, :], coef[:].rearrange("p n e -> p (n e)"), reads=[coef])
    S.barrier()
    release(len(guards) - n_ph3)

    X1 = dscr("X1", [S_LEN, D])
    coef = sb("coef5", [128, NT * 8])
    S.dma("sp", coef[:], CF[:, :], writes=[coef])
    lnw_b = sb("lnw_b", [128, 512]); lnb_b = sb("lnb_b", [128, 512]); wb2 = sb("nw2b", [128, D]); wfb = sb("nwfb", [128, D])
    S.dma("sp", lnw_b[:], ln_x_w.partition_broadcast(128), writes=[lnw_b])
    S.dma("sp", lnb_b[:], ln_x_b.partition_broadcast(128), writes=[lnb_b])
    S.dma("sp", wb2[:], norm2_w.partition_broadcast(128), writes=[wb2])
    S.dma("sp", wfb[:], norm_f_w.partition_broadcast(128), writes=[wfb])
    stg = sb("stg", [128, 2048])
    gupA = sb("gupA", [128, 512], BF16); gupB = sb("gupB", [32, 512], BF16)
    S.dma("sp", stg[:, 0:512], g_up[0:128, :], writes=[stg])
    S.dma("sp", stg[0:32, 512:1024], g_up[128:160, :], writes=[stg])
    S.op("dve", "tensor_copy", reads=[stg], writes=[gupA], out=gupA[:], in_=stg[:, 0:512])
    S.op("dve", "tensor_copy", reads=[stg], writes=[gupB], out=gupB[:], in_=stg[0:32, 512:1024])
    woutb = sb("woutb", [128, 8, D], BF16)
    for kc in range(0, 8, 2):
        S.dma("sp", stg[:].rearrange("p (k n) -> p k n", k=2), w_out[:, kc:kc + 2, :], writes=[stg])
        S.op("dve", "tensor_copy", reads=[stg], writes=[woutb], out=woutb[:, kc:kc + 2, :], in_=stg[:].rearrange("p (k n) -> p k n", k=2))
    if peer:
        wqb = sb("wqb", [128, 8, 2048], BF16)
        for kc in range(8):
            S.dma("sp", stg[:], wq[:, kc, :], writes=[stg])
            S.op("dve", "tensor_copy", reads=[stg], writes=[wqb], out=wqb[:, kc, :], in_=stg[:])
        keyb = sb("keyb", [128, 16, 128], BF16)
        S.dma("sp", stg[:].rearrange("p (k n) -> p k n", k=16), keysT[:, :, :], writes=[stg])
        S.op("dve", "tensor_copy", reads=[stg], writes=[keyb], out=keyb[:], in_=stg[:].rearrange("p (k n) -> p k n", k=16))
        iota_i = sb("iota_i", [128, 256], I32); iota_f = sb("iota_f", [128, 256])
        S.op("pool", "iota", writes=[iota_i], out=iota_i[:], pattern=[[1, 256]], base=0, channel_multiplier=0)
        S.op("dve", "tensor_copy", reads=[iota_i], writes=[iota_f], out=iota_f[:], in_=iota_i[:])
        sc2 = sb("sc2", [128, 2048])
        m1 = sb("m1", [128, 256]); i1 = sb("i1", [128, 256], U32); idxf = sb("idxf", [128, 256]); idx128 = sb("idx128", [128, 256])
        cand = sb("cand", [128, 2048])
        r1u = sb("r1u", [128, 128], U32); r2u = sb("r2u", [128, 128], U32); r2f = sb("r2f", [128, 128])
        e1v = sb("e1v", [128, 128]); e2v = sb("e2v", [128, 128])
        sc16 = sb("sc16", [128, 128]); pos = sb("pos", [128, 128], U32); posf = sb("posf", [128, 128])
        eidf = sb("eidf", [128, 128]); eid = sb("eid", [128, 128], U32)
        gsm = sb("gsm", [128, 16]); gate = sb("gate", [128, 128]); aact = sb("aact", [128, 128]); wgt = sb("wgt", [128, 128])
        junk = sb("junk", [128, D], BF16)
        NG = 7
        m1c = [Buf() for _ in range(16)]; i1c = [Buf() for _ in range(16)]; sc2c = [Buf() for _ in range(16)]
        s16c = [Buf() for _ in range(8)]; posc = [Buf() for _ in range(8)]
        eidc = [Buf() for _ in range(128)]; aactc = [Buf() for _ in range(128)]
        accb = Buf("accb")
        uvg = [sb("uvg%d" % i, [128, 2 * D], BF16) for i in range(NG)]
        dg = [sb("dg%d" % i, [128, 128], BF16) for i in range(NG)]
        gel = sb("gel", [128, 128]); gelc = [Buf() for _ in range(128)]
        gateb = sb("gateb", [128, 128]); h2bb = sb("h2bb", [128, D])
        qT = sb("qT", [128, 16, 128], BF16)
    ot = sb("ot", [128, 512]); tm = sb("tm5", [128, 512]); vt5 = sb("vt5", [128, 512])
    st8 = sb("st8", [128, 64])
    ybf = sb("ybf", [128, 512], BF16); yT = sb("yT5", [128, 4, 128], BF16); ypT = sb("ypT", [128, 4, 128], BF16)
    xt5 = sb("xt5", [128, D]); x1 = sb("x1", [128, D]); h2 = sb("h2", [128, D]); h2b = sb("h2b", [128, D], BF16)
    h2T = sb("h2T", [128, 8, 128], BF16)
    ssq5 = sb("ssq5", [128, 8])
    junk2 = junk if peer else sb("junk2", [128, D], BF16)
    P2 = T(pall.t[:, 0:2, :], "P2"); P2.b.excl = True
    P4 = T(pall.t[:, 4:6, :], "P4"); P4.b.excl = True
    PV = T(pall.t[:, 6:8, :], "PV"); PV.b.excl = True
    b2, b3 = banks[2], banks[3]

    def rms(src, dst_f32, wbt, k0):
        S.op("act", "activation", reads=[src], writes=[junk2, ssq5], out=junk2[:], in_=src[:], func=AF.Square,
             accum_out=ssq5[:, k0:k0 + 1])
        S.op("act", "activation", reads=[ssq5], writes=[ssq5], out=ssq5[:, k0 + 1:k0 + 2], in_=ssq5[:, k0:k0 + 1], func=AF.Sqrt,
             scale=1.0 / D, bias=epsc[:, 0:1])
        S.op("dve", "reciprocal", reads=[ssq5], writes=[ssq5], out=ssq5[:, k0 + 2:k0 + 3], in_=ssq5[:, k0 + 1:k0 + 2])
        S.op("dve", "scalar_tensor_tensor", reads=[src, ssq5, wbt], writes=[dst_f32], out=dst_f32[:], in0=src[:],
             scalar=ssq5[:, k0 + 2:k0 + 3], in1=wbt[:], op0=ALU.mult, op1=ALU.mult)

    x1s = [x1, sb("x1b", [128, D])]
    h2s = [h2, h2bb] if peer else [h2, h2]
    gates = [gate, gateb] if peer else None
    eids = [eid, sb("eidb", [128, 128], U32)] if peer else None

    def partA(i):
        tsl = slice(i * 128, (i + 1) * 128)
        x1 = x1s[i % 2]
        eid = eids[i % 2] if peer else None
        h2 = h2s[i % 2]
        gate = gates[i % 2] if peer else None
        S.dma("sp", ot[:], OB[tsl, :], writes=[ot])
        S.dma("act", vt5[:], VTOK[tsl, :], writes=[vt5])
        S.dma("sp", ypT[:], YP[:, tsl].rearrange("(g p) t -> p g t", p=128), writes=[ypT])
        S.dma("act", xt5[:], x[tsl, :], writes=[xt5])
        o3 = ot[:].rearrange("p (h n) -> p h n", h=8)
        t3 = tm[:].rearrange("p (h n) -> p h n", h=8)
        S.op("dve", "tensor_reduce", reads=[ot], writes=[st8], out=st8[:, 0:8], in_=o3, axis=AX.X, op=ALU.add)
        S.op("pool", "tensor_tensor", reads=[ot], writes=[tm], out=tm[:], in0=ot[:], in1=ot[:], op=ALU.mult)
        S.op("dve", "tensor_reduce", reads=[tm], writes=[st8], out=st8[:, 8:16], in_=t3, axis=AX.X, op=ALU.add)
        S.op("dve", "tensor_scalar", reads=[st8], writes=[st8], out=st8[:, 16:24], in0=st8[:, 0:8], scalar1=1.0 / 64, scalar2=None, op0=ALU.mult)
        S.op("dve", "tensor_tensor", reads=[st8], writes=[st8], out=st8[:, 24:32], in0=st8[:, 16:24], in1=st8[:, 16:24], op=ALU.mult)
        S.op("dve", "scalar_tensor_tensor", reads=[st8], writes=[st8], out=st8[:, 32:40], in0=st8[:, 8:16], scalar=1.0 / 64, in1=st8[:, 24:32],
             op0=ALU.mult, op1=ALU.subtract)
        S.op("act", "activation", reads=[st8], writes=[st8], out=st8[:, 40:48], in_=st8[:, 32:40], func=AF.Sqrt, bias=epsc[:, 1:2])
        S.op("dve", "reciprocal", reads=[st8], writes=[st8], out=st8[:, 48:56], in_=st8[:, 40:48])
        S.op("dve", "tensor_tensor", reads=[ot, st8], writes=[tm], out=t3, in0=o3, in1=st8[:, 16:24].unsqueeze(2).to_broadcast([128, 8, 64]),
             op=ALU.subtract)
        S.op("dve", "tensor_tensor", reads=[tm, st8], writes=[tm], out=t3, in0=t3, in1=st8[:, 48:56].unsqueeze(2).to_broadcast([128, 8, 64]),
             op=ALU.mult)
        S.op("pool", "tensor_tensor", reads=[tm, lnw_b], writes=[tm], out=tm[:], in0=tm[:], in1=lnw_b[:], op=ALU.mult)
        S.op("pool", "tensor_tensor", reads=[tm, lnb_b], writes=[tm], out=tm[:], in0=tm[:], in1=lnb_b[:], op=ALU.add)
        S.op("dve", "tensor_tensor", reads=[vt5, coef], writes=[vt5], out=vt5[:].rearrange("p (h n) -> p h n", h=8),
             in0=vt5[:].rearrange("p (h n) -> p h n", h=8), in1=coef[:, i * 8:(i + 1) * 8].unsqueeze(2).to_broadcast([128, 8, 64]), op=ALU.mult)
        S.op("dve", "tensor_tensor", reads=[tm, vt5], writes=[tm], out=tm[:], in0=tm[:], in1=vt5[:], op=ALU.add)
        S.op("pe", "matmul", reads=[sgA, gupA], writes=[b2], out=b2[:, :], lhsT=sgA[:, tsl], rhs=gupA[:], start=True, stop=False)
        S.op("pe", "matmul", reads=[sgB, gupB], writes=[b2], out=b2[:, :], lhsT=sgB[:, tsl], rhs=gupB[:], start=False, stop=True)
        S.op("dve", "tensor_tensor", reads=[tm, b2], writes=[ybf], out=ybf[:], in0=tm[:], in1=b2[:, :], op=ALU.mult)
        b3b = b3.t.bitcast(BF16)
        for q in range(4):
            S.op("pe", "transpose", reads=[ybf, identb], writes=[b3], out=b3b[:, q * 128:(q + 1) * 128], in_=ybf[:, q * 128:(q + 1) * 128],
                 identity=identb[:])
        S.op("act", "activation", reads=[b3], writes=[yT], out=yT[:], in_=b3b[:, 0:512].rearrange("p (q t) -> p q t", q=4), func=AF.Copy)
        for hf in range(2):
            for kc in range(8):
                lt = yT[:, kc, :] if kc < 4 else ypT[:, kc - 4, :]
                S.op("pe", "matmul", reads=[yT, ypT, woutb], writes=[P2], out=P2[:, hf, :], lhsT=lt, rhs=woutb[:, kc, hf * 512:(hf + 1) * 512],
                     start=(kc == 0), stop=(kc == 7))
        S.op("dve", "tensor_tensor", reads=[P2, xt5], writes=[x1], out=x1[:].rearrange("p (a n) -> p a n", a=2), in0=P2[:, :, :],
             in1=xt5[:].rearrange("p (a n) -> p a n", a=2), op=ALU.add)
        if "X1" in debug:
            S.dma("sp", X1[tsl, :], x1[:], reads=[x1])
        if not peer:
            rms(x1, h2, wfb, 0)
            S.dma("sp", out[tsl, :], h2[:], reads=[h2])
            return
        rms(x1, h2, wb2, 0)
        S.op("pool", "tensor_copy", reads=[h2], writes=[h2b], out=h2b[:], in_=h2[:])
        for kc in range(8):
            S.op("pe", "transpose", reads=[h2b, identb], writes=[b3], out=b3b[:, kc * 128:(kc + 1) * 128], in_=h2b[:, kc * 128:(kc + 1) * 128],
                 identity=identb[:])
        S.op("act", "activation", reads=[b3], writes=[h2T], out=h2T[:], in_=b3b[:, :].rearrange("p (k t) -> p k t", k=8), func=AF.Copy)
        for c4 in range(4):
            bk = b2 if c4 % 2 == 0 else b3
            for cq in range(4):
                ch = c4 * 4 + cq
                for kc in range(8):
                    S.op("pe", "matmul", reads=[wqb, h2T], writes=[bk], out=bk[:, cq * 128:(cq + 1) * 128], lhsT=wqb[:, kc, ch * 128:(ch + 1) * 128],
                         rhs=h2T[:, kc, :], start=(kc == 0), stop=(kc == 7))
            S.op("act" if c4 % 2 == 0 else "dve", "activation" if c4 % 2 == 0 else "tensor_copy", reads=[bk], writes=[qT],
                 out=qT[:, c4 * 4:(c4 + 1) * 4, :], in_=bk[:, :].rearrange("p (c t) -> p c t", c=4), **({"func": AF.Copy} if c4 % 2 == 0 else {}))
        for half in range(2):
            for c8 in range(8):
                ch = half * 8 + c8
                S.op("pe", "matmul", reads=[qT, keyb], writes=[P4], out=P4[:, c8 // 4, (c8 % 4) * 128:(c8 % 4 + 1) * 128], lhsT=qT[:, ch, :],
                     rhs=keyb[:, ch, :], start=True, stop=True)
            S.op("dve", "tensor_copy", reads=[P4], writes=[stg], out=stg[:, half * 1024:(half + 1) * 1024].rearrange("p (a n) -> p a n", a=2),
                 in_=P4[:, :, :])
        scv = stg[:].rearrange("p (c n) -> p c n", c=16)
        sc2v = sc2[:].rearrange("p (c n) -> p c n", c=16)
        m1v = m1[:].rearrange("p (c n) -> p c n", c=16)
        i1v = i1[:].rearrange("p (c n) -> p c n", c=16)
        for ch in range(16):
            S.op("dve", "max", reads=[stg], writes=[m1c[ch]], out=m1v[:, ch, 0:8], in_=scv[:, ch, :])
        for ch in range(16):
            S.op("dve", "max_index", reads=[stg, m1c[ch]], writes=[i1c[ch]], out=i1v[:, ch, 0:8], in_max=m1v[:, ch, 0:8], in_values=scv[:, ch, :])
        for ch in range(16):
            S.op("dve", "match_replace", reads=[stg, m1c[ch]], writes=[sc2c[ch]], out=sc2v[:, ch, :], in_to_replace=m1v[:, ch, 0:8],
                 in_values=scv[:, ch, :], imm_value=-1e30)
        for ch in range(16):
            S.op("dve", "max", reads=[sc2c[ch]], writes=[m1c[ch]], out=m1v[:, ch, 8:16], in_=sc2v[:, ch, :])
        for ch in range(16):
            S.op("dve", "max_index", reads=[sc2c[ch], m1c[ch]], writes=[i1c[ch]], out=i1v[:, ch, 8:16], in_max=m1v[:, ch, 8:16],
                 in_values=sc2v[:, ch, :])
        S.op("dve", "tensor_copy", reads=i1c, writes=[idxf], out=idxf[:], in_=i1[:])
        S.op("dve", "tensor_scalar", reads=[idxf], writes=[idx128], out=idx128[:], in0=idxf[:], scalar1=128.0, scalar2=None, op0=ALU.mult)
        m4 = m1[:].rearrange("p (h a n) -> p h a n", h=8, a=2)
        x4 = idxf[:].rearrange("p (h a n) -> p h a n", h=8, a=2)
        y4 = idx128[:].rearrange("p (h a n) -> p h a n", h=8, a=2)
        c4v = cand[:].rearrange("p (h i j) -> p h i j", h=8, i=16)
        S.op("dve", "tensor_tensor", reads=m1c, writes=[cand], out=c4v, in0=m4[:, :, 0, :].unsqueeze(3).to_broadcast([128, 8, 16, 16]),
             in1=m4[:, :, 1, :].unsqueeze(2).to_broadcast([128, 8, 16, 16]), op=ALU.add)
        cv = cand[:].rearrange("p (h n) -> p h n", h=8)
        c2v = sc2[:].rearrange("p (h n) -> p h n", h=8)
        s16 = sc16[:].rearrange("p (h n) -> p h n", h=8)
        p16 = pos[:].rearrange("p (h n) -> p h n", h=8)
        for hh in range(8):
            S.op("dve", "max", reads=[cand], writes=[s16c[hh]], out=s16[:, hh, 0:8], in_=cv[:, hh, :])
        for hh in range(8):
            S.op("dve", "max_index", reads=[cand, s16c[hh]], writes=[posc[hh]], out=p16[:, hh, 0:8], in_max=s16[:, hh, 0:8], in_values=cv[:, hh, :])
        for hh in range(8):
            S.op("dve", "match_replace", reads=[cand, s16c[hh]], writes=[sc2c[2 * hh], sc2c[2 * hh + 1]], out=c2v[:, hh, :],
                 in_to_replace=s16[:, hh, 0:8], in_values=cv[:, hh, :], imm_value=-1e30)
        for hh in range(8):
            S.op("dve", "max", reads=[sc2c[2 * hh], sc2c[2 * hh + 1]], writes=[s16c[hh]], out=s16[:, hh, 8:16], in_=c2v[:, hh, :])
        for hh in range(8):
            S.op("dve", "max_index", reads=[sc2c[2 * hh], sc2c[2 * hh + 1], s16c[hh]], writes=[posc[hh]], out=p16[:, hh, 8:16],
                 in_max=s16[:, hh, 8:16], in_values=c2v[:, hh, :])
        S.op("dve", "tensor_scalar", reads=posc, writes=[r1u], out=r1u[:], in0=pos[:], scalar1=4, scalar2=None, op0=ALU.logical_shift_right)
        S.op("dve", "tensor_scalar", reads=posc, writes=[r2u], out=r2u[:], in0=pos[:], scalar1=15, scalar2=None, op0=ALU.bitwise_and)
        S.op("dve", "tensor_copy", reads=[r1u], writes=[posf], out=posf[:], in_=r1u[:])
        S.op("dve", "tensor_copy", reads=[r2u], writes=[r2f], out=r2f[:], in_=r2u[:])
        io4 = iota_f[:, 0:16].unsqueeze(1).unsqueeze(1).to_broadcast([128, 8, 16, 16])
        for (rf_, src4, dstv) in ((posf, y4[:, :, 0, :], e1v), (r2f, x4[:, :, 1, :], e2v)):
            S.op("dve", "tensor_tensor", reads=[iota_f, rf_], writes=[cand], out=c4v, in0=io4,
                 in1=rf_[:].rearrange("p (h j) -> p h j", h=8).unsqueeze(3).to_broadcast([128, 8, 16, 16]), op=ALU.is_equal)
            S.op("dve", "tensor_tensor", reads=[cand, idxf, idx128], writes=[cand], out=c4v, in0=c4v,
                 in1=src4.unsqueeze(2).to_broadcast([128, 8, 16, 16]), op=ALU.mult)
            S.op("dve", "tensor_reduce", reads=[cand], writes=[dstv], out=dstv[:], in_=cand[:].rearrange("p (q i) -> p q i", i=16), axis=AX.X, op=ALU.add)
        S.op("dve", "tensor_tensor", reads=[e1v, e2v], writes=eidc, out=eidf[:], in0=e1v[:], in1=e2v[:], op=ALU.add)
        S.op("dve", "tensor_scalar", reads=eidc, writes=eidc, out=eidf[:], in0=eidf[:], scalar1=0.0, scalar2=16383.0, op0=ALU.max, op1=ALU.min)
        S.op("dve", "tensor_copy", reads=eidc, writes=[eid], out=eid[:], in_=eidf[:])
        g3 = gate[:].rearrange("p (h n) -> p h n", h=8)
        S.op("dve", "tensor_tensor", reads=s16c, writes=[gate], out=g3, in0=s16, in1=s16[:, :, 0:1].to_broadcast([128, 8, 16]), op=ALU.subtract)
        S.op("act", "activation", reads=[gate], writes=[gate], out=gate[:], in_=gate[:], func=AF.Exp)
        S.op("dve", "tensor_reduce", reads=[gate], writes=[gsm], out=gsm[:, 0:8], in_=g3, axis=AX.X, op=ALU.add)
        S.op("dve", "reciprocal", reads=[gsm], writes=[gsm], out=gsm[:, 8:16], in_=gsm[:, 0:8])
        S.op("dve", "tensor_tensor", reads=[gate, gsm], writes=[gate], out=g3, in0=g3, in1=gsm[:, 8:16].unsqueeze(2).to_broadcast([128, 8, 16]),
             op=ALU.mult)

    LAG = 2

    def partUV(i, interleave):
        tsl = slice(i * 128, (i + 1) * 128)
        eid = eids[i % 2]
        h2 = h2s[i % 2]
        gate = gates[i % 2]
        per = (len(S.pending) + 127) // 128 if interleave else 0

        def tail(hj):
            uv_ = uvg[hj % NG]
            d_ = dg[hj % NG]
            S.op("pool", "tensor_scalar", reads=[identb, gelc[hj], gate], writes=[d_], out=d_[:], in0=identb[:], scalar1=gel[:, hj:hj + 1],
                 scalar2=gate[:, hj:hj + 1], op0=ALU.mult, op1=ALU.mult)
            for hf in range(2):
                S.op("pe", "matmul", reads=[d_, uv_], writes=[PV], out=PV[:, hf, :], lhsT=d_[:], rhs=uv_[:, D + hf * 512:D + (hf + 1) * 512],
                     start=(hj == 0), stop=(hj == 127))

        for hj in range(128):
            uv_ = uvg[hj % NG]
            S.dma("pool", uv_[:], UVB[:, :], reads=[eid], writes=[uv_], indirect=bass.IndirectOffsetOnAxis(ap=eid[:, hj:hj + 1], axis=0))
            S.op("dve", "scalar_tensor_tensor", reads=[uv_, h2], writes=[aactc[hj], accb], out=junk[:], in0=uv_[:, 0:D], scalar=1.0, in1=h2[:],
                 op0=ALU.mult, op1=ALU.mult, accum_out=aact[:, hj:hj + 1])
            S.op("act", "activation", reads=[aactc[hj]], writes=[gelc[hj]], out=gel[:, hj:hj + 1], in_=aact[:, hj:hj + 1], func=AF.Gelu)
            if hj >= LAG:
                tail(hj - LAG)
            if per:
                S.flush(per)
        for hj in range(128 - LAG, 128):
            tail(hj)
        S.flush()

    def partF(i):
        tsl = slice(i * 128, (i + 1) * 128)
        x1 = x1s[i % 2]
        S.op("dve", "tensor_tensor", reads=[PV, x1], writes=[x1], out=x1[:].rearrange("p (a n) -> p a n", a=2), in0=PV[:, :, :],
             in1=x1[:].rearrange("p (a n) -> p a n", a=2), op=ALU.add)
        rms(x1, x1, wfb, 4)
        S.dma("sp", out[tsl, :], x1[:], reads=[x1])

    if not peer:
        for i in range(NT):
            partA(i)
    else:
        partA(0)
        for i in range(NT):
            if i + 1 < NT:
                S.rec = True
                partA(i + 1)
                S.rec = False
            partUV(i, True)
            partF(i)

    S.barrier()
    print("ops", S.nops, "waits", S.nwait, "dmas", S.dcount)
    return nc


def make_consts():
    idx = np.arange(128)
    ident = np.eye(128, dtype=np.float32)
    mus = (idx[None, :] > idx[:, None]).astype(np.float32)
    mui = (idx[None, :] >= idx[:, None]).astype(np.float32)
    mls = (idx[None, :] < idx[:, None]).astype(np.float32)
    mli = (idx[None, :] <= idx[:, None]).astype(np.float32)
    masks = np.stack([mus, mui, mls, mli], axis=1)
    bones = (idx[None, :] // 64 == idx[:, None] // 64).astype(np.float32)
    sel = (idx[:, None] // 64 == np.arange(2)[None, :]).astype(np.float32)
    reset = np.ones((128, 1024), np.float32)
    reset[:, ::128] = 0.0
    invcnt = np.zeros((128, 4, 16), np.float32)
    for gi, win in enumerate((2, 4, 8, 16)):
        half = win // 2
        for t in range(half):
            invcnt[:, gi, t] = 1.0 / (t + half)
        for q in range(half - 1):
            t = S_LEN - (half - 1) + q
            invcnt[:, gi, 8 + q] = 1.0 / (S_LEN - t + half)
    return dict(c_ident=ident, c_masks=masks, c_bones=bones, c_sel=sel, c_reset=reset, c_invcnt=invcnt)


def make_in_maps(inp, peer=True):
    f = lambda a: np.ascontiguousarray(np.asarray(a, dtype=np.float32))
    shared = dict(
        w_in=f(inp["w_in"][0].reshape(8, 128, D_IN).transpose(1, 0, 2)),
        shift_mu=f(inp["shift_mu"][0]), norm1_w=f(inp["norm1_w"][0]),
        w0=f(inp["w0"][0]), a0=f(inp["a0"][0]),
        w_up=f(inp["w_up"][0].reshape(128, 512)), a_up=f(inp["a_up"][0].reshape(128, 512)),
        g_up=f(inp["g_up"][0]), k_k=f(inp["k_k"][0]), k_a=f(inp["k_a"][0]), r_k=f(inp["r_k"][0].reshape(512)),
        ln_x_w=f(inp["ln_x_w"][0]), ln_x_b=f(inp["ln_x_b"][0]),
        pool_w=f(inp["pool_w"][0].transpose(1, 0, 2)), pool_scale=f(inp["pool_scale"][0]),
        w_out=f(inp["w_out"][0].reshape(8, 128, D).transpose(1, 0, 2)),
        norm2_w=f(inp["norm2_w"][0]), norm_f_w=f(inp["norm_f_w"]),
        wq=f(inp["peer_wq"][0].reshape(8, 128, 2048).transpose(1, 0, 2)),
        keysT=f(inp["peer_keys"][0].transpose(3, 0, 1, 2).reshape(128, 16, 128)),
        peer_u=f(inp["peer_u"][0]), peer_v=f(inp["peer_v"][0]),
    )
    shared.update(make_consts())
    if not peer:
        del shared["peer_u"], shared["peer_v"]
    xs = np.asarray(inp["x"], dtype=np.float32)
    return [dict(shared, x=np.ascontiguousarray(xs[c])) for c in range(8)]


def kernel(**inputs):
    nc = build()
    in_maps = make_in_maps(inputs)
    res = run_bass_kernel_spmd(nc, in_maps, core_ids=list(range(8)))
    return np.stack([np.asarray(r["out"], dtype=np.float32) for r in res.results], axis=0)
```

```python
import numpy as np
import ml_dtypes
import concourse.bass as bass
import concourse.mybir as mybir
from concourse.bass_utils import run_bass_kernel_spmd

F32 = mybir.dt.float32
BF16 = mybir.dt.bfloat16
I32 = mybir.dt.int32
U32 = mybir.dt.uint32
AF = mybir.ActivationFunctionType
ALU = mybir.AluOpType
AX = mybir.AxisListType

S_LEN = 4096
D = 1024
NT = S_LEN // 128
RWKV_IN = 1952
D_IN = 2464
C0 = float(np.exp(-0.5))
RMS_EPS = 1e-5
GN_EPS = 64e-5
PAD = 8


class Buf:
    __slots__ = ("name", "w", "r", "excl")

    def __init__(self, name=""):
        self.name = name
        self.w = None
        self.r = {}
        self.excl = False


class T:
    def __init__(self, t, name=""):
        self.t = t
        self.b = Buf(name)

    def __getitem__(self, k):
        return self.t[k]


def _b(x):
    return x.b if isinstance(x, T) else x


class Sched:
    def __init__(self, nc, ndma=32):
        self.nc = nc
        self.es = {"pe": nc.tensor, "act": nc.scalar, "dve": nc.vector, "pool": nc.gpsimd, "sp": nc.sync}
        self.sem = {}
        self.tick = {k: 0 for k in self.es}
        self.seen = {k: {} for k in self.es}
        self._ctx = []
        for k in self.es:
            cm = nc.semaphore("s_" + k)
            self.sem[k] = cm.__enter__()
            self._ctx.append(cm)
        self.ndma = ndma
        self.dsem = []
        for i in range(ndma):
            cm = nc.semaphore("d_%d" % i)
            self.dsem.append(cm.__enter__())
            self._ctx.append(cm)
        self.dcount = 0
        self.rec = False
        self.pending = []
        self.nwait = 0
        self.nops = 0

    def _need(self, e, deps):
        best = {}
        for d in deps:
            if d is None:
                continue
            key, val = d
            if key == e and e == "pe":
                continue
            if best.get(key, 0) < val:
                best[key] = val
        for key, val in best.items():
            if self.seen[e].get(key, 0) >= val:
                continue
            sem = self.sem[key] if isinstance(key, str) else self.dsem[key]
            self.es[e].wait_ge(sem, val)
            self.nwait += 1
            self.seen[e][key] = val

    def _deps(self, reads, writes):
        deps = []
        for b in reads:
            deps.append(b.w)
        for b in writes:
            deps.append(b.w)
            for k, v in b.r.items():
                deps.append((k, v))
        return deps

    def flush(self, n=None):
        pend = self.pending
        k = len(pend) if n is None else min(n, len(pend))
        todo, self.pending = pend[:k], pend[k:]
        rec, self.rec = self.rec, False
        for kind, a, kw in todo:
            if kind == "op":
                self.op(*a, **kw)
            else:
                self.dma(*a, **kw)
        self.rec = rec

    def op(self, e, name, reads=(), writes=(), **kw):
        if self.rec:
            self.pending.append(("op", (e, name, reads, writes), kw))
            return None
        reads = [_b(x) for x in reads]
        writes = [_b(x) for x in writes]
        writes = writes + [b for b in reads if b.excl and b not in writes]
        reads = [b for b in reads if not b.excl]
        self._need(e, self._deps(reads, writes))
        ins = getattr(self.es[e], name)(**kw)
        self.tick[e] += 1
        self.nops += 1
        ins.then_inc(self.sem[e], 1)
        tk = self.tick[e]
        for b in reads:
            b.r[e] = tk
        for b in writes:
            b.w = (e, tk)
            b.r = {}
        return ins

    def dma(self, e, out, in_, reads=(), writes=(), indirect=None, **kw):
        if self.rec:
            self.pending.append(("dma", (e, out, in_, reads, writes, indirect), kw))
            return None
        reads = [_b(x) for x in reads]
        writes = [_b(x) for x in writes]
        j = self.dcount
        self.dcount += 1
        slot = j % self.ndma
        rnd = j // self.ndma
        deps = self._deps(reads, writes)
        if rnd > 0:
            deps.append((slot, 16 * rnd))
        self._need(e, deps)
        if indirect is None:
            ins = self.es[e].dma_start(out=out, in_=in_, **kw)
        else:
            ins = self.es[e].indirect_dma_start(out=out, out_offset=None, in_=in_, in_offset=indirect, **kw)
        ins.then_inc(self.dsem[slot], 16)
        self.nops += 1
        val = 16 * (rnd + 1)
        for b in reads:
            b.r[slot] = val
        for b in writes:
            b.w = (slot, val)
            b.r = {}
        return ins

    def barrier(self):
        deps = [(k, self.tick[k]) for k in self.es if self.tick[k] > 0]
        for j in range(min(self.dcount, self.ndma)):
            cnt = (self.dcount - 1 - j) // self.ndma + 1
            deps.append((j, 16 * cnt))
        for e in self.es:
            self._need(e, deps)


def build(debug=(), peer=True, cut=99, nblk=99):
    nc = bass.Bass("TRN2", target_bir_lowering=False)
    S = Sched(nc)
    guards = []

    def din(name, shape, dt=F32):
        return nc.dram_tensor(name, list(shape), dt, kind="ExternalInput").ap()

    def dscr(name, shape, dt=F32):
        kind = "ExternalOutput" if name in debug else "Internal"
        return nc.dram_tensor(name, list(shape), dt, kind=kind).ap()

    def sb(name, shape, dt=F32):
        g = nc.sbuf_tensor(name, list(shape), dt)
        t = g.__enter__()
        guards.append(g)
        return T(t, name)

    def ps(name, shape, dt=F32):
        g = nc.psum_tensor(name, list(shape), dt)
        t = g.__enter__()
        guards.append(g)
        return T(t, name)

    def release(n):
        for _ in range(n):
            g = guards.pop()
            g.__exit__(None, None, None)

    x = din("x", [S_LEN, D])
    w_in = din("w_in", [128, 8, D_IN])
    shift_mu = din("shift_mu", [2, RWKV_IN])
    norm1_w = din("norm1_w", [D])
    w0 = din("w0", [2, 512]); a0 = din("a0", [2, 512])
    w_up = din("w_up", [128, 512]); a_up = din("a_up", [128, 512])
    g_up = din("g_up", [160, 512])
    k_k = din("k_k", [512]); k_a = din("k_a", [512]); r_k = din("r_k", [512])
    ln_x_w = din("ln_x_w", [512]); ln_x_b = din("ln_x_b", [512])
    pool_w = din("pool_w", [128, 4, 128]); pool_scale = din("pool_scale", [512])
    w_out = din("w_out", [128, 8, D])
    norm2_w = din("norm2_w", [D]); norm_f_w = din("norm_f_w", [D])
    wq = din("wq", [128, 8, 2048])
    keysT = din("keysT", [128, 16, 128])
    if peer:
        peer_u = din("peer_u", [16384, D]); peer_v = din("peer_v", [16384, D])
    c_ident = din("c_ident", [128, 128])
    c_masks = din("c_masks", [128, 4, 128])
    c_bones = din("c_bones", [128, 128])
    c_sel = din("c_sel", [128, 2])
    c_reset = din("c_reset", [128, 1024])
    c_invcnt = din("c_invcnt", [128, 4, 16])
    out = nc.dram_tensor("out", [S_LEN, D], F32, kind="ExternalOutput").ap()

    RT = dscr("RT", [512, S_LEN]); KT = dscr("KT", [512, S_LEN])
    VTOK = dscr("VTOK", [S_LEN, 512])
    YP = dscr("YP", [512, S_LEN], BF16)

    identf = sb("identf", [128, 128]); identb = sb("identb", [128, 128], BF16)
    S.dma("sp", identf[:], c_ident[:, :], writes=[identf])
    S.op("dve", "tensor_copy", reads=[identf], writes=[identb], out=identb[:], in_=identf[:])
    epsc = sb("epsc", [128, 2])
    S.op("pool", "memset", writes=[epsc], ap=epsc[:, 0:1], constant=RMS_EPS)
    S.op("pool", "memset", writes=[epsc], ap=epsc[:, 1:2], constant=GN_EPS)
    NPC = 16 + 16 + 16 + 8 + 8 + 4 + 4 + 4 + 4 + 4
    pc = sb("pc", [128, NPC])
    S.op("pool", "memset", writes=[pc], ap=pc[:], constant=0.0)
    col = {}
    o = 0

    def ldcol(name, vec, n):
        nonlocal o
        col[name] = o
        nfull = n // 128
        if nfull:
            S.dma("sp", pc[:, o:o + nfull], vec[0:nfull * 128].rearrange("(c p) -> p c", p=128), writes=[pc],
                  allow_slow_non_contiguous=True)
        rem = n - nfull * 128
        if rem:
            S.dma("sp", pc[0:rem, o + nfull:o + nfull + 1], vec[nfull * 128:n].rearrange("(c p) -> p c", p=rem),
                  writes=[pc], allow_slow_non_contiguous=True)
        o += (n + 127) // 128

    ldcol("mu0", shift_mu[0, :], RWKV_IN); ldcol("mu1", shift_mu[1, :], RWKV_IN)
    col["muc"] = o; o += 16
    ldcol("w0_0", w0[0, :], 512); ldcol("w0_1", w0[1, :], 512)
    ldcol("a0_0", a0[0, :], 512); ldcol("a0_1", a0[1, :], 512)
    ldcol("k_k", k_k, 512); ldcol("k_a", k_a, 512); ldcol("r_k", r_k, 512); ldcol("pscale", pool_scale, 512)
    col["omka"] = o; o += 4
    assert o == NPC, (o, NPC)
    S.op("dve", "tensor_tensor", reads=[pc], writes=[pc], out=pc[:, col["muc"]:col["muc"] + 16],
         in0=pc[:, col["mu0"]:col["mu0"] + 16], in1=pc[:, col["mu1"]:col["mu1"] + 16], op=ALU.add)
    S.op("dve", "tensor_scalar", reads=[pc], writes=[pc], out=pc[:, col["muc"]:col["muc"] + 16],
         in0=pc[:, col["muc"]:col["muc"] + 16], scalar1=-1.0, scalar2=1.0, op0=ALU.mult, op1=ALU.add)
    S.op("dve", "tensor_scalar", reads=[pc], writes=[pc], out=pc[:, col["omka"]:col["omka"] + 4],
         in0=pc[:, col["k_a"]:col["k_a"] + 4], scalar1=-1.0, scalar2=1.0, op0=ALU.mult, op1=ALU.add)

    twd = sb("twd", [128, S_LEN], BF16)
    adT = sb("adT", [128, S_LEN], BF16)
    sgA = sb("sgA", [128, S_LEN], BF16)
    sgB = sb("sgB", [32, S_LEN], BF16)

    pall = ps("pall", [128, 8, 512])
    banks = [T(pall.t[:, i, :], "bank%d" % i) for i in range(8)]
    for bk_ in banks:
        bk_.b.excl = True

    n_ph = len(guards)
    hT = sb("hT", [128, 8, S_LEN], BF16)
    pbuf = [sb("pbuf%d" % i, [128, S_LEN + 2 * PAD]) for i in range(2)]
    for pb in pbuf:
        S.op("pool", "memset", writes=[pb], ap=pb[:, 0:PAD], constant=0.0)
        S.op("pool", "memset", writes=[pb], ap=pb[:, PAD + S_LEN:], constant=0.0)
    tA = sb("tA", [128, S_LEN + 2 * PAD]); tB = sb("tB", [128, S_LEN + 2 * PAD])
    wb1 = sb("nw1b", [128, D])
    S.dma("sp", wb1[:], norm1_w.partition_broadcast(128), writes=[wb1])
    ssq = sb("ssq", [128, 4])
    hn = [sb("hn%d" % i, [128, D], BF16) for i in range(2)]
    wf = [sb("wf%d" % i, [128, 8, 128]) for i in range(2)]
    wb = [sb("wb%d" % i, [128, 8, 128], BF16) for i in range(2)]

    xts = [tA, tB]
    ptb = [T(banks[i].t[:].bitcast(BF16), "ptb%d" % i) for i in range(2)]
    for pt_, bk in zip(ptb, banks[:2]):
        pt_.b = bk.b
    for i in range(NT):
        xt = xts[i % 2]
        S.dma("sp" if i % 2 == 0 else "act", xt[:, 0:D], x[i * 128:(i + 1) * 128, :], writes=[xt])
        h_ = hn[i % 2]
        S.op("act", "activation", reads=[xt], writes=[h_, ssq], out=h_[:], in_=xt[:, 0:D], func=AF.Square,
             accum_out=ssq[:, 0:1])
        S.op("act", "activation", reads=[ssq], writes=[ssq], out=ssq[:, 1:2], in_=ssq[:, 0:1], func=AF.Sqrt,
             scale=1.0 / D, bias=epsc[:, 0:1])
        S.op("dve", "reciprocal", reads=[ssq], writes=[ssq], out=ssq[:, 2:3], in_=ssq[:, 1:2])
        S.op("dve", "scalar_tensor_tensor", reads=[xt, ssq, wb1], writes=[h_], out=h_[:], in0=xt[:, 0:D],
             scalar=ssq[:, 2:3], in1=wb1[:], op0=ALU.mult, op1=ALU.mult)
        pt_ = ptb[i % 2]
        for kc in range(8):
            S.op("pe", "transpose", reads=[h_, identb], writes=[pt_], out=pt_.t[:, kc * 128:(kc + 1) * 128],
                 in_=h_[:, kc * 128:(kc + 1) * 128], identity=identb[:])
        S.op("act" if i % 2 == 0 else "dve", "activation" if i % 2 == 0 else "tensor_copy", reads=[pt_], writes=[hT],
             out=hT[:, :, i * 128:(i + 1) * 128], in_=pt_.t.rearrange("p (k t) -> p k t", k=8),
             **({"func": AF.Copy} if i % 2 == 0 else {}))

    chunks = []
    for i in range(4):
        chunks.append((i * 128, 128, "r", i))
    for i in range(4):
        chunks.append((512 + i * 128, 128, "k", i))
    for i in range(4):
        chunks.append((1024 + i * 128, 128, "v", i))
    chunks.append((1536, 128, "wd", 0))
    chunks.append((1664, 128, "ad", 0))
    chunks.append((1792, 128, "gdA", 0))
    chunks.append((1920, 32, "gdB", 0))
    for i in range(4):
        chunks.append((RWKV_IN + i * 128, 128, "pool", i))

    poolw_f = sb("poolw_f", [128, 4, 128]); poolw_b = sb("poolw_b", [128, 4, 128], BF16)
    S.dma("sp", poolw_f[:], pool_w[:, :, :], writes=[poolw_f])
    S.op("pool", "tensor_copy", reads=[poolw_f], writes=[poolw_b], out=poolw_b[:], in_=poolw_f[:])
    invcnt = sb("invcnt", [128, 4, 16])
    S.dma("sp", invcnt[:], c_invcnt[:, :, :], writes=[invcnt])
    plb = sb("plb", [128, S_LEN], BF16)
    ypb = sb("ypb", [128, S_LEN], BF16)
    vtk = [sb("vtk%d" % i, [128, 4, 128]) for i in range(2)]

    evac_i = 0
    for ci, (c0, w, kind, idx) in enumerate(chunks):
        wf_, wb_, pb = wf[ci % 2], wb[ci % 2], pbuf[ci % 2]
        S.dma("sp", wf_[:, :, 0:w], w_in[:, :, c0:c0 + w], writes=[wf_])
        S.op("pool", "tensor_copy", reads=[wf_], writes=[wb_], out=wb_[:, :, 0:w], in_=wf_[:, :, 0:w])
        for j in range(8):
            bk = banks[2 + (evac_i % 4)]
            for kc in range(8):
                S.op("pe", "matmul", reads=[wb_, hT], writes=[bk], out=bk[0:w, :], lhsT=wb_[:, kc, 0:w],
                     rhs=hT[:, kc, j * 512:(j + 1) * 512], start=(kc == 0), stop=(kc == 7))
            if evac_i % 2 == 0:
                S.op("act", "activation", reads=[bk], writes=[pb], out=pb[0:w, PAD + j * 512:PAD + (j + 1) * 512],
                     in_=bk[0:w, :], func=AF.Copy)
            else:
                S.op("dve", "tensor_copy", reads=[bk], writes=[pb], out=pb[0:w, PAD + j * 512:PAD + (j + 1) * 512],
                     in_=bk[0:w, :])
            evac_i += 1
        if kind != "pool":
            cc = c0 // 128
            m0 = pc[0:w, col["mu0"] + cc:col["mu0"] + cc + 1]
            m1 = pc[0:w, col["mu1"] + cc:col["mu1"] + cc + 1]
            mc = pc[0:w, col["muc"] + cc:col["muc"] + cc + 1]
            HS = S_LEN // 2
            for hf in range(2):
                lo = PAD + hf * HS
                S.op("act", "activation", reads=[pb, pc], writes=[tA], out=tA[0:w, lo:lo + HS],
                     in_=pb[0:w, lo - 1:lo - 1 + HS], func=AF.Copy, scale=m0)
                S.op("dve", "scalar_tensor_tensor", reads=[pb, pc, tA], writes=[tA], out=tA[0:w, lo:lo + HS],
                     in0=pb[0:w, lo + 1:lo + 1 + HS], scalar=m1, in1=tA[0:w, lo:lo + HS], op0=ALU.mult, op1=ALU.add)
                S.op("dve", "scalar_tensor_tensor", reads=[pb, pc, tA], writes=[tB], out=tB[0:w, lo:lo + HS],
                     in0=pb[0:w, lo:lo + HS], scalar=mc, in1=tA[0:w, lo:lo + HS], op0=ALU.mult, op1=ALU.add)
            res = tB
            R_ = res[0:w, PAD:PAD + S_LEN]
            if kind == "r":
                S.dma("sp", RT[idx * 128:(idx + 1) * 128, :], R_, reads=[res])
            elif kind == "k":
                S.dma("sp", KT[idx * 128:(idx + 1) * 128, :], R_, reads=[res])
            elif kind == "v":
                for g4 in range(8):
                    bk = banks[6 + g4 % 2]
                    for q in range(4):
                        tt = g4 * 4 + q
                        S.op("pe", "transpose", reads=[res, identf], writes=[bk], out=bk[:, q * 128:(q + 1) * 128],
                             in_=res[:, PAD + tt * 128:PAD + (tt + 1) * 128], identity=identf[:])
                    vt = vtk[g4 % 2]
                    S.op("act", "activation", reads=[bk], writes=[vt], out=vt[:],
                         in_=bk.t.rearrange("p (n c) -> p n c", n=4), func=AF.Copy)
                    S.dma("act", VTOK[g4 * 512:(g4 + 1) * 512, idx * 128:(idx + 1) * 128].rearrange("(n p) c -> p n c", p=128),
                          vt[:], reads=[vt])
            elif kind == "wd":
                S.op("act", "activation", reads=[res], writes=[twd], out=twd[:], in_=R_, func=AF.Tanh)
            elif kind == "ad":
                S.op("act", "activation", reads=[res], writes=[adT], out=adT[:], in_=R_, func=AF.Copy)
            elif kind == "gdA":
                S.op("act", "activation", reads=[res], writes=[sgA], out=sgA[:], in_=R_, func=AF.Sigmoid)
            elif kind == "gdB":
                S.op("act", "activation", reads=[res], writes=[sgB], out=sgB[:], in_=R_, func=AF.Sigmoid)
        else:
            gi = idx
            win = (2, 4, 8, 16)[gi]
            half = win // 2
            W_ = S_LEN + 2 * PAD
            S.op("dve", "tensor_tensor", reads=[pb], writes=[tA], out=tA[:, 1:W_], in0=pb[:, 0:W_ - 1], in1=pb[:, 1:W_],
                 op=ALU.add)
            cur, oth = tA, tB
            lo_, hi_ = 1, W_
            sh = 1
            for lev in range(gi):
                nlo, nhi = lo_ + sh, hi_ - sh
                S.op("dve" if lev % 2 else "pool", "tensor_tensor", reads=[cur], writes=[oth], out=oth[:, nlo:nhi],
                     in0=cur[:, nlo - sh:nhi - sh], in1=cur[:, nlo + sh:nhi + sh], op=ALU.add)
                cur, oth = oth, cur
                lo_, hi_ = nlo, nhi
                sh *= 2
            S.op("dve", "scalar_tensor_tensor", reads=[cur, pb], writes=[plb], out=plb[:], in0=cur[:, PAD:PAD + S_LEN],
                 scalar=1.0 / win, in1=pb[:, PAD:PAD + S_LEN], op0=ALU.mult, op1=ALU.subtract)
            S.op("dve", "tensor_tensor", reads=[cur, invcnt], writes=[oth], out=oth[:, 0:half], in0=cur[:, PAD:PAD + half],
                 in1=invcnt[:, gi, 0:half], op=ALU.mult)
            S.op("dve", "tensor_tensor", reads=[oth, pb], writes=[plb], out=plb[:, 0:half], in0=oth[:, 0:half],
                 in1=pb[:, PAD:PAD + half], op=ALU.subtract)
            if half > 1:
                nr = half - 1
                S.op("dve", "tensor_tensor", reads=[cur, invcnt], writes=[oth], out=oth[:, 8:8 + nr],
                     in0=cur[:, PAD + S_LEN - nr:PAD + S_LEN], in1=invcnt[:, gi, 8:8 + nr], op=ALU.mult)
                S.op("dve", "tensor_tensor", reads=[oth, pb], writes=[plb], out=plb[:, S_LEN - nr:S_LEN],
                     in0=oth[:, 8:8 + nr], in1=pb[:, PAD + S_LEN - nr:PAD + S_LEN], op=ALU.subtract)
            for j in range(8):
                bk = banks[2 + (evac_i % 4)]
                evac_i += 1
                S.op("pe", "matmul", reads=[poolw_b, plb], writes=[bk], out=bk[:, :], lhsT=poolw_b[:, gi, :],
                     rhs=plb[:, j * 512:(j + 1) * 512], start=True, stop=True)
                S.op("act", "activation", reads=[bk, pc], writes=[ypb], out=ypb[:, j * 512:(j + 1) * 512], in_=bk[:, :],
                     func=AF.Copy, scale=pc[:, col["pscale"] + gi:col["pscale"] + gi + 1])
            S.dma("sp", YP[gi * 128:(gi + 1) * 128, :], ypb[:], reads=[ypb])

    S.barrier()
    release(len(guards) - n_ph)

    OB = dscr("OB", [S_LEN, 512])
    n_ph3 = len(guards)
    obuf = sb("obuf", [128, NT, 512])
    coef = sb("coef", [128, NT, 8])
    S.op("pool", "memset", writes=[coef], ap=coef[:], constant=0.0)
    ctmp = sb("ctmp", [128, 4, 128])
    S.dma("sp", ctmp[:], c_masks[:, :, :], writes=[ctmp])
    mT2 = [sb("mT2_%d" % d, [128, 2, 256], BF16) for d in range(2)]
    mN2 = [sb("mN2_%d" % d, [128, 2, 128], BF16) for d in range(2)]
    for d in range(2):
        src = ctmp[:, 0:2, :] if d == 0 else ctmp[:, 2:4, :]
        nsrc = ctmp[:, 2, :] if d == 0 else ctmp[:, 0, :]
        for e in range(2):
            S.op("dve", "tensor_copy", reads=[ctmp], writes=[mT2[d]], out=mT2[d][:, e, :].rearrange("p (a t) -> p a t", a=2), in_=src)
            S.op("dve", "tensor_copy", reads=[ctmp], writes=[mN2[d]], out=mN2[d][:, e, :], in_=nsrc)
    ident2 = sb("ident2", [128, 2, 128], BF16)
    for e in range(2):
        S.op("dve", "tensor_copy", reads=[identf], writes=[ident2], out=ident2[:, e, :], in_=identf[:])
    ident2s = sb("ident2s", [128, 64])
    S.op("dve", "tensor_tensor", reads=[identf], writes=[ident2s], out=ident2s[:], in0=identf[:, 0:64], in1=identf[:, 64:128], op=ALU.add)
    ctmp2 = sb("ctmp2", [128, 130])
    S.dma("sp", ctmp2[:, 0:128], c_bones[:, :], writes=[ctmp2])
    S.dma("sp", ctmp2[:, 128:130], c_sel[:, :], writes=[ctmp2])
    bones = sb("bones", [128, 128], BF16); selb = sb("selb", [128, 2], BF16)
    S.op("dve", "tensor_copy", reads=[ctmp2], writes=[bones], out=bones[:], in_=ctmp2[:, 0:128])
    S.op("dve", "tensor_copy", reads=[ctmp2], writes=[selb], out=selb[:], in_=ctmp2[:, 128:130])
    BLK = 512
    NB = S_LEN // BLK
    CPB = BLK // 128
    reset = sb("reset", [128, BLK])
    S.dma("sp", reset[:], c_reset[:, 0:BLK], writes=[reset])
    upf = sb("upf", [128, 1024]); wupb = sb("wupb", [128, 512], BF16); aupb = sb("aupb", [128, 512], BF16)
    S.dma("sp", upf[:, 0:512], w_up[:, :], writes=[upf])
    S.dma("sp", upf[:, 512:1024], a_up[:, :], writes=[upf])
    S.op("dve", "tensor_copy", reads=[upf], writes=[wupb], out=wupb[:], in_=upf[:, 0:512])
    S.op("dve", "tensor_copy", reads=[upf], writes=[aupb], out=aupb[:], in_=upf[:, 512:1024])

    def f32t(n):
        return sb(n, [128, BLK])
    rF, kF, alpha, sg, cs, cum, t1, t2, t3, t4, t5 = [f32t("p3_%d" % i) for i in range(11)]
    vtF = sb("vtF", [128, CPB, 128])
    sqb = sb("sqb", [128, BLK], BF16); xbb = sb("xbb", [128, BLK], BF16)
    etot = sb("etot", [128, CPB])
    NPB = 2
    prep = []
    for i in range(NPB):
        prep.append(dict(
            ar=sb("ar%d" % i, [128, CPB, 2, 128], BF16), kt=sb("kt%d" % i, [128, BLK], BF16), bt=sb("bt%d" % i, [128, BLK], BF16),
            kh=sb("kh%d" % i, [128, BLK], BF16), bh=sb("bh%d" % i, [128, BLK], BF16),
            atk=sb("atk%d" % i, [128, CPB, 128], BF16), bhk=sb("bhk%d" % i, [128, CPB, 128], BF16),
            khk=sb("khk%d" % i, [128, CPB, 128], BF16), vbf=sb("vbf%d" % i, [128, CPB, 128], BF16),
            etot=sb("etot%d" % i, [128, CPB])))
    NSLOT = 4
    slots = []
    for i in range(NSLOT):
        sl = dict(
            Z=None,
            arb=sb("arb%d" % i, [128, 2, 128], BF16), s2b=sb("s2b%d" % i, [128, 2, 256], BF16),
            P=[sb("P%d_%d" % (i, j), [128, 2, 128], BF16) for j in range(2)],
            PTT=[sb("PTT%d_%d" % (i, j), [128, 2, 256], BF16) for j in range(2)],
            ysb=sb("ysb%d" % i, [128, 2, 64], BF16), w1m=sb("w1m%d" % i, [128, 2, 128], BF16),
            phiT=sb("phiT%d" % i, [128, 64]), xT=sb("xT%d" % i, [128, 128]))
        sl["Z"] = T(pall.t[:, 2 * i:2 * i + 2, :], "Z%d" % i)
        sl["Z"].b.excl = True
        slots.append(sl)
    Hs = [sb("H%d" % i, [128, 64]) for i in range(2)]
    pbk = []
    for k_ in range(2 * NSLOT):
        t_ = T(pall.t[:, k_, :], "pbk%d" % k_)
        t_.b = slots[k_ // 2]["Z"].b
        pbk.append(t_)
    NPBK = len(pbk)
    pbi = 0
    evc = [0]

    def evac_copy(out, in_, reads, writes):
        evc[0] += 1
        if evc[0] % 2 == 0:
            S.op("act", "activation", reads=reads, writes=writes, out=out, in_=in_, func=AF.Copy)
        else:
            S.op("dve", "tensor_copy", reads=reads, writes=writes, out=out, in_=in_)

    def evac2(out3, in3, reads, writes, eng=None):
        if eng is None:
            evc[0] += 1
            eng = "act" if evc[0] % 2 == 0 else "dve"
        if eng == "act":
            for e in range(2):
                S.op("act", "activation", reads=reads, writes=writes, out=out3[:, e, :], in_=in3[:, e, :], func=AF.Copy)
        else:
            S.op("dve", "tensor_copy", reads=reads, writes=writes, out=out3, in_=in3)

    def v3(ap_, c=CPB):
        return ap_.rearrange("p (c t) -> p c t", c=c)

    if peer:
        UVB = dscr("UVB", [16384, 2 * D], BF16)
        cvf = [sb("cvf%d" % i, [128, 2 * D]) for i in range(2)]
        cvb = [sb("cvb%d" % i, [128, 2 * D], BF16) for i in range(2)]
        S.rec = True
        ci_ = 0
        for tbl, dst in ((peer_u, UVB[:, 0:D]), (peer_v, UVB[:, D:2 * D])):
            for c in range(64):
                f_, b_ = cvf[ci_ % 2], cvb[ci_ % 2]
                rows = slice(c * 256, (c + 1) * 256)
                S.dma("sp", f_[:], tbl[rows, :].rearrange("(p r) d -> p (r d)", r=2), writes=[f_])
                S.op("pool", "tensor_copy", reads=[f_], writes=[b_], out=b_[:], in_=f_[:])
                S.dma("sp", dst[rows, :].rearrange("(p r) d -> p r d", r=2), b_[:].rearrange("p (r d) -> p r d", r=2), reads=[b_])
                ci_ += 1
        S.rec = False
        cv_pending = S.pending
        S.pending = []
    else:
        cv_pending = []

    def cv_flush(n):
        nonlocal cv_pending
        keep = S.pending
        S.pending = cv_pending
        S.flush(n)
        cv_pending = S.pending
        S.pending = keep

    for hp in range(4):
        hsl = slice(hp * 128, (hp + 1) * 128)
        for d in range(2):
            hcur = 0
            S.op("pool", "memset", writes=[Hs[0]], ap=Hs[0][:], constant=0.0)
            blks = range(NB) if d == 0 else range(NB - 1, -1, -1)
            for bi, blk in enumerate(blks):
                if hp * 16 + d * 8 + bi >= nblk:
                    continue
                cv_flush(6)
                tsl = slice(blk * BLK, (blk + 1) * BLK)
                pr = prep[bi % NPB]
                S.dma("sp", rF[:], RT[hsl, tsl], writes=[rF])
                S.dma("act", kF[:], KT[hsl, tsl], writes=[kF])
                S.dma("sp", vtF[:], VTOK[tsl, hsl].rearrange("(n p) c -> p n c", p=128), writes=[vtF])
                S.op("pool", "tensor_copy", reads=[vtF], writes=[pr["vbf"]], out=pr["vbf"][:], in_=vtF[:])
                bk = pbk[pbi % NPBK]; pbi += 1
                S.op("pe", "matmul", reads=[aupb, adT], writes=[bk], out=bk[:, :], lhsT=aupb[64 * d:64 * d + 64, hsl],
                     rhs=adT[64 * d:64 * d + 64, tsl], start=True, stop=True)
                S.op("act", "activation", reads=[bk, pc], writes=[alpha], out=alpha[:], in_=bk[:, :], func=AF.Sigmoid,
                     bias=pc[:, col["a0_%d" % d] + hp:col["a0_%d" % d] + hp + 1])
                bk = pbk[pbi % NPBK]; pbi += 1
                S.op("pe", "matmul", reads=[wupb, twd], writes=[bk], out=bk[:, :], lhsT=wupb[64 * d:64 * d + 64, hsl],
                     rhs=twd[64 * d:64 * d + 64, tsl], start=True, stop=True)
                S.op("act", "activation", reads=[bk, pc], writes=[sg], out=sg[:], in_=bk[:, :], func=AF.Sigmoid,
                     bias=pc[:, col["w0_%d" % d] + hp:col["w0_%d" % d] + hp + 1])
                S.op("dve", "tensor_tensor_scan", reads=[reset, sg], writes=[cs], out=cs[:], data0=reset[:], data1=sg[:],
                     initial=0.0, op0=ALU.mult, op1=ALU.add)
                S.op("act", "activation", reads=[cs], writes=[pr["etot"]], out=pr["etot"][:], in_=v3(cs[:])[:, :, 127], func=AF.Exp,
                     scale=-C0)
                if d == 0:
                    cm = cs
                else:
                    S.op("dve", "tensor_tensor", reads=[sg, cs], writes=[t1], out=t1[:], in0=sg[:], in1=cs[:], op=ALU.subtract)
                    S.op("dve", "tensor_tensor", reads=[t1, cs], writes=[cum], out=v3(cum[:]), in0=v3(t1[:]),
                         in1=v3(cs[:])[:, :, 127:128].to_broadcast([128, CPB, 128]), op=ALU.add)
                    cm = cum
                S.op("pool", "tensor_tensor", reads=[cm, sg], writes=[t1], out=t1[:], in0=cm[:], in1=sg[:], op=ALU.subtract)
                S.op("act", "activation", reads=[t1], writes=[t1], out=t1[:], in_=t1[:], func=AF.Exp, scale=-C0)
                S.op("act", "activation", reads=[cm], writes=[t2], out=t2[:], in_=cm[:], func=AF.Exp, scale=-C0)
                S.op("act", "activation", reads=[cm], writes=[t3], out=t3[:], in_=cm[:], func=AF.Exp, scale=C0)
                S.op("dve", "tensor_scalar", reads=[kF, pc], writes=[t4], out=t4[:], in0=kF[:],
                     scalar1=pc[:, col["k_k"] + hp:col["k_k"] + hp + 1], scalar2=None, op0=ALU.mult)
                S.op("pool", "tensor_tensor", reads=[t4], writes=[sqb], out=sqb[:], in0=t4[:], in1=t4[:], op=ALU.mult)
                bk = pbk[pbi % NPBK]; pbi += 1
                S.op("pe", "matmul", reads=[bones, sqb], writes=[bk], out=bk[:, :], lhsT=bones[:], rhs=sqb[:], start=True, stop=True)
                S.op("act", "activation", reads=[bk], writes=[t5], out=t5[:], in_=bk[:, :], func=AF.Sqrt)
                S.op("dve", "tensor_scalar", reads=[t5], writes=[t5], out=t5[:], in0=t5[:], scalar1=1e-12, scalar2=None, op0=ALU.max)
                S.op("dve", "reciprocal", reads=[t5], writes=[t5], out=t5[:], in_=t5[:])
                S.op("dve", "tensor_tensor", reads=[t4, t5], writes=[t4], out=t4[:], in0=t4[:], in1=t5[:], op=ALU.mult)
                S.op("act", "activation", reads=[alpha, pc], writes=[t5], out=t5[:], in_=alpha[:], func=AF.Identity,
                     scale=pc[:, col["k_a"] + hp:col["k_a"] + hp + 1], bias=pc[:, col["omka"] + hp:col["omka"] + hp + 1])
                S.op("dve", "tensor_tensor", reads=[t5, kF], writes=[t5], out=t5[:], in0=t5[:], in1=kF[:], op=ALU.mult)
                ar = pr["ar"]
                S.op("dve", "tensor_tensor", reads=[rF, t2], writes=[ar], out=ar[:, :, 1, :], in0=v3(rF[:]), in1=v3(t2[:]), op=ALU.mult)
                S.op("dve", "scalar_tensor_tensor", reads=[t4, t1], writes=[ar], out=ar[:, :, 0, :], in0=v3(t4[:]), scalar=-1.0,
                     in1=v3(t1[:]), op0=ALU.mult, op1=ALU.mult)
                S.op("pool", "tensor_tensor", reads=[t5, t3], writes=[pr["kt"]], out=pr["kt"][:], in0=t5[:], in1=t3[:], op=ALU.mult)
                S.op("pool", "tensor_tensor", reads=[t4, alpha], writes=[t2], out=t2[:], in0=t4[:], in1=alpha[:], op=ALU.mult)
                S.op("pool", "tensor_tensor", reads=[t2, t3], writes=[pr["bt"]], out=pr["bt"][:], in0=t2[:], in1=t3[:], op=ALU.mult)
                etb = pr["etot"][:, 0:CPB].unsqueeze(2).to_broadcast([128, CPB, 128])
                S.op("dve", "tensor_tensor", reads=[pr["kt"], pr["etot"]], writes=[pr["kh"]], out=v3(pr["kh"][:]), in0=v3(pr["kt"][:]),
                     in1=etb, op=ALU.mult)
                S.op("dve", "tensor_tensor", reads=[pr["bt"], pr["etot"]], writes=[pr["bh"]], out=v3(pr["bh"][:]), in0=v3(pr["bt"][:]),
                     in1=etb, op=ALU.mult)
                S.op("dve", "scalar_tensor_tensor", reads=[rF, pc, t5], writes=[xbb], out=xbb[:], in0=rF[:],
                     scalar=pc[:, col["r_k"] + hp:col["r_k"] + hp + 1], in1=t5[:], op0=ALU.mult, op1=ALU.mult)
                bk = pbk[pbi % NPBK]; pbi += 1
                for c in range(CPB):
                    S.op("pe", "matmul", reads=[xbb, selb], writes=[bk], out=bk[:, 2 * c:2 * c + 2], lhsT=xbb[:, c * 128:(c + 1) * 128],
                         rhs=selb[:], start=True, stop=True)
                cf = coef[:, blk * CPB:(blk + 1) * CPB, 2 * hp:2 * hp + 2]
                S.op("dve", "tensor_tensor", reads=[bk, coef], writes=[coef], out=cf, in0=bk[:, 0:2 * CPB].rearrange("p (c e) -> p c e", e=2),
                     in1=cf, op=ALU.add)
                for nm, srcT in (("atk", None), ("bhk", pr["bh"]), ("khk", pr["kh"])):
                    bk = pbk[pbi % NPBK]; pbi += 1
                    bkb = bk.t[:].bitcast(BF16)
                    for c in range(CPB):
                        in_ = ar[:, c, 0, :] if srcT is None else srcT[:, c * 128:(c + 1) * 128]
                        S.op("pe", "transpose", reads=[ar if srcT is None else srcT, identb], writes=[bk],
                             out=bkb[:, c * 128:(c + 1) * 128], in_=in_, identity=identb[:])
                    evac_copy(pr[nm][:], bkb[:, 0:CPB * 128].rearrange("p (c t) -> p c t", c=CPB), [bk], [pr[nm]])

                if cut <= 1:
                    continue
                corder = list(range(CPB)) if d == 0 else list(range(CPB - 1, -1, -1))
                for g0 in range(0, CPB, NSLOT):
                    grp = corder[g0:g0 + NSLOT]
                    for si, c in enumerate(grp):
                        sl = slots[si]
                        Z = sl["Z"]
                        csl = slice(c * 128, (c + 1) * 128)
                        for e in range(2):
                            ps_ = slice(64 * e, 64 * e + 64)
                            S.op("pe", "matmul", reads=[pr["bt"], ar], writes=[Z], out=Z[:, e, 0:256], lhsT=pr["bt"][ps_, csl],
                                 rhs=ar[ps_, c, :, :], start=True, stop=True)
                            S.op("pe", "matmul", reads=[pr["kt"], ar], writes=[Z], out=Z[:, e, 256:512], lhsT=pr["kt"][ps_, csl],
                                 rhs=ar[ps_, c, :, :], start=True, stop=True)
                        PTT0 = sl["PTT"][0]; PTT1 = sl["PTT"][1]
                        S.op("dve", "tensor_tensor", reads=[Z, mT2[d]], writes=[PTT0], out=PTT0[:, :, 0:128], in0=Z[:, :, 0:128],
                             in1=mT2[d][:, :, 0:128], op=ALU.mult)
                        S.op("dve", "tensor_tensor", reads=[Z, mT2[d]], writes=[sl["arb"]], out=sl["arb"][:], in0=Z[:, :, 128:256],
                             in1=mT2[d][:, :, 128:256], op=ALU.mult)
                        S.op("dve", "tensor_tensor", reads=[Z, mT2[d]], writes=[sl["s2b"]], out=sl["s2b"][:], in0=Z[:, :, 256:512], in1=mT2[d][:],
                             op=ALU.mult)
                        S.op("pool", "tensor_tensor", reads=[PTT0, ident2], writes=[PTT1], out=PTT1[:, :, 128:256], in0=PTT0[:, :, 0:128],
                             in1=ident2[:], op=ALU.add)
                    for si, c in enumerate(grp):
                        sl = slots[si]
                        Z = sl["Z"]
                        for e in range(2):
                            ps_ = slice(64 * e, 64 * e + 64)
                            S.op("pe", "matmul", reads=[ar, pr["bt"]], writes=[Z], out=Z[:, e, 0:128], lhsT=ar[ps_, c, 0, :],
                                 rhs=pr["bt"][ps_, c * 128:(c + 1) * 128], start=True, stop=True)
                        S.op("dve", "tensor_tensor", reads=[Z, mN2[d]], writes=[sl["P"][0]], out=sl["P"][0][:], in0=Z[:, :, 0:128], in1=mN2[d][:],
                             op=ALU.mult)
                    if cut <= 2:
                        continue
                    for k in range(7):
                        for si, c in enumerate(grp):
                            sl = slots[si]
                            Z = sl["Z"]
                            Px, Py = sl["P"][k % 2], sl["P"][(k + 1) % 2]
                            Tx, Ty = sl["PTT"][k % 2], sl["PTT"][(k + 1) % 2]
                            for e in range(2):
                                if k == 0:
                                    S.op("pe", "matmul", reads=[Px, Tx], writes=[Z], out=Z[:, e, 0:128], lhsT=Px[:, e, :],
                                         rhs=Tx[:, e, 0:128], start=True, stop=True)
                                elif k <= 4:
                                    S.op("pe", "matmul", reads=[Px, Tx], writes=[Z], out=Z[:, e, 0:256], lhsT=Px[:, e, :],
                                         rhs=Tx[:, e, :], start=True, stop=True)
                                else:
                                    S.op("pe", "matmul", reads=[Px, Tx], writes=[Z], out=Z[:, e, 128:256], lhsT=Px[:, e, :],
                                         rhs=Tx[:, e, 128:256], start=True, stop=True)
                                if k <= 5:
                                    S.op("pe", "matmul", reads=[Px, Tx], writes=[Z], out=Z[:, e, 256:384], lhsT=Tx[:, e, 0:128],
                                         rhs=Px[:, e, :], start=True, stop=True)
                            if k <= 5:
                                evac2(Py[:], Z[:, :, 256:384], [Z], [Py], eng="act")
                            if k <= 4:
                                S.op("dve", "tensor_copy", reads=[Z], writes=[Ty], out=Ty[:, :, 0:128], in_=Z[:, :, 0:128])
                            if k >= 1:
                                S.op("dve", "tensor_tensor", reads=[Z, Tx], writes=[Ty], out=Ty[:, :, 128:256], in0=Z[:, :, 128:256],
                                     in1=Tx[:, :, 128:256], op=ALU.add)
                    if cut <= 3:
                        continue
                    TTF = 1
                    for si, c in enumerate(grp):
                        sl = slots[si]
                        Z = sl["Z"]
                        for e in range(2):
                            S.op("pe", "matmul", reads=[sl["s2b"], pr["vbf"]], writes=[Z], out=Z[:, e, 384:448], lhsT=sl["s2b"][:, e, 0:128],
                                 rhs=pr["vbf"][:, c, 64 * e:64 * e + 64], start=True, stop=True)
                        evac2(sl["ysb"][:], Z[:, :, 384:448], [Z], [sl["ysb"]], eng="act")
                    if cut <= 3.1:
                        continue
                    for si, c in enumerate(grp):
                        sl = slots[si]
                        Z = sl["Z"]
                        TT = sl["PTT"][TTF]
                        for e in range(2):
                            S.op("pe", "matmul", reads=[TT, sl["ysb"]], writes=[Z], out=Z[:, e, 0:64], lhsT=TT[:, e, 128:256],
                                 rhs=sl["ysb"][:, e, :], start=True, stop=True)
                            S.op("pe", "matmul", reads=[TT, pr["atk"]], writes=[Z], out=Z[:, e, 64:128], lhsT=TT[:, e, 128:256],
                                 rhs=pr["atk"][:, c, 64 * e:64 * e + 64], start=True, stop=True)
                        evac2(sl["w1m"][:], Z[:, :, 0:128], [Z], [sl["w1m"]], eng="act")
                    if cut <= 3.2:
                        continue
                    for si, c in enumerate(grp):
                        sl = slots[si]
                        Z = sl["Z"]
                        w1m = sl["w1m"]
                        for e in range(2):
                            ps_ = slice(64 * e, 64 * e + 64)
                            S.op("pe", "matmul", reads=[w1m, pr["bhk"]], writes=[Z], out=Z[ps_, e, 448:512], lhsT=w1m[:, e, 64:128],
                                 rhs=pr["bhk"][:, c, 64 * e:64 * e + 64], start=True, stop=True)
                            S.op("pe", "matmul", reads=[w1m, sl["arb"]], writes=[Z], out=Z[ps_, e, 128:256], lhsT=w1m[:, e, 64:128],
                                 rhs=sl["arb"][:, e, :], start=True, stop=True)
                        if cut <= 3.3:
                            continue
                        for e in range(2):
                            ps_ = slice(64 * e, 64 * e + 64)
                            S.op("dve", "scalar_tensor_tensor", reads=[ident2s, pr["etot"], Z], writes=[sl["phiT"]], out=sl["phiT"][ps_, :],
                                 in0=ident2s[ps_, :], scalar=pr["etot"][ps_, c:c + 1], in1=Z[ps_, e, 448:512], op0=ALU.mult, op1=ALU.add)
                            S.op("dve", "tensor_tensor", reads=[Z, ar], writes=[sl["xT"]], out=sl["xT"][ps_, :], in0=Z[ps_, e, 128:256],
                                 in1=ar[ps_, c, 1, :], op=ALU.add)
                    if cut <= 4:
                        continue
                    for si, c in enumerate(grp):
                        sl = slots[si]
                        Z = sl["Z"]
                        w1m = sl["w1m"]
                        Hc, Hn = Hs[hcur], Hs[1 - hcur]
                        for e in range(2):
                            ps_ = slice(64 * e, 64 * e + 64)
                            vv = pr["vbf"][:, c, 64 * e:64 * e + 64]
                            S.op("pe", "matmul", reads=[sl["arb"], w1m], writes=[Z], out=Z[:, e, 256:320], lhsT=sl["arb"][:, e, :],
                                 rhs=w1m[:, e, 0:64], start=True, stop=False)
                            S.op("pe", "matmul", reads=[sl["s2b"], pr["vbf"]], writes=[Z], out=Z[:, e, 256:320],
                                 lhsT=sl["s2b"][:, e, 128:256], rhs=vv, start=False, stop=False)
                            S.op("pe", "matmul", reads=[sl["xT"], Hc], writes=[Z], out=Z[:, e, 256:320], lhsT=sl["xT"][ps_, :],
                                 rhs=Hc[ps_, :], start=False, stop=True)
                            S.op("pe", "matmul", reads=[pr["bhk"], w1m], writes=[Z], out=Z[ps_, e, 320:384], lhsT=pr["bhk"][:, c, 64 * e:64 * e + 64],
                                 rhs=w1m[:, e, 0:64], start=True, stop=False)
                            S.op("pe", "matmul", reads=[pr["khk"], pr["vbf"]], writes=[Z], out=Z[ps_, e, 320:384],
                                 lhsT=pr["khk"][:, c, 64 * e:64 * e + 64], rhs=vv, start=False, stop=False)
                            S.op("pe", "matmul", reads=[sl["phiT"], Hc], writes=[Z], out=Z[ps_, e, 320:384], lhsT=sl["phiT"][ps_, :],
                                 rhs=Hc[ps_, :], start=False, stop=True)
                        for e in range(2):
                            ps_ = slice(64 * e, 64 * e + 64)
                            S.op("act", "activation", reads=[Z], writes=[Hn], out=Hn[ps_, :], in_=Z[ps_, e, 320:384], func=AF.Copy)
                        hcur = 1 - hcur
                        tile_i = blk * CPB + c
                        osl = obuf[:, tile_i, hp * 128:(hp + 1) * 128].rearrange("p (e n) -> p e n", e=2)
                        if d == 0:
                            S.op("dve", "tensor_copy", reads=[Z], writes=[obuf], out=osl, in_=Z[:, :, 256:320])
                        else:
                            S.op("dve", "tensor_tensor", reads=[Z, obuf], writes=[obuf], out=osl, in0=Z[:, :, 256:320], in1=osl, op=ALU.add)

    cv_flush(None)
    for q in range(8):
        S.dma("sp" if q % 2 == 0 else "act", OB[q * 512:(q + 1) * 512, :].rearrange("(n p) c -> p n c", p=128), obuf[:, q * 4:(q + 1) * 4, :], reads=[obuf])
    CF = dscr("CF", [128, NT * 8])
    S.dma("sp", CF[:, :], coef[:].rearrange("p n e -> p (n e)"), reads=[coef])
    S.barrier()
    release(len(guards) - n_ph3)

    X1 = dscr("X1", [S_LEN, D])
    coef = sb("coef5", [128, NT * 8])
    S.dma("sp", coef[:], CF[:, :], writes=[coef])
    lnw_b = sb("lnw_b", [128, 512]); lnb_b = sb("lnb_b", [128, 512]); wb2 = sb("nw2b", [128, D]); wfb = sb("nwfb", [128, D])
    S.dma("sp", lnw_b[:], ln_x_w.partition_broadcast(128), writes=[lnw_b])
    S.dma("sp", lnb_b[:], ln_x_b.partition_broadcast(128), writes=[lnb_b])
    S.dma("sp", wb2[:], norm2_w.partition_broadcast(128), writes=[wb2])
    S.dma("sp", wfb[:], norm_f_w.partition_broadcast(128), writes=[wfb])
    stg = sb("stg", [128, 2048])
    gupA = sb("gupA", [128, 512], BF16); gupB = sb("gupB", [32, 512], BF16)
    S.dma("sp", stg[:, 0:512], g_up[0:128, :], writes=[stg])
    S.dma("sp", stg[0:32, 512:1024], g_up[128:160, :], writes=[stg])
    S.op("dve", "tensor_copy", reads=[stg], writes=[gupA], out=gupA[:], in_=stg[:, 0:512])
    S.op("dve", "tensor_copy", reads=[stg], writes=[gupB], out=gupB[:], in_=stg[0:32, 512:1024])
    woutb = sb("woutb", [128, 8, D], BF16)
    for kc in range(0, 8, 2):
        S.dma("sp", stg[:].rearrange("p (k n) -> p k n", k=2), w_out[:, kc:kc + 2, :], writes=[stg])
        S.op("dve", "tensor_copy", reads=[stg], writes=[woutb], out=woutb[:, kc:kc + 2, :], in_=stg[:].rearrange("p (k n) -> p k n", k=2))
    if peer:
        wqb = sb("wqb", [128, 8, 2048], BF16)
        for kc in range(8):
            S.dma("sp", stg[:], wq[:, kc, :], writes=[stg])
            S.op("dve", "tensor_copy", reads=[stg], writes=[wqb], out=wqb[:, kc, :], in_=stg[:])
        keyb = sb("keyb", [128, 16, 128], BF16)
        S.dma("sp", stg[:].rearrange("p (k n) -> p k n", k=16), keysT[:, :, :], writes=[stg])
        S.op("dve", "tensor_copy", reads=[stg], writes=[keyb], out=keyb[:], in_=stg[:].rearrange("p (k n) -> p k n", k=16))
        iota_i = sb("iota_i", [128, 256], I32); iota_f = sb("iota_f", [128, 256])
        S.op("pool", "iota", writes=[iota_i], out=iota_i[:], pattern=[[1, 256]], base=0, channel_multiplier=0)
        S.op("dve", "tensor_copy", reads=[iota_i], writes=[iota_f], out=iota_f[:], in_=iota_i[:])
        sc2 = sb("sc2", [128, 2048])
        m1 = sb("m1", [128, 256]); i1 = sb("i1", [128, 256], U32); idxf = sb("idxf", [128, 256]); idx128 = sb("idx128", [128, 256])
        cand = sb("cand", [128, 2048])
        r1u = sb("r1u", [128, 128], U32); r2u = sb("r2u", [128, 128], U32); r2f = sb("r2f", [128, 128])
        e1v = sb("e1v", [128, 128]); e2v = sb("e2v", [128, 128])
        sc16 = sb("sc16", [128, 128]); pos = sb("pos", [128, 128], U32); posf = sb("posf", [128, 128])
        eidf = sb("eidf", [128, 128]); eid = sb("eid", [128, 128], U32)
        gsm = sb("gsm", [128, 16]); gate = sb("gate", [128, 128]); aact = sb("aact", [128, 128]); wgt = sb("wgt", [128, 128])
        junk = sb("junk", [128, D], BF16)
        NG = 7
        m1c = [Buf() for _ in range(16)]; i1c = [Buf() for _ in range(16)]; sc2c = [Buf() for _ in range(16)]
        s16c = [Buf() for _ in range(8)]; posc = [Buf() for _ in range(8)]
        eidc = [Buf() for _ in range(128)]; aactc = [Buf() for _ in range(128)]
        accb = Buf("accb")
        uvg = [sb("uvg%d" % i, [128, 2 * D], BF16) for i in range(NG)]
        dg = [sb("dg%d" % i, [128, 128], BF16) for i in range(NG)]
        gel = sb("gel", [128, 128]); gelc = [Buf() for _ in range(128)]
        gateb = sb("gateb", [128, 128]); h2bb = sb("h2bb", [128, D])
        qT = sb("qT", [128, 16, 128], BF16)
    ot = sb("ot", [128, 512]); tm = sb("tm5", [128, 512]); vt5 = sb("vt5", [128, 512])
    st8 = sb("st8", [128, 64])
    ybf = sb("ybf", [128, 512], BF16); yT = sb("yT5", [128, 4, 128], BF16); ypT = sb("ypT", [128, 4, 128], BF16)
    xt5 = sb("xt5", [128, D]); x1 = sb("x1", [128, D]); h2 = sb("h2", [128, D]); h2b = sb("h2b", [128, D], BF16)
    h2T = sb("h2T", [128, 8, 128], BF16)
    ssq5 = sb("ssq5", [128, 8])
    junk2 = junk if peer else sb("junk2", [128, D], BF16)
    P2 = T(pall.t[:, 0:2, :], "P2"); P2.b.excl = True
    P4 = T(pall.t[:, 4:6, :], "P4"); P4.b.excl = True
    PV = T(pall.t[:, 6:8, :], "PV"); PV.b.excl = True
    b2, b3 = banks[2], banks[3]

    def rms(src, dst_f32, wbt, k0):
        S.op("act", "activation", reads=[src], writes=[junk2, ssq5], out=junk2[:], in_=src[:], func=AF.Square,
             accum_out=ssq5[:, k0:k0 + 1])
        S.op("act", "activation", reads=[ssq5], writes=[ssq5], out=ssq5[:, k0 + 1:k0 + 2], in_=ssq5[:, k0:k0 + 1], func=AF.Sqrt,
             scale=1.0 / D, bias=epsc[:, 0:1])
        S.op("dve", "reciprocal", reads=[ssq5], writes=[ssq5], out=ssq5[:, k0 + 2:k0 + 3], in_=ssq5[:, k0 + 1:k0 + 2])
        S.op("dve", "scalar_tensor_tensor", reads=[src, ssq5, wbt], writes=[dst_f32], out=dst_f32[:], in0=src[:],
             scalar=ssq5[:, k0 + 2:k0 + 3], in1=wbt[:], op0=ALU.mult, op1=ALU.mult)

    x1s = [x1, sb("x1b", [128, D])]
    h2s = [h2, h2bb] if peer else [h2, h2]
    gates = [gate, gateb] if peer else None
    eids = [eid, sb("eidb", [128, 128], U32)] if peer else None

    def partA(i):
        tsl = slice(i * 128, (i + 1) * 128)
        x1 = x1s[i % 2]
        eid = eids[i % 2] if peer else None
        h2 = h2s[i % 2]
        gate = gates[i % 2] if peer else None
        S.dma("sp", ot[:], OB[tsl, :], writes=[ot])
        S.dma("act", vt5[:], VTOK[tsl, :], writes=[vt5])
        S.dma("sp", ypT[:], YP[:, tsl].rearrange("(g p) t -> p g t", p=128), writes=[ypT])
        S.dma("act", xt5[:], x[tsl, :], writes=[xt5])
        o3 = ot[:].rearrange("p (h n) -> p h n", h=8)
        t3 = tm[:].rearrange("p (h n) -> p h n", h=8)
        S.op("dve", "tensor_reduce", reads=[ot], writes=[st8], out=st8[:, 0:8], in_=o3, axis=AX.X, op=ALU.add)
        S.op("pool", "tensor_tensor", reads=[ot], writes=[tm], out=tm[:], in0=ot[:], in1=ot[:], op=ALU.mult)
        S.op("dve", "tensor_reduce", reads=[tm], writes=[st8], out=st8[:, 8:16], in_=t3, axis=AX.X, op=ALU.add)
        S.op("dve", "tensor_scalar", reads=[st8], writes=[st8], out=st8[:, 16:24], in0=st8[:, 0:8], scalar1=1.0 / 64, scalar2=None, op0=ALU.mult)
        S.op("dve", "tensor_tensor", reads=[st8], writes=[st8], out=st8[:, 24:32], in0=st8[:, 16:24], in1=st8[:, 16:24], op=ALU.mult)
        S.op("dve", "scalar_tensor_tensor", reads=[st8], writes=[st8], out=st8[:, 32:40], in0=st8[:, 8:16], scalar=1.0 / 64, in1=st8[:, 24:32],
             op0=ALU.mult, op1=ALU.subtract)
        S.op("act", "activation", reads=[st8], writes=[st8], out=st8[:, 40:48], in_=st8[:, 32:40], func=AF.Sqrt, bias=epsc[:, 1:2])
        S.op("dve", "reciprocal", reads=[st8], writes=[st8], out=st8[:, 48:56], in_=st8[:, 40:48])
        S.op("dve", "tensor_tensor", reads=[ot, st8], writes=[tm], out=t3, in0=o3, in1=st8[:, 16:24].unsqueeze(2).to_broadcast([128, 8, 64]),
             op=ALU.subtract)
        S.op("dve", "tensor_tensor", reads=[tm, st8], writes=[tm], out=t3, in0=t3, in1=st8[:, 48:56].unsqueeze(2).to_broadcast([128, 8, 64]),
             op=ALU.mult)
        S.op("pool", "tensor_tensor", reads=[tm, lnw_b], writes=[tm], out=tm[:], in0=tm[:], in1=lnw_b[:], op=ALU.mult)
        S.op("pool", "tensor_tensor", reads=[tm, lnb_b], writes=[tm], out=tm[:], in0=tm[:], in1=lnb_b[:], op=ALU.add)
        S.op("dve", "tensor_tensor", reads=[vt5, coef], writes=[vt5], out=vt5[:].rearrange("p (h n) -> p h n", h=8),
             in0=vt5[:].rearrange("p (h n) -> p h n", h=8), in1=coef[:, i * 8:(i + 1) * 8].unsqueeze(2).to_broadcast([128, 8, 64]), op=ALU.mult)
        S.op("dve", "tensor_tensor", reads=[tm, vt5], writes=[tm], out=tm[:], in0=tm[:], in1=vt5[:], op=ALU.add)
        S.op("pe", "matmul", reads=[sgA, gupA], writes=[b2], out=b2[:, :], lhsT=sgA[:, tsl], rhs=gupA[:], start=True, stop=False)
        S.op("pe", "matmul", reads=[sgB, gupB], writes=[b2], out=b2[:, :], lhsT=sgB[:, tsl], rhs=gupB[:], start=False, stop=True)
        S.op("dve", "tensor_tensor", reads=[tm, b2], writes=[ybf], out=ybf[:], in0=tm[:], in1=b2[:, :], op=ALU.mult)
        b3b = b3.t.bitcast(BF16)
        for q in range(4):
            S.op("pe", "transpose", reads=[ybf, identb], writes=[b3], out=b3b[:, q * 128:(q + 1) * 128], in_=ybf[:, q * 128:(q + 1) * 128],
                 identity=identb[:])
        S.op("act", "activation", reads=[b3], writes=[yT], out=yT[:], in_=b3b[:, 0:512].rearrange("p (q t) -> p q t", q=4), func=AF.Copy)
        for hf in range(2):
            for kc in range(8):
                lt = yT[:, kc, :] if kc < 4 else ypT[:, kc - 4, :]
                S.op("pe", "matmul", reads=[yT, ypT, woutb], writes=[P2], out=P2[:, hf, :], lhsT=lt, rhs=woutb[:, kc, hf * 512:(hf + 1) * 512],
                     start=(kc == 0), stop=(kc == 7))
        S.op("dve", "tensor_tensor", reads=[P2, xt5], writes=[x1], out=x1[:].rearrange("p (a n) -> p a n", a=2), in0=P2[:, :, :],
             in1=xt5[:].rearrange("p (a n) -> p a n", a=2), op=ALU.add)
        if "X1" in debug:
            S.dma("sp", X1[tsl, :], x1[:], reads=[x1])
        if not peer:
            rms(x1, h2, wfb, 0)
            S.dma("sp", out[tsl, :], h2[:], reads=[h2])
            return
        rms(x1, h2, wb2, 0)
        S.op("pool", "tensor_copy", reads=[h2], writes=[h2b], out=h2b[:], in_=h2[:])
        for kc in range(8):
            S.op("pe", "transpose", reads=[h2b, identb], writes=[b3], out=b3b[:, kc * 128:(kc + 1) * 128], in_=h2b[:, kc * 128:(kc + 1) * 128],
                 identity=identb[:])
        S.op("act", "activation", reads=[b3], writes=[h2T], out=h2T[:], in_=b3b[:, :].rearrange("p (k t) -> p k t", k=8), func=AF.Copy)
        for c4 in range(4):
            bk = b2 if c4 % 2 == 0 else b3
            for cq in range(4):
                ch = c4 * 4 + cq
                for kc in range(8):
                    S.op("pe", "matmul", reads=[wqb, h2T], writes=[bk], out=bk[:, cq * 128:(cq + 1) * 128], lhsT=wqb[:, kc, ch * 128:(ch + 1) * 128],
                         rhs=h2T[:, kc, :], start=(kc == 0), stop=(kc == 7))
            S.op("act" if c4 % 2 == 0 else "dve", "activation" if c4 % 2 == 0 else "tensor_copy", reads=[bk], writes=[qT],
                 out=qT[:, c4 * 4:(c4 + 1) * 4, :], in_=bk[:, :].rearrange("p (c t) -> p c t", c=4), **({"func": AF.Copy} if c4 % 2 == 0 else {}))
        for half in range(2):
            for c8 in range(8):
                ch = half * 8 + c8
                S.op("pe", "matmul", reads=[qT, keyb], writes=[P4], out=P4[:, c8 // 4, (c8 % 4) * 128:(c8 % 4 + 1) * 128], lhsT=qT[:, ch, :],
                     rhs=keyb[:, ch, :], start=True, stop=True)
            S.op("dve", "tensor_copy", reads=[P4], writes=[stg], out=stg[:, half * 1024:(half + 1) * 1024].rearrange("p (a n) -> p a n", a=2),
                 in_=P4[:, :, :])
        scv = stg[:].rearrange("p (c n) -> p c n", c=16)
        sc2v = sc2[:].rearrange("p (c n) -> p c n", c=16)
        m1v = m1[:].rearrange("p (c n) -> p c n", c=16)
        i1v = i1[:].rearrange("p (c n) -> p c n", c=16)
        for ch in range(16):
            S.op("dve", "max", reads=[stg], writes=[m1c[ch]], out=m1v[:, ch, 0:8], in_=scv[:, ch, :])
        for ch in range(16):
            S.op("dve", "max_index", reads=[stg, m1c[ch]], writes=[i1c[ch]], out=i1v[:, ch, 0:8], in_max=m1v[:, ch, 0:8], in_values=scv[:, ch, :])
        for ch in range(16):
            S.op("dve", "match_replace", reads=[stg, m1c[ch]], writes=[sc2c[ch]], out=sc2v[:, ch, :], in_to_replace=m1v[:, ch, 0:8],
                 in_values=scv[:, ch, :], imm_value=-1e30)
        for ch in range(16):
            S.op("dve", "max", reads=[sc2c[ch]], writes=[m1c[ch]], out=m1v[:, ch, 8:16], in_=sc2v[:, ch, :])
        for ch in range(16):
            S.op("dve", "max_index", reads=[sc2c[ch], m1c[ch]], writes=[i1c[ch]], out=i1v[:, ch, 8:16], in_max=m1v[:, ch, 8:16],
                 in_values=sc2v[:, ch, :])
        S.op("dve", "tensor_copy", reads=i1c, writes=[idxf], out=idxf[:], in_=i1[:])
        S.op("dve", "tensor_scalar", reads=[idxf], writes=[idx128], out=idx128[:], in0=idxf[:], scalar1=128.0, scalar2=None, op0=ALU.mult)
        m4 = m1[:].rearrange("p (h a n) -> p h a n", h=8, a=2)
        x4 = idxf[:].rearrange("p (h a n) -> p h a n", h=8, a=2)
        y4 = idx128[:].rearrange("p (h a n) -> p h a n", h=8, a=2)
        c4v = cand[:].rearrange("p (h i j) -> p h i j", h=8, i=16)
        S.op("dve", "tensor_tensor", reads=m1c, writes=[cand], out=c4v, in0=m4[:, :, 0, :].unsqueeze(3).to_broadcast([128, 8, 16, 16]),
             in1=m4[:, :, 1, :].unsqueeze(2).to_broadcast([128, 8, 16, 16]), op=ALU.add)
        cv = cand[:].rearrange("p (h n) -> p h n", h=8)
        c2v = sc2[:].rearrange("p (h n) -> p h n", h=8)
        s16 = sc16[:].rearrange("p (h n) -> p h n", h=8)
        p16 = pos[:].rearrange("p (h n) -> p h n", h=8)
        for hh in range(8):
            S.op("dve", "max", reads=[cand], writes=[s16c[hh]], out=s16[:, hh, 0:8], in_=cv[:, hh, :])
        for hh in range(8):
            S.op("dve", "max_index", reads=[cand, s16c[hh]], writes=[posc[hh]], out=p16[:, hh, 0:8], in_max=s16[:, hh, 0:8], in_values=cv[:, hh, :])
        for hh in range(8):
            S.op("dve", "match_replace", reads=[cand, s16c[hh]], writes=[sc2c[2 * hh], sc2c[2 * hh + 1]], out=c2v[:, hh, :],
                 in_to_replace=s16[:, hh, 0:8], in_values=cv[:, hh, :], imm_value=-1e30)
        for hh in range(8):
            S.op("dve", "max", reads=[sc2c[2 * hh], sc2c[2 * hh + 1]], writes=[s16c[hh]], out=s16[:, hh, 8:16], in_=c2v[:, hh, :])
        for hh in range(8):
            S.op("dve", "max_index", reads=[sc2c[2 * hh], sc2c[2 * hh + 1], s16c[hh]], writes=[posc[hh]], out=p16[:, hh, 8:16],
                 in_max=s16[:, hh, 8:16], in_values=c2v[:, hh, :])
        S.op("dve", "tensor_scalar", reads=posc, writes=[r1u], out=r1u[:], in0=pos[:], scalar1=4, scalar2=None, op0=ALU.logical_shift_right)
        S.op("dve", "tensor_scalar", reads=posc, writes=[r2u], out=r2u[:], in0=pos[:], scalar1=15, scalar2=None, op0=ALU.bitwise_and)
        S.op("dve", "tensor_copy", reads=[r1u], writes=[posf], out=posf[:], in_=r1u[:])
        S.op("dve", "tensor_copy", reads=[r2u], writes=[r2f], out=r2f[:], in_=r2u[:])
        io4 = iota_f[:, 0:16].unsqueeze(1).unsqueeze(1).to_broadcast([128, 8, 16, 16])
        for (rf_, src4, dstv) in ((posf, y4[:, :, 0, :], e1v), (r2f, x4[:, :, 1, :], e2v)):
            S.op("dve", "tensor_tensor", reads=[iota_f, rf_], writes=[cand], out=c4v, in0=io4,
                 in1=rf_[:].rearrange("p (h j) -> p h j", h=8).unsqueeze(3).to_broadcast([128, 8, 16, 16]), op=ALU.is_equal)
            S.op("dve", "tensor_tensor", reads=[cand, idxf, idx128], writes=[cand], out=c4v, in0=c4v,
                 in1=src4.unsqueeze(2).to_broadcast([128, 8, 16, 16]), op=ALU.mult)
            S.op("dve", "tensor_reduce", reads=[cand], writes=[dstv], out=dstv[:], in_=cand[:].rearrange("p (q i) -> p q i", i=16), axis=AX.X, op=ALU.add)
        S.op("dve", "tensor_tensor", reads=[e1v, e2v], writes=eidc, out=eidf[:], in0=e1v[:], in1=e2v[:], op=ALU.add)
        S.op("dve", "tensor_scalar", reads=eidc, writes=eidc, out=eidf[:], in0=eidf[:], scalar1=0.0, scalar2=16383.0, op0=ALU.max, op1=ALU.min)
        S.op("dve", "tensor_copy", reads=eidc, writes=[eid], out=eid[:], in_=eidf[:])
        g3 = gate[:].rearrange("p (h n) -> p h n", h=8)
        S.op("dve", "tensor_tensor", reads=s16c, writes=[gate], out=g3, in0=s16, in1=s16[:, :, 0:1].to_broadcast([128, 8, 16]), op=ALU.subtract)
        S.op("act", "activation", reads=[gate], writes=[gate], out=gate[:], in_=gate[:], func=AF.Exp)
        S.op("dve", "tensor_reduce", reads=[gate], writes=[gsm], out=gsm[:, 0:8], in_=g3, axis=AX.X, op=ALU.add)
        S.op("dve", "reciprocal", reads=[gsm], writes=[gsm], out=gsm[:, 8:16], in_=gsm[:, 0:8])
        S.op("dve", "tensor_tensor", reads=[gate, gsm], writes=[gate], out=g3, in0=g3, in1=gsm[:, 8:16].unsqueeze(2).to_broadcast([128, 8, 16]),
             op=ALU.mult)

    LAG = 4

    def partUV(i, interleave):
        tsl = slice(i * 128, (i + 1) * 128)
        eid = eids[i % 2]
        h2 = h2s[i % 2]
        gate = gates[i % 2]
        per = (len(S.pending) + 127) // 128 if interleave else 0

        def tail(hj):
            uv_ = uvg[hj % NG]
            d_ = dg[hj % NG]
            S.op("pool", "tensor_scalar", reads=[identb, gelc[hj], gate], writes=[d_], out=d_[:], in0=identb[:], scalar1=gel[:, hj:hj + 1],
                 scalar2=gate[:, hj:hj + 1], op0=ALU.mult, op1=ALU.mult)
            for hf in range(2):
                S.op("pe", "matmul", reads=[d_, uv_], writes=[PV], out=PV[:, hf, :], lhsT=d_[:], rhs=uv_[:, D + hf * 512:D + (hf + 1) * 512],
                     start=(hj == 0), stop=(hj == 127))

        for hj in range(128):
            uv_ = uvg[hj % NG]
            S.dma("pool", uv_[:], UVB[:, :], reads=[eid], writes=[uv_], indirect=bass.IndirectOffsetOnAxis(ap=eid[:, hj:hj + 1], axis=0))
            S.op("dve", "scalar_tensor_tensor", reads=[uv_, h2], writes=[aactc[hj], accb], out=junk[:], in0=uv_[:, 0:D], scalar=1.0, in1=h2[:],
                 op0=ALU.mult, op1=ALU.mult, accum_out=aact[:, hj:hj + 1])
            S.op("act", "activation", reads=[aactc[hj]], writes=[gelc[hj]], out=gel[:, hj:hj + 1], in_=aact[:, hj:hj + 1], func=AF.Gelu)
            if hj >= LAG:
                tail(hj - LAG)
            if per:
                S.flush(per)
        for hj in range(128 - LAG, 128):
            tail(hj)
        S.flush()

    def partF(i):
        tsl = slice(i * 128, (i + 1) * 128)
        x1 = x1s[i % 2]
        S.op("dve", "tensor_tensor", reads=[PV, x1], writes=[x1], out=x1[:].rearrange("p (a n) -> p a n", a=2), in0=PV[:, :, :],
             in1=x1[:].rearrange("p (a n) -> p a n", a=2), op=ALU.add)
        rms(x1, x1, wfb, 4)
        S.dma("sp", out[tsl, :], x1[:], reads=[x1])

    if not peer:
        for i in range(NT):
            partA(i)
    else:
        partA(0)
        for i in range(NT):
            if i + 1 < NT:
                S.rec = True
                partA(i + 1)
                S.rec = False
            partUV(i, True)
            partF(i)

    S.barrier()
    print("ops", S.nops, "waits", S.nwait, "dmas", S.dcount)
    return nc


def make_consts():
    idx = np.arange(128)
    ident = np.eye(128, dtype=np.float32)
    mus = (idx[None, :] > idx[:, None]).astype(np.float32)
    mui = (idx[None, :] >= idx[:, None]).astype(np.float32)
    mls = (idx[None, :] < idx[:, None]).astype(np.float32)
    mli = (idx[None, :] <= idx[:, None]).astype(np.float32)
    masks = np.stack([mus, mui, mls, mli], axis=1)
    bones = (idx[None, :] // 64 == idx[:, None] // 64).astype(np.float32)
    sel = (idx[:, None] // 64 == np.arange(2)[None, :]).astype(np.float32)
    reset = np.ones((128, 1024), np.float32)
    reset[:, ::128] = 0.0
    invcnt = np.zeros((128, 4, 16), np.float32)
    for gi, win in enumerate((2, 4, 8, 16)):
        half = win // 2
        for t in range(half):
            invcnt[:, gi, t] = 1.0 / (t + half)
        for q in range(half - 1):
            t = S_LEN - (half - 1) + q
            invcnt[:, gi, 8 + q] = 1.0 / (S_LEN - t + half)
    return dict(c_ident=ident, c_masks=masks, c_bones=bones, c_sel=sel, c_reset=reset, c_invcnt=invcnt)


def make_in_maps(inp, peer=True):
    f = lambda a: np.ascontiguousarray(np.asarray(a, dtype=np.float32))
    shared = dict(
        w_in=f(inp["w_in"][0].reshape(8, 128, D_IN).transpose(1, 0, 2)),
        shift_mu=f(inp["shift_mu"][0]), norm1_w=f(inp["norm1_w"][0]),
        w0=f(inp["w0"][0]), a0=f(inp["a0"][0]),
        w_up=f(inp["w_up"][0].reshape(128, 512)), a_up=f(inp["a_up"][0].reshape(128, 512)),
        g_up=f(inp["g_up"][0]), k_k=f(inp["k_k"][0]), k_a=f(inp["k_a"][0]), r_k=f(inp["r_k"][0].reshape(512)),
        ln_x_w=f(inp["ln_x_w"][0]), ln_x_b=f(inp["ln_x_b"][0]),
        pool_w=f(inp["pool_w"][0].transpose(1, 0, 2)), pool_scale=f(inp["pool_scale"][0]),
        w_out=f(inp["w_out"][0].reshape(8, 128, D).transpose(1, 0, 2)),
        norm2_w=f(inp["norm2_w"][0]), norm_f_w=f(inp["norm_f_w"]),
        wq=f(inp["peer_wq"][0].reshape(8, 128, 2048).transpose(1, 0, 2)),
        keysT=f(inp["peer_keys"][0].transpose(3, 0, 1, 2).reshape(128, 16, 128)),
        peer_u=f(inp["peer_u"][0]), peer_v=f(inp["peer_v"][0]),
    )
    shared.update(make_consts())
    if not peer:
        del shared["peer_u"], shared["peer_v"]
    xs = np.asarray(inp["x"], dtype=np.float32)
    return [dict(shared, x=np.ascontiguousarray(xs[c])) for c in range(8)]


def kernel(**inputs):
    nc = build()
    in_maps = make_in_maps(inputs)
    res = run_bass_kernel_spmd(nc, in_maps, core_ids=list(range(8)))
    return np.stack([np.asarray(r["out"], dtype=np.float32) for r in res.results], axis=0)
```

```python
import numpy as np
import ml_dtypes
import concourse.bass as bass
import concourse.mybir as mybir
from concourse.bass_utils import run_bass_kernel_spmd

F32 = mybir.dt.float32
BF16 = mybir.dt.bfloat16
I32 = mybir.dt.int32
U32 = mybir.dt.uint32
AF = mybir.ActivationFunctionType
ALU = mybir.AluOpType
AX = mybir.AxisListType

S_LEN = 4096
D = 1024
NT = S_LEN // 128
RWKV_IN = 1952
D_IN = 2464
C0 = float(np.exp(-0.5))
RMS_EPS = 1e-5
GN_EPS = 64e-5
PAD = 8


class Buf:
    __slots__ = ("name", "w", "r", "excl")

    def __init__(self, name=""):
        self.name = name
        self.w = None
        self.r = {}
        self.excl = False


class T:
    def __init__(self, t, name=""):
        self.t = t
        self.b = Buf(name)

    def __getitem__(self, k):
        return self.t[k]


def _b(x):
    return x.b if isinstance(x, T) else x


class Sched:
    def __init__(self, nc, ndma=32):
        self.nc = nc
        self.es = {"pe": nc.tensor, "act": nc.scalar, "dve": nc.vector, "pool": nc.gpsimd, "sp": nc.sync}
        self.sem = {}
        self.tick = {k: 0 for k in self.es}
        self.seen = {k: {} for k in self.es}
        self._ctx = []
        for k in self.es:
            cm = nc.semaphore("s_" + k)
            self.sem[k] = cm.__enter__()
            self._ctx.append(cm)
        self.ndma = ndma
        self.dsem = []
        for i in range(ndma):
            cm = nc.semaphore("d_%d" % i)
            self.dsem.append(cm.__enter__())
            self._ctx.append(cm)
        self.dcount = 0
        self.rec = False
        self.pending = []
        self.nwait = 0
        self.nops = 0

    def _need(self, e, deps):
        best = {}
        for d in deps:
            if d is None:
                continue
            key, val = d
            if key == e and e == "pe":
                continue
            if best.get(key, 0) < val:
                best[key] = val
        for key, val in best.items():
            if self.seen[e].get(key, 0) >= val:
                continue
            sem = self.sem[key] if isinstance(key, str) else self.dsem[key]
            self.es[e].wait_ge(sem, val)
            self.nwait += 1
            self.seen[e][key] = val

    def _deps(self, reads, writes):
        deps = []
        for b in reads:
            deps.append(b.w)
        for b in writes:
            deps.append(b.w)
            for k, v in b.r.items():
                deps.append((k, v))
        return deps

    def flush(self, n=None):
        pend = self.pending
        k = len(pend) if n is None else min(n, len(pend))
        todo, self.pending = pend[:k], pend[k:]
        rec, self.rec = self.rec, False
        for kind, a, kw in todo:
            if kind == "op":
                self.op(*a, **kw)
            else:
                self.dma(*a, **kw)
        self.rec = rec

    def op(self, e, name, reads=(), writes=(), **kw):
        if self.rec:
            self.pending.append(("op", (e, name, reads, writes), kw))
            return None
        reads = [_b(x) for x in reads]
        writes = [_b(x) for x in writes]
        writes = writes + [b for b in reads if b.excl and b not in writes]
        reads = [b for b in reads if not b.excl]
        self._need(e, self._deps(reads, writes))
        ins = getattr(self.es[e], name)(**kw)
        self.tick[e] += 1
        self.nops += 1
        ins.then_inc(self.sem[e], 1)
        tk = self.tick[e]
        for b in reads:
            b.r[e] = tk
        for b in writes:
            b.w = (e, tk)
            b.r = {}
        return ins

    def dma(self, e, out, in_, reads=(), writes=(), indirect=None, **kw):
        if self.rec:
            self.pending.append(("dma", (e, out, in_, reads, writes, indirect), kw))
            return None
        reads = [_b(x) for x in reads]
        writes = [_b(x) for x in writes]
        j = self.dcount
        self.dcount += 1
        slot = j % self.ndma
        rnd = j // self.ndma
        deps = self._deps(reads, writes)
        if rnd > 0:
            deps.append((slot, 16 * rnd))
        self._need(e, deps)
        if indirect is None:
            ins = self.es[e].dma_start(out=out, in_=in_, **kw)
        else:
            ins = self.es[e].indirect_dma_start(out=out, out_offset=None, in_=in_, in_offset=indirect, **kw)
        ins.then_inc(self.dsem[slot], 16)
        self.nops += 1
        val = 16 * (rnd + 1)
        for b in reads:
            b.r[slot] = val
        for b in writes:
            b.w = (slot, val)
            b.r = {}
        return ins

    def barrier(self):
        deps = [(k, self.tick[k]) for k in self.es if self.tick[k] > 0]
        for j in range(min(self.dcount, self.ndma)):
            cnt = (self.dcount - 1 - j) // self.ndma + 1
            deps.append((j, 16 * cnt))
        for e in self.es:
            self._need(e, deps)


def build(debug=(), peer=True, cut=99, nblk=99):
    nc = bass.Bass("TRN2", target_bir_lowering=False)
    S = Sched(nc)
    guards = []

    def din(name, shape, dt=F32):
        return nc.dram_tensor(name, list(shape), dt, kind="ExternalInput").ap()

    def dscr(name, shape, dt=F32):
        kind = "ExternalOutput" if name in debug else "Internal"
        return nc.dram_tensor(name, list(shape), dt, kind=kind).ap()

    def sb(name, shape, dt=F32):
        g = nc.sbuf_tensor(name, list(shape), dt)
        t = g.__enter__()
        guards.append(g)
        return T(t, name)

    def ps(name, shape, dt=F32):
        g = nc.psum_tensor(name, list(shape), dt)
        t = g.__enter__()
        guards.append(g)
        return T(t, name)

    def release(n):
        for _ in range(n):
            g = guards.pop()
            g.__exit__(None, None, None)

    x = din("x", [S_LEN, D])
    w_in = din("w_in", [128, 8, D_IN])
    shift_mu = din("shift_mu", [2, RWKV_IN])
    norm1_w = din("norm1_w", [D])
    w0 = din("w0", [2, 512]); a0 = din("a0", [2, 512])
    w_up = din("w_up", [128, 512]); a_up = din("a_up", [128, 512])
    g_up = din("g_up", [160, 512])
    k_k = din("k_k", [512]); k_a = din("k_a", [512]); r_k = din("r_k", [512])
    ln_x_w = din("ln_x_w", [512]); ln_x_b = din("ln_x_b", [512])
    pool_w = din("pool_w", [128, 4, 128]); pool_scale = din("pool_scale", [512])
    w_out = din("w_out", [128, 8, D])
    norm2_w = din("norm2_w", [D]); norm_f_w = din("norm_f_w", [D])
    wq = din("wq", [128, 8, 2048])
    keysT = din("keysT", [128, 16, 128])
    if peer:
        peer_u = din("peer_u", [16384, D]); peer_v = din("peer_v", [16384, D])
    c_ident = din("c_ident", [128, 128])
    c_masks = din("c_masks", [128, 4, 128])
    c_bones = din("c_bones", [128, 128])
    c_sel = din("c_sel", [128, 2])
    c_reset = din("c_reset", [128, 1024])
    c_invcnt = din("c_invcnt", [128, 4, 16])
    out = nc.dram_tensor("out", [S_LEN, D], F32, kind="ExternalOutput").ap()

    RT = dscr("RT", [512, S_LEN]); KT = dscr("KT", [512, S_LEN])
    VTOK = dscr("VTOK", [S_LEN, 512])
    YP = dscr("YP", [512, S_LEN], BF16)

    identf = sb("identf", [128, 128]); identb = sb("identb", [128, 128], BF16)
    S.dma("sp", identf[:], c_ident[:, :], writes=[identf])
    S.op("dve", "tensor_copy", reads=[identf], writes=[identb], out=identb[:], in_=identf[:])
    epsc = sb("epsc", [128, 2])
    S.op("pool", "memset", writes=[epsc], ap=epsc[:, 0:1], constant=RMS_EPS)
    S.op("pool", "memset", writes=[epsc], ap=epsc[:, 1:2], constant=GN_EPS)
    NPC = 16 + 16 + 16 + 8 + 8 + 4 + 4 + 4 + 4 + 4
    pc = sb("pc", [128, NPC])
    S.op("pool", "memset", writes=[pc], ap=pc[:], constant=0.0)
    col = {}
    o = 0

    def ldcol(name, vec, n):
        nonlocal o
        col[name] = o
        nfull = n // 128
        if nfull:
            S.dma("sp", pc[:, o:o + nfull], vec[0:nfull * 128].rearrange("(c p) -> p c", p=128), writes=[pc],
                  allow_slow_non_contiguous=True)
        rem = n - nfull * 128
        if rem:
            S.dma("sp", pc[0:rem, o + nfull:o + nfull + 1], vec[nfull * 128:n].rearrange("(c p) -> p c", p=rem),
                  writes=[pc], allow_slow_non_contiguous=True)
        o += (n + 127) // 128

    ldcol("mu0", shift_mu[0, :], RWKV_IN); ldcol("mu1", shift_mu[1, :], RWKV_IN)
    col["muc"] = o; o += 16
    ldcol("w0_0", w0[0, :], 512); ldcol("w0_1", w0[1, :], 512)
    ldcol("a0_0", a0[0, :], 512); ldcol("a0_1", a0[1, :], 512)
    ldcol("k_k", k_k, 512); ldcol("k_a", k_a, 512); ldcol("r_k", r_k, 512); ldcol("pscale", pool_scale, 512)
    col["omka"] = o; o += 4
    assert o == NPC, (o, NPC)
    S.op("dve", "tensor_tensor", reads=[pc], writes=[pc], out=pc[:, col["muc"]:col["muc"] + 16],
         in0=pc[:, col["mu0"]:col["mu0"] + 16], in1=pc[:, col["mu1"]:col["mu1"] + 16], op=ALU.add)
    S.op("dve", "tensor_scalar", reads=[pc], writes=[pc], out=pc[:, col["muc"]:col["muc"] + 16],
         in0=pc[:, col["muc"]:col["muc"] + 16], scalar1=-1.0, scalar2=1.0, op0=ALU.mult, op1=ALU.add)
    S.op("dve", "tensor_scalar", reads=[pc], writes=[pc], out=pc[:, col["omka"]:col["omka"] + 4],
         in0=pc[:, col["k_a"]:col["k_a"] + 4], scalar1=-1.0, scalar2=1.0, op0=ALU.mult, op1=ALU.add)

    twd = sb("twd", [128, S_LEN], BF16)
    adT = sb("adT", [128, S_LEN], BF16)
    sgA = sb("sgA", [128, S_LEN], BF16)
    sgB = sb("sgB", [32, S_LEN], BF16)

    pall = ps("pall", [128, 8, 512])
    banks = [T(pall.t[:, i, :], "bank%d" % i) for i in range(8)]
    for bk_ in banks:
        bk_.b.excl = True

    n_ph = len(guards)
    hT = sb("hT", [128, 8, S_LEN], BF16)
    pbuf = [sb("pbuf%d" % i, [128, S_LEN + 2 * PAD]) for i in range(2)]
    for pb in pbuf:
        S.op("pool", "memset", writes=[pb], ap=pb[:, 0:PAD], constant=0.0)
        S.op("pool", "memset", writes=[pb], ap=pb[:, PAD + S_LEN:], constant=0.0)
    tA = sb("tA", [128, S_LEN + 2 * PAD]); tB = sb("tB", [128, S_LEN + 2 * PAD])
    wb1 = sb("nw1b", [128, D])
    S.dma("sp", wb1[:], norm1_w.partition_broadcast(128), writes=[wb1])
    ssq = sb("ssq", [128, 4])
    hn = [sb("hn%d" % i, [128, D], BF16) for i in range(2)]
    wf = [sb("wf%d" % i, [128, 8, 128]) for i in range(2)]
    wb = [sb("wb%d" % i, [128, 8, 128], BF16) for i in range(2)]

    xts = [tA, tB]
    ptb = [T(banks[i].t[:].bitcast(BF16), "ptb%d" % i) for i in range(2)]
    for pt_, bk in zip(ptb, banks[:2]):
        pt_.b = bk.b
    for i in range(NT):
        xt = xts[i % 2]
        S.dma("sp" if i % 2 == 0 else "act", xt[:, 0:D], x[i * 128:(i + 1) * 128, :], writes=[xt])
        h_ = hn[i % 2]
        S.op("act", "activation", reads=[xt], writes=[h_, ssq], out=h_[:], in_=xt[:, 0:D], func=AF.Square,
             accum_out=ssq[:, 0:1])
        S.op("act", "activation", reads=[ssq], writes=[ssq], out=ssq[:, 1:2], in_=ssq[:, 0:1], func=AF.Sqrt,
             scale=1.0 / D, bias=epsc[:, 0:1])
        S.op("dve", "reciprocal", reads=[ssq], writes=[ssq], out=ssq[:, 2:3], in_=ssq[:, 1:2])
        S.op("dve", "scalar_tensor_tensor", reads=[xt, ssq, wb1], writes=[h_], out=h_[:], in0=xt[:, 0:D],
             scalar=ssq[:, 2:3], in1=wb1[:], op0=ALU.mult, op1=ALU.mult)
        pt_ = ptb[i % 2]
        for kc in range(8):
            S.op("pe", "transpose", reads=[h_, identb], writes=[pt_], out=pt_.t[:, kc * 128:(kc + 1) * 128],
                 in_=h_[:, kc * 128:(kc + 1) * 128], identity=identb[:])
        S.op("act" if i % 2 == 0 else "dve", "activation" if i % 2 == 0 else "tensor_copy", reads=[pt_], writes=[hT],
             out=hT[:, :, i * 128:(i + 1) * 128], in_=pt_.t.rearrange("p (k t) -> p k t", k=8),
             **({"func": AF.Copy} if i % 2 == 0 else {}))

    chunks = []
    for i in range(4):
        chunks.append((i * 128, 128, "r", i))
    for i in range(4):
        chunks.append((512 + i * 128, 128, "k", i))
    for i in range(4):
        chunks.append((1024 + i * 128, 128, "v", i))
    chunks.append((1536, 128, "wd", 0))
    chunks.append((1664, 128, "ad", 0))
    chunks.append((1792, 128, "gdA", 0))
    chunks.append((1920, 32, "gdB", 0))
    for i in range(4):
        chunks.append((RWKV_IN + i * 128, 128, "pool", i))

    poolw_f = sb("poolw_f", [128, 4, 128]); poolw_b = sb("poolw_b", [128, 4, 128], BF16)
    S.dma("sp", poolw_f[:], pool_w[:, :, :], writes=[poolw_f])
    S.op("pool", "tensor_copy", reads=[poolw_f], writes=[poolw_b], out=poolw_b[:], in_=poolw_f[:])
    invcnt = sb("invcnt", [128, 4, 16])
    S.dma("sp", invcnt[:], c_invcnt[:, :, :], writes=[invcnt])
    plb = sb("plb", [128, S_LEN], BF16)
    ypb = sb("ypb", [128, S_LEN], BF16)
    vtk = [sb("vtk%d" % i, [128, 4, 128]) for i in range(2)]

    evac_i = 0
    for ci, (c0, w, kind, idx) in enumerate(chunks):
        wf_, wb_, pb = wf[ci % 2], wb[ci % 2], pbuf[ci % 2]
        S.dma("sp", wf_[:, :, 0:w], w_in[:, :, c0:c0 + w], writes=[wf_])
        S.op("pool", "tensor_copy", reads=[wf_], writes=[wb_], out=wb_[:, :, 0:w], in_=wf_[:, :, 0:w])
        for j in range(8):
            bk = banks[2 + (evac_i % 4)]
            for kc in range(8):
                S.op("pe", "matmul", reads=[wb_, hT], writes=[bk], out=bk[0:w, :], lhsT=wb_[:, kc, 0:w],
                     rhs=hT[:, kc, j * 512:(j + 1) * 512], start=(kc == 0), stop=(kc == 7))
            if evac_i % 2 == 0:
                S.op("act", "activation", reads=[bk], writes=[pb], out=pb[0:w, PAD + j * 512:PAD + (j + 1) * 512],
                     in_=bk[0:w, :], func=AF.Copy)
            else:
                S.op("dve", "tensor_copy", reads=[bk], writes=[pb], out=pb[0:w, PAD + j * 512:PAD + (j + 1) * 512],
                     in_=bk[0:w, :])
            evac_i += 1
        if kind != "pool":
            cc = c0 // 128
            m0 = pc[0:w, col["mu0"] + cc:col["mu0"] + cc + 1]
            m1 = pc[0:w, col["mu1"] + cc:col["mu1"] + cc + 1]
            mc = pc[0:w, col["muc"] + cc:col["muc"] + cc + 1]
            HS = S_LEN // 2
            for hf in range(2):
                lo = PAD + hf * HS
                S.op("act", "activation", reads=[pb, pc], writes=[tA], out=tA[0:w, lo:lo + HS],
                     in_=pb[0:w, lo - 1:lo - 1 + HS], func=AF.Copy, scale=m0)
                S.op("dve", "scalar_tensor_tensor", reads=[pb, pc, tA], writes=[tA], out=tA[0:w, lo:lo + HS],
                     in0=pb[0:w, lo + 1:lo + 1 + HS], scalar=m1, in1=tA[0:w, lo:lo + HS], op0=ALU.mult, op1=ALU.add)
                S.op("dve", "scalar_tensor_tensor", reads=[pb, pc, tA], writes=[tB], out=tB[0:w, lo:lo + HS],
                     in0=pb[0:w, lo:lo + HS], scalar=mc, in1=tA[0:w, lo:lo + HS], op0=ALU.mult, op1=ALU.add)
            res = tB
            R_ = res[0:w, PAD:PAD + S_LEN]
            if kind == "r":
                S.dma("sp", RT[idx * 128:(idx + 1) * 128, :], R_, reads=[res])
            elif kind == "k":
                S.dma("sp", KT[idx * 128:(idx + 1) * 128, :], R_, reads=[res])
            elif kind == "v":
                for g4 in range(8):
                    bk = banks[6 + g4 % 2]
                    for q in range(4):
                        tt = g4 * 4 + q
                        S.op("pe", "transpose", reads=[res, identf], writes=[bk], out=bk[:, q * 128:(q + 1) * 128],
                             in_=res[:, PAD + tt * 128:PAD + (tt + 1) * 128], identity=identf[:])
                    vt = vtk[g4 % 2]
                    S.op("act", "activation", reads=[bk], writes=[vt], out=vt[:],
                         in_=bk.t.rearrange("p (n c) -> p n c", n=4), func=AF.Copy)
                    S.dma("act", VTOK[g4 * 512:(g4 + 1) * 512, idx * 128:(idx + 1) * 128].rearrange("(n p) c -> p n c", p=128),
                          vt[:], reads=[vt])
            elif kind == "wd":
                S.op("act", "activation", reads=[res], writes=[twd], out=twd[:], in_=R_, func=AF.Tanh)
            elif kind == "ad":
                S.op("act", "activation", reads=[res], writes=[adT], out=adT[:], in_=R_, func=AF.Copy)
            elif kind == "gdA":
                S.op("act", "activation", reads=[res], writes=[sgA], out=sgA[:], in_=R_, func=AF.Sigmoid)
            elif kind == "gdB":
                S.op("act", "activation", reads=[res], writes=[sgB], out=sgB[:], in_=R_, func=AF.Sigmoid)
        else:
            gi = idx
            win = (2, 4, 8, 16)[gi]
            half = win // 2
            W_ = S_LEN + 2 * PAD
            S.op("dve", "tensor_tensor", reads=[pb], writes=[tA], out=tA[:, 1:W_], in0=pb[:, 0:W_ - 1], in1=pb[:, 1:W_],
                 op=ALU.add)
            cur, oth = tA, tB
            lo_, hi_ = 1, W_
            sh = 1
            for lev in range(gi):
                nlo, nhi = lo_ + sh, hi_ - sh
                S.op("dve" if lev % 2 else "pool", "tensor_tensor", reads=[cur], writes=[oth], out=oth[:, nlo:nhi],
                     in0=cur[:, nlo - sh:nhi - sh], in1=cur[:, nlo + sh:nhi + sh], op=ALU.add)
                cur, oth = oth, cur
                lo_, hi_ = nlo, nhi
                sh *= 2
            S.op("dve", "scalar_tensor_tensor", reads=[cur, pb], writes=[plb], out=plb[:], in0=cur[:, PAD:PAD + S_LEN],
                 scalar=1.0 / win, in1=pb[:, PAD:PAD + S_LEN], op0=ALU.mult, op1=ALU.subtract)
            S.op("dve", "tensor_tensor", reads=[cur, invcnt], writes=[oth], out=oth[:, 0:half], in0=cur[:, PAD:PAD + half],
                 in1=invcnt[:, gi, 0:half], op=ALU.mult)
            S.op("dve", "tensor_tensor", reads=[oth, pb], writes=[plb], out=plb[:, 0:half], in0=oth[:, 0:half],
                 in1=pb[:, PAD:PAD + half], op=ALU.subtract)
            if half > 1:
                nr = half - 1
                S.op("dve", "tensor_tensor", reads=[cur, invcnt], writes=[oth], out=oth[:, 8:8 + nr],
                     in0=cur[:, PAD + S_LEN - nr:PAD + S_LEN], in1=invcnt[:, gi, 8:8 + nr], op=ALU.mult)
                S.op("dve", "tensor_tensor", reads=[oth, pb], writes=[plb], out=plb[:, S_LEN - nr:S_LEN],
                     in0=oth[:, 8:8 + nr], in1=pb[:, PAD + S_LEN - nr:PAD + S_LEN], op=ALU.subtract)
            for j in range(8):
                bk = banks[2 + (evac_i % 4)]
                evac_i += 1
                S.op("pe", "matmul", reads=[poolw_b, plb], writes=[bk], out=bk[:, :], lhsT=poolw_b[:, gi, :],
                     rhs=plb[:, j * 512:(j + 1) * 512], start=True, stop=True)
                S.op("act", "activation", reads=[bk, pc], writes=[ypb], out=ypb[:, j * 512:(j + 1) * 512], in_=bk[:, :],
                     func=AF.Copy, scale=pc[:, col["pscale"] + gi:col["pscale"] + gi + 1])
            S.dma("sp", YP[gi * 128:(gi + 1) * 128, :], ypb[:], reads=[ypb])

    S.barrier()
    release(len(guards) - n_ph)

    OB = dscr("OB", [S_LEN, 512])
    n_ph3 = len(guards)
    obuf = sb("obuf", [128, NT, 512])
    coef = sb("coef", [128, NT, 8])
    S.op("pool", "memset", writes=[coef], ap=coef[:], constant=0.0)
    ctmp = sb("ctmp", [128, 4, 128])
    S.dma("sp", ctmp[:], c_masks[:, :, :], writes=[ctmp])
    mT2 = [sb("mT2_%d" % d, [128, 2, 256], BF16) for d in range(2)]
    mN2 = [sb("mN2_%d" % d, [128, 2, 128], BF16) for d in range(2)]
    for d in range(2):
        src = ctmp[:, 0:2, :] if d == 0 else ctmp[:, 2:4, :]
        nsrc = ctmp[:, 2, :] if d == 0 else ctmp[:, 0, :]
        for e in range(2):
            S.op("dve", "tensor_copy", reads=[ctmp], writes=[mT2[d]], out=mT2[d][:, e, :].rearrange("p (a t) -> p a t", a=2), in_=src)
            S.op("dve", "tensor_copy", reads=[ctmp], writes=[mN2[d]], out=mN2[d][:, e, :], in_=nsrc)
    ident2 = sb("ident2", [128, 2, 128], BF16)
    for e in range(2):
        S.op("dve", "tensor_copy", reads=[identf], writes=[ident2], out=ident2[:, e, :], in_=identf[:])
    ident2s = sb("ident2s", [128, 64])
    S.op("dve", "tensor_tensor", reads=[identf], writes=[ident2s], out=ident2s[:], in0=identf[:, 0:64], in1=identf[:, 64:128], op=ALU.add)
    ctmp2 = sb("ctmp2", [128, 130])
    S.dma("sp", ctmp2[:, 0:128], c_bones[:, :], writes=[ctmp2])
    S.dma("sp", ctmp2[:, 128:130], c_sel[:, :], writes=[ctmp2])
    bones = sb("bones", [128, 128], BF16); selb = sb("selb", [128, 2], BF16)
    S.op("dve", "tensor_copy", reads=[ctmp2], writes=[bones], out=bones[:], in_=ctmp2[:, 0:128])
    S.op("dve", "tensor_copy", reads=[ctmp2], writes=[selb], out=selb[:], in_=ctmp2[:, 128:130])
    BLK = 512
    NB = S_LEN // BLK
    CPB = BLK // 128
    reset = sb("reset", [128, BLK])
    S.dma("sp", reset[:], c_reset[:, 0:BLK], writes=[reset])
    upf = sb("upf", [128, 1024]); wupb = sb("wupb", [128, 512], BF16); aupb = sb("aupb", [128, 512], BF16)
    S.dma("sp", upf[:, 0:512], w_up[:, :], writes=[upf])
    S.dma("sp", upf[:, 512:1024], a_up[:, :], writes=[upf])
    S.op("dve", "tensor_copy", reads=[upf], writes=[wupb], out=wupb[:], in_=upf[:, 0:512])
    S.op("dve", "tensor_copy", reads=[upf], writes=[aupb], out=aupb[:], in_=upf[:, 512:1024])

    def f32t(n):
        return sb(n, [128, BLK])
    rF, kF, alpha, sg, cs, cum, t1, t2, t3, t4, t5 = [f32t("p3_%d" % i) for i in range(11)]
    vtF = sb("vtF", [128, CPB, 128])
    sqb = sb("sqb", [128, BLK], BF16); xbb = sb("xbb", [128, BLK], BF16)
    etot = sb("etot", [128, CPB])
    NPB = 2
    prep = []
    for i in range(NPB):
        prep.append(dict(
            ar=sb("ar%d" % i, [128, CPB, 2, 128], BF16), kt=sb("kt%d" % i, [128, BLK], BF16), bt=sb("bt%d" % i, [128, BLK], BF16),
            kh=sb("kh%d" % i, [128, BLK], BF16), bh=sb("bh%d" % i, [128, BLK], BF16),
            atk=sb("atk%d" % i, [128, CPB, 128], BF16), bhk=sb("bhk%d" % i, [128, CPB, 128], BF16),
            khk=sb("khk%d" % i, [128, CPB, 128], BF16), vbf=sb("vbf%d" % i, [128, CPB, 128], BF16),
            etot=sb("etot%d" % i, [128, CPB])))
    NSLOT = 4
    slots = []
    for i in range(NSLOT):
        sl = dict(
            Z=None,
            arb=sb("arb%d" % i, [128, 2, 128], BF16), s2b=sb("s2b%d" % i, [128, 2, 256], BF16),
            P=[sb("P%d_%d" % (i, j), [128, 2, 128], BF16) for j in range(2)],
            PTT=[sb("PTT%d_%d" % (i, j), [128, 2, 256], BF16) for j in range(2)],
            ysb=sb("ysb%d" % i, [128, 2, 64], BF16), w1m=sb("w1m%d" % i, [128, 2, 128], BF16),
            phiT=sb("phiT%d" % i, [128, 64]), xT=sb("xT%d" % i, [128, 128]))
        sl["Z"] = T(pall.t[:, 2 * i:2 * i + 2, :], "Z%d" % i)
        sl["Z"].b.excl = True
        slots.append(sl)
    Hs = [sb("H%d" % i, [128, 64]) for i in range(2)]
    pbk = []
    for k_ in range(2 * NSLOT):
        t_ = T(pall.t[:, k_, :], "pbk%d" % k_)
        t_.b = slots[k_ // 2]["Z"].b
        pbk.append(t_)
    NPBK = len(pbk)
    pbi = 0
    evc = [0]

    def evac_copy(out, in_, reads, writes):
        evc[0] += 1
        if evc[0] % 2 == 0:
            S.op("act", "activation", reads=reads, writes=writes, out=out, in_=in_, func=AF.Copy)
        else:
            S.op("dve", "tensor_copy", reads=reads, writes=writes, out=out, in_=in_)

    def evac2(out3, in3, reads, writes, eng=None):
        if eng is None:
            evc[0] += 1
            eng = "act" if evc[0] % 2 == 0 else "dve"
        if eng == "act":
            for e in range(2):
                S.op("act", "activation", reads=reads, writes=writes, out=out3[:, e, :], in_=in3[:, e, :], func=AF.Copy)
        else:
            S.op("dve", "tensor_copy", reads=reads, writes=writes, out=out3, in_=in3)

    def v3(ap_, c=CPB):
        return ap_.rearrange("p (c t) -> p c t", c=c)

    if peer:
        UVB = dscr("UVB", [16384, 2 * D], BF16)
        cvf = [sb("cvf%d" % i, [128, 2 * D]) for i in range(2)]
        cvb = [sb("cvb%d" % i, [128, 2 * D], BF16) for i in range(2)]
        S.rec = True
        ci_ = 0
        for tbl, dst in ((peer_u, UVB[:, 0:D]), (peer_v, UVB[:, D:2 * D])):
            for c in range(64):
                f_, b_ = cvf[ci_ % 2], cvb[ci_ % 2]
                rows = slice(c * 256, (c + 1) * 256)
                S.dma("sp", f_[:], tbl[rows, :].rearrange("(p r) d -> p (r d)", r=2), writes=[f_])
                S.op("pool", "tensor_copy", reads=[f_], writes=[b_], out=b_[:], in_=f_[:])
                S.dma("sp", dst[rows, :].rearrange("(p r) d -> p r d", r=2), b_[:].rearrange("p (r d) -> p r d", r=2), reads=[b_])
                ci_ += 1
        S.rec = False
        cv_pending = S.pending
        S.pending = []
    else:
        cv_pending = []

    def cv_flush(n):
        nonlocal cv_pending
        keep = S.pending
        S.pending = cv_pending
        S.flush(n)
        cv_pending = S.pending
        S.pending = keep

    for hp in range(4):
        hsl = slice(hp * 128, (hp + 1) * 128)
        for d in range(2):
            hcur = 0
            S.op("pool", "memset", writes=[Hs[0]], ap=Hs[0][:], constant=0.0)
            blks = range(NB) if d == 0 else range(NB - 1, -1, -1)
            for bi, blk in enumerate(blks):
                if hp * 16 + d * 8 + bi >= nblk:
                    continue
                cv_flush(6)
                tsl = slice(blk * BLK, (blk + 1) * BLK)
                pr = prep[bi % NPB]
                S.dma("sp", rF[:], RT[hsl, tsl], writes=[rF])
                S.dma("act", kF[:], KT[hsl, tsl], writes=[kF])
                S.dma("sp", vtF[:], VTOK[tsl, hsl].rearrange("(n p) c -> p n c", p=128), writes=[vtF])
                S.op("pool", "tensor_copy", reads=[vtF], writes=[pr["vbf"]], out=pr["vbf"][:], in_=vtF[:])
                bk = pbk[pbi % NPBK]; pbi += 1
                S.op("pe", "matmul", reads=[aupb, adT], writes=[bk], out=bk[:, :], lhsT=aupb[64 * d:64 * d + 64, hsl],
                     rhs=adT[64 * d:64 * d + 64, tsl], start=True, stop=True)
                S.op("act", "activation", reads=[bk, pc], writes=[alpha], out=alpha[:], in_=bk[:, :], func=AF.Sigmoid,
                     bias=pc[:, col["a0_%d" % d] + hp:col["a0_%d" % d] + hp + 1])
                bk = pbk[pbi % NPBK]; pbi += 1
                S.op("pe", "matmul", reads=[wupb, twd], writes=[bk], out=bk[:, :], lhsT=wupb[64 * d:64 * d + 64, hsl],
                     rhs=twd[64 * d:64 * d + 64, tsl], start=True, stop=True)
                S.op("act", "activation", reads=[bk, pc], writes=[sg], out=sg[:], in_=bk[:, :], func=AF.Sigmoid,
                     bias=pc[:, col["w0_%d" % d] + hp:col["w0_%d" % d] + hp + 1])
                S.op("dve", "tensor_tensor_scan", reads=[reset, sg], writes=[cs], out=cs[:], data0=reset[:], data1=sg[:],
                     initial=0.0, op0=ALU.mult, op1=ALU.add)
                S.op("act", "activation", reads=[cs], writes=[pr["etot"]], out=pr["etot"][:], in_=v3(cs[:])[:, :, 127], func=AF.Exp,
                     scale=-C0)
                if d == 0:
                    cm = cs
                else:
                    S.op("dve", "tensor_tensor", reads=[sg, cs], writes=[t1], out=t1[:], in0=sg[:], in1=cs[:], op=ALU.subtract)
                    S.op("dve", "tensor_tensor", reads=[t1, cs], writes=[cum], out=v3(cum[:]), in0=v3(t1[:]),
                         in1=v3(cs[:])[:, :, 127:128].to_broadcast([128, CPB, 128]), op=ALU.add)
                    cm = cum
                S.op("pool", "tensor_tensor", reads=[cm, sg], writes=[t1], out=t1[:], in0=cm[:], in1=sg[:], op=ALU.subtract)
                S.op("act", "activation", reads=[t1], writes=[t1], out=t1[:], in_=t1[:], func=AF.Exp, scale=-C0)
                S.op("act", "activation", reads=[cm], writes=[t2], out=t2[:], in_=cm[:], func=AF.Exp, scale=-C0)
                S.op("act", "activation", reads=[cm], writes=[t3], out=t3[:], in_=cm[:], func=AF.Exp, scale=C0)
                S.op("dve", "tensor_scalar", reads=[kF, pc], writes=[t4], out=t4[:], in0=kF[:],
                     scalar1=pc[:, col["k_k"] + hp:col["k_k"] + hp + 1], scalar2=None, op0=ALU.mult)
                S.op("pool", "tensor_tensor", reads=[t4], writes=[sqb], out=sqb[:], in0=t4[:], in1=t4[:], op=ALU.mult)
                bk = pbk[pbi % NPBK]; pbi += 1
                S.op("pe", "matmul", reads=[bones, sqb], writes=[bk], out=bk[:, :], lhsT=bones[:], rhs=sqb[:], start=True, stop=True)
                S.op("act", "activation", reads=[bk], writes=[t5], out=t5[:], in_=bk[:, :], func=AF.Sqrt)
                S.op("dve", "tensor_scalar", reads=[t5], writes=[t5], out=t5[:], in0=t5[:], scalar1=1e-12, scalar2=None, op0=ALU.max)
                S.op("dve", "reciprocal", reads=[t5], writes=[t5], out=t5[:], in_=t5[:])
                S.op("dve", "tensor_tensor", reads=[t4, t5], writes=[t4], out=t4[:], in0=t4[:], in1=t5[:], op=ALU.mult)
                S.op("act", "activation", reads=[alpha, pc], writes=[t5], out=t5[:], in_=alpha[:], func=AF.Identity,
                     scale=pc[:, col["k_a"] + hp:col["k_a"] + hp + 1], bias=pc[:, col["omka"] + hp:col["omka"] + hp + 1])
                S.op("dve", "tensor_tensor", reads=[t5, kF], writes=[t5], out=t5[:], in0=t5[:], in1=kF[:], op=ALU.mult)
                ar = pr["ar"]
                S.op("dve", "tensor_tensor", reads=[rF, t2], writes=[ar], out=ar[:, :, 1, :], in0=v3(rF[:]), in1=v3(t2[:]), op=ALU.mult)
                S.op("dve", "scalar_tensor_tensor", reads=[t4, t1], writes=[ar], out=ar[:, :, 0, :], in0=v3(t4[:]), scalar=-1.0,
                     in1=v3(t1[:]), op0=ALU.mult, op1=ALU.mult)
                S.op("pool", "tensor_tensor", reads=[t5, t3], writes=[pr["kt"]], out=pr["kt"][:], in0=t5[:], in1=t3[:], op=ALU.mult)
                S.op("pool", "tensor_tensor", reads=[t4, alpha], writes=[t2], out=t2[:], in0=t4[:], in1=alpha[:], op=ALU.mult)
                S.op("pool", "tensor_tensor", reads=[t2, t3], writes=[pr["bt"]], out=pr["bt"][:], in0=t2[:], in1=t3[:], op=ALU.mult)
                etb = pr["etot"][:, 0:CPB].unsqueeze(2).to_broadcast([128, CPB, 128])
                S.op("dve", "tensor_tensor", reads=[pr["kt"], pr["etot"]], writes=[pr["kh"]], out=v3(pr["kh"][:]), in0=v3(pr["kt"][:]),
                     in1=etb, op=ALU.mult)
                S.op("dve", "tensor_tensor", reads=[pr["bt"], pr["etot"]], writes=[pr["bh"]], out=v3(pr["bh"][:]), in0=v3(pr["bt"][:]),
                     in1=etb, op=ALU.mult)
                S.op("dve", "scalar_tensor_tensor", reads=[rF, pc, t5], writes=[xbb], out=xbb[:], in0=rF[:],
                     scalar=pc[:, col["r_k"] + hp:col["r_k"] + hp + 1], in1=t5[:], op0=ALU.mult, op1=ALU.mult)
                bk = pbk[pbi % NPBK]; pbi += 1
                for c in range(CPB):
                    S.op("pe", "matmul", reads=[xbb, selb], writes=[bk], out=bk[:, 2 * c:2 * c + 2], lhsT=xbb[:, c * 128:(c + 1) * 128],
                         rhs=selb[:], start=True, stop=True)
                cf = coef[:, blk * CPB:(blk + 1) * CPB, 2 * hp:2 * hp + 2]
                S.op("dve", "tensor_tensor", reads=[bk, coef], writes=[coef], out=cf, in0=bk[:, 0:2 * CPB].rearrange("p (c e) -> p c e", e=2),
                     in1=cf, op=ALU.add)
                for nm, srcT in (("atk", None), ("bhk", pr["bh"]), ("khk", pr["kh"])):
                    bk = pbk[pbi % NPBK]; pbi += 1
                    bkb = bk.t[:].bitcast(BF16)
                    for c in range(CPB):
                        in_ = ar[:, c, 0, :] if srcT is None else srcT[:, c * 128:(c + 1) * 128]
                        S.op("pe", "transpose", reads=[ar if srcT is None else srcT, identb], writes=[bk],
                             out=bkb[:, c * 128:(c + 1) * 128], in_=in_, identity=identb[:])
                    evac_copy(pr[nm][:], bkb[:, 0:CPB * 128].rearrange("p (c t) -> p c t", c=CPB), [bk], [pr[nm]])

                if cut <= 1:
                    continue
                corder = list(range(CPB)) if d == 0 else list(range(CPB - 1, -1, -1))
                for g0 in range(0, CPB, NSLOT):
                    grp = corder[g0:g0 + NSLOT]
                    for si, c in enumerate(grp):
                        sl = slots[si]
                        Z = sl["Z"]
                        csl = slice(c * 128, (c + 1) * 128)
                        for e in range(2):
                            ps_ = slice(64 * e, 64 * e + 64)
                            S.op("pe", "matmul", reads=[pr["bt"], ar], writes=[Z], out=Z[:, e, 0:256], lhsT=pr["bt"][ps_, csl],
                                 rhs=ar[ps_, c, :, :], start=True, stop=True)
                            S.op("pe", "matmul", reads=[pr["kt"], ar], writes=[Z], out=Z[:, e, 256:512], lhsT=pr["kt"][ps_, csl],
                                 rhs=ar[ps_, c, :, :], start=True, stop=True)
                        PTT0 = sl["PTT"][0]; PTT1 = sl["PTT"][1]
                        S.op("dve", "tensor_tensor", reads=[Z, mT2[d]], writes=[PTT0], out=PTT0[:, :, 0:128], in0=Z[:, :, 0:128],
                             in1=mT2[d][:, :, 0:128], op=ALU.mult)
                        S.op("dve", "tensor_tensor", reads=[Z, mT2[d]], writes=[sl["arb"]], out=sl["arb"][:], in0=Z[:, :, 128:256],
                             in1=mT2[d][:, :, 128:256], op=ALU.mult)
                        S.op("dve", "tensor_tensor", reads=[Z, mT2[d]], writes=[sl["s2b"]], out=sl["s2b"][:], in0=Z[:, :, 256:512], in1=mT2[d][:],
                             op=ALU.mult)
                        S.op("pool", "tensor_tensor", reads=[PTT0, ident2], writes=[PTT1], out=PTT1[:, :, 128:256], in0=PTT0[:, :, 0:128],
                             in1=ident2[:], op=ALU.add)
                    for si, c in enumerate(grp):
                        sl = slots[si]
                        Z = sl["Z"]
                        for e in range(2):
                            ps_ = slice(64 * e, 64 * e + 64)
                            S.op("pe", "matmul", reads=[ar, pr["bt"]], writes=[Z], out=Z[:, e, 0:128], lhsT=ar[ps_, c, 0, :],
                                 rhs=pr["bt"][ps_, c * 128:(c + 1) * 128], start=True, stop=True)
                        S.op("dve", "tensor_tensor", reads=[Z, mN2[d]], writes=[sl["P"][0]], out=sl["P"][0][:], in0=Z[:, :, 0:128], in1=mN2[d][:],
                             op=ALU.mult)
                    if cut <= 2:
                        continue
                    for k in range(7):
                        for si, c in enumerate(grp):
                            sl = slots[si]
                            Z = sl["Z"]
                            Px, Py = sl["P"][k % 2], sl["P"][(k + 1) % 2]
                            Tx, Ty = sl["PTT"][k % 2], sl["PTT"][(k + 1) % 2]
                            for e in range(2):
                                if k == 0:
                                    S.op("pe", "matmul", reads=[Px, Tx], writes=[Z], out=Z[:, e, 0:128], lhsT=Px[:, e, :],
                                         rhs=Tx[:, e, 0:128], start=True, stop=True)
                                elif k <= 4:
                                    S.op("pe", "matmul", reads=[Px, Tx], writes=[Z], out=Z[:, e, 0:256], lhsT=Px[:, e, :],
                                         rhs=Tx[:, e, :], start=True, stop=True)
                                else:
                                    S.op("pe", "matmul", reads=[Px, Tx], writes=[Z], out=Z[:, e, 128:256], lhsT=Px[:, e, :],
                                         rhs=Tx[:, e, 128:256], start=True, stop=True)
                                if k <= 5:
                                    S.op("pe", "matmul", reads=[Px, Tx], writes=[Z], out=Z[:, e, 256:384], lhsT=Tx[:, e, 0:128],
                                         rhs=Px[:, e, :], start=True, stop=True)
                            if k <= 5:
                                evac2(Py[:], Z[:, :, 256:384], [Z], [Py], eng="act")
                            if k <= 4:
                                S.op("dve", "tensor_copy", reads=[Z], writes=[Ty], out=Ty[:, :, 0:128], in_=Z[:, :, 0:128])
                            if k >= 1:
                                S.op("dve", "tensor_tensor", reads=[Z, Tx], writes=[Ty], out=Ty[:, :, 128:256], in0=Z[:, :, 128:256],
                                     in1=Tx[:, :, 128:256], op=ALU.add)
                    if cut <= 3:
                        continue
                    TTF = 1
                    for si, c in enumerate(grp):
                        sl = slots[si]
                        Z = sl["Z"]
                        for e in range(2):
                            S.op("pe", "matmul", reads=[sl["s2b"], pr["vbf"]], writes=[Z], out=Z[:, e, 384:448], lhsT=sl["s2b"][:, e, 0:128],
                                 rhs=pr["vbf"][:, c, 64 * e:64 * e + 64], start=True, stop=True)
                        evac2(sl["ysb"][:], Z[:, :, 384:448], [Z], [sl["ysb"]], eng="act")
                    if cut <= 3.1:
                        continue
                    for si, c in enumerate(grp):
                        sl = slots[si]
                        Z = sl["Z"]
                        TT = sl["PTT"][TTF]
                        for e in range(2):
                            S.op("pe", "matmul", reads=[TT, sl["ysb"]], writes=[Z], out=Z[:, e, 0:64], lhsT=TT[:, e, 128:256],
                                 rhs=sl["ysb"][:, e, :], start=True, stop=True)
                            S.op("pe", "matmul", reads=[TT, pr["atk"]], writes=[Z], out=Z[:, e, 64:128], lhsT=TT[:, e, 128:256],
                                 rhs=pr["atk"][:, c, 64 * e:64 * e + 64], start=True, stop=True)
                        evac2(sl["w1m"][:], Z[:, :, 0:128], [Z], [sl["w1m"]], eng="act")
                    if cut <= 3.2:
                        continue
                    for si, c in enumerate(grp):
                        sl = slots[si]
                        Z = sl["Z"]
                        w1m = sl["w1m"]
                        for e in range(2):
                            ps_ = slice(64 * e, 64 * e + 64)
                            S.op("pe", "matmul", reads=[w1m, pr["bhk"]], writes=[Z], out=Z[ps_, e, 448:512], lhsT=w1m[:, e, 64:128],
                                 rhs=pr["bhk"][:, c, 64 * e:64 * e + 64], start=True, stop=True)
                            S.op("pe", "matmul", reads=[w1m, sl["arb"]], writes=[Z], out=Z[ps_, e, 128:256], lhsT=w1m[:, e, 64:128],
                                 rhs=sl["arb"][:, e, :], start=True, stop=True)
                        if cut <= 3.3:
                            continue
                        for e in range(2):
                            ps_ = slice(64 * e, 64 * e + 64)
                            S.op("dve", "scalar_tensor_tensor", reads=[ident2s, pr["etot"], Z], writes=[sl["phiT"]], out=sl["phiT"][ps_, :],
                                 in0=ident2s[ps_, :], scalar=pr["etot"][ps_, c:c + 1], in1=Z[ps_, e, 448:512], op0=ALU.mult, op1=ALU.add)
                            S.op("dve", "tensor_tensor", reads=[Z, ar], writes=[sl["xT"]], out=sl["xT"][ps_, :], in0=Z[ps_, e, 128:256],
                                 in1=ar[ps_, c, 1, :], op=ALU.add)
                    if cut <= 4:
                        continue
                    for si, c in enumerate(grp):
                        sl = slots[si]
                        Z = sl["Z"]
                        w1m = sl["w1m"]
                        Hc, Hn = Hs[hcur], Hs[1 - hcur]
                        for e in range(2):
                            ps_ = slice(64 * e, 64 * e + 64)
                            vv = pr["vbf"][:, c, 64 * e:64 * e + 64]
                            S.op("pe", "matmul", reads=[sl["arb"], w1m], writes=[Z], out=Z[:, e, 256:320], lhsT=sl["arb"][:, e, :],
                                 rhs=w1m[:, e, 0:64], start=True, stop=False)
                            S.op("pe", "matmul", reads=[sl["s2b"], pr["vbf"]], writes=[Z], out=Z[:, e, 256:320],
                                 lhsT=sl["s2b"][:, e, 128:256], rhs=vv, start=False, stop=False)
                            S.op("pe", "matmul", reads=[sl["xT"], Hc], writes=[Z], out=Z[:, e, 256:320], lhsT=sl["xT"][ps_, :],
                                 rhs=Hc[ps_, :], start=False, stop=True)
                            S.op("pe", "matmul", reads=[pr["bhk"], w1m], writes=[Z], out=Z[ps_, e, 320:384], lhsT=pr["bhk"][:, c, 64 * e:64 * e + 64],
                                 rhs=w1m[:, e, 0:64], start=True, stop=False)
                            S.op("pe", "matmul", reads=[pr["khk"], pr["vbf"]], writes=[Z], out=Z[ps_, e, 320:384],
                                 lhsT=pr["khk"][:, c, 64 * e:64 * e + 64], rhs=vv, start=False, stop=False)
                            S.op("pe", "matmul", reads=[sl["phiT"], Hc], writes=[Z], out=Z[ps_, e, 320:384], lhsT=sl["phiT"][ps_, :],
                                 rhs=Hc[ps_, :], start=False, stop=True)
                        for e in range(2):
                            ps_ = slice(64 * e, 64 * e + 64)
                            S.op("act", "activation", reads=[Z], writes=[Hn], out=Hn[ps_, :], in_=Z[ps_, e, 320:384], func=AF.Copy)
                        hcur = 1 - hcur
                        tile_i = blk * CPB + c
                        osl = obuf[:, tile_i, hp * 128:(hp + 1) * 128].rearrange("p (e n) -> p e n", e=2)
                        if d == 0:
                            S.op("dve", "tensor_copy", reads=[Z], writes=[obuf], out=osl, in_=Z[:, :, 256:320])
                        else:
                            S.op("dve", "tensor_tensor", reads=[Z, obuf], writes=[obuf], out=osl, in0=Z[:, :, 256:320], in1=osl, op=ALU.add)

    cv_flush(None)
    for q in range(8):
        S.dma("sp" if q % 2 == 0 else "act", OB[q * 512:(q + 1) * 512, :].rearrange("(n p) c -> p n c", p=128), obuf[:, q * 4:(q + 1) * 4, :], reads=[obuf])
    CF = dscr("CF", [128, NT * 8])
    S.dma("sp", CF[:, :], coef[:].rearrange("p n e -> p (n e)"), reads=[coef])
    S.barrier()
    release(len(guards) - n_ph3)

    X1 = dscr("X1", [S_LEN, D])
    coef = sb("coef5", [128, NT * 8])
    S.dma("sp", coef[:], CF[:, :], writes=[coef])
    lnw_b = sb("lnw_b", [128, 512]); lnb_b = sb("lnb_b", [128, 512]); wb2 = sb("nw2b", [128, D]); wfb = sb("nwfb", [128, D])
    S.dma("sp", lnw_b[:], ln_x_w.partition_broadcast(128), writes=[lnw_b])
    S.dma("sp", lnb_b[:], ln_x_b.partition_broadcast(128), writes=[lnb_b])
    S.dma("sp", wb2[:], norm2_w.partition_broadcast(128), writes=[wb2])
    S.dma("sp", wfb[:], norm_f_w.partition_broadcast(128), writes=[wfb])
    stg = sb("stg", [128, 2048])
    gupA = sb("gupA", [128, 512], BF16); gupB = sb("gupB", [32, 512], BF16)
    S.dma("sp", stg[:, 0:512], g_up[0:128, :], writes=[stg])
    S.dma("sp", stg[0:32, 512:1024], g_up[128:160, :], writes=[stg])
    S.op("dve", "tensor_copy", reads=[stg], writes=[gupA], out=gupA[:], in_=stg[:, 0:512])
    S.op("dve", "tensor_copy", reads=[stg], writes=[gupB], out=gupB[:], in_=stg[0:32, 512:1024])
    woutb = sb("woutb", [128, 8, D], BF16)
    for kc in range(0, 8, 2):
        S.dma("sp", stg[:].rearrange("p (k n) -> p k n", k=2), w_out[:, kc:kc + 2, :], writes=[stg])
        S.op("dve", "tensor_copy", reads=[stg], writes=[woutb], out=woutb[:, kc:kc + 2, :], in_=stg[:].rearrange("p (k n) -> p k n", k=2))
    if peer:
        wqb = sb("wqb", [128, 8, 2048], BF16)
        for kc in range(8):
            S.dma("sp", stg[:], wq[:, kc, :], writes=[stg])
            S.op("dve", "tensor_copy", reads=[stg], writes=[wqb], out=wqb[:, kc, :], in_=stg[:])
        keyb = sb("keyb", [128, 16, 128], BF16)
        S.dma("sp", stg[:].rearrange("p (k n) -> p k n", k=16), keysT[:, :, :], writes=[stg])
        S.op("dve", "tensor_copy", reads=[stg], writes=[keyb], out=keyb[:], in_=stg[:].rearrange("p (k n) -> p k n", k=16))
        iota_i = sb("iota_i", [128, 256], I32); iota_f = sb("iota_f", [128, 256])
        S.op("pool", "iota", writes=[iota_i], out=iota_i[:], pattern=[[1, 256]], base=0, channel_multiplier=0)
        S.op("dve", "tensor_copy", reads=[iota_i], writes=[iota_f], out=iota_f[:], in_=iota_i[:])
        sc2 = sb("sc2", [128, 2048])
        m1 = sb("m1", [128, 256]); i1 = sb("i1", [128, 256], U32); idxf = sb("idxf", [128, 256]); idx128 = sb("idx128", [128, 256])
        cand = stg
        r1u = sb("r1u", [128, 128], U32); r2u = sb("r2u", [128, 128], U32); r2f = sb("r2f", [128, 128])
        e1v = sb("e1v", [128, 128]); e2v = sb("e2v", [128, 128])
        sc16 = sb("sc16", [128, 128]); pos = sb("pos", [128, 128], U32); posf = sb("posf", [128, 128])
        eidf = sb("eidf", [128, 128]); eid = sb("eid", [128, 128], U32)
        gsm = sb("gsm", [128, 16]); gate = sb("gate", [128, 128]); aact = sb("aact", [128, 128]); wgt = sb("wgt", [128, 128])
        junk = sb("junk", [128, D], BF16)
        NG = 8
        m1c = [Buf() for _ in range(16)]; i1c = [Buf() for _ in range(16)]; sc2c = [Buf() for _ in range(16)]
        s16c = [Buf() for _ in range(8)]; posc = [Buf() for _ in range(8)]
        eidc = [Buf() for _ in range(128)]; aactc = [Buf() for _ in range(128)]
        accb = Buf("accb")
        uvg = [sb("uvg%d" % i, [128, 2 * D], BF16) for i in range(NG)]
        dg = [sb("dg%d" % i, [128, 128], BF16) for i in range(NG)]
        gel = sb("gel", [128, 128]); gelc = [Buf() for _ in range(128)]
        gateb = sb("gateb", [128, 128]); h2bb = sb("h2bb", [128, D])
        qT = sb("qT", [128, 16, 128], BF16)
    ot = sb("ot", [128, 512]); tm = sb("tm5", [128, 512]); vt5 = sb("vt5", [128, 512])
    st8 = sb("st8", [128, 64])
    ybf = sb("ybf", [128, 512], BF16); yT = sb("yT5", [128, 4, 128], BF16); ypT = sb("ypT", [128, 4, 128], BF16)
    xt5 = sb("xt5", [128, D]); x1 = sb("x1", [128, D]); h2 = sb("h2", [128, D]); h2b = sb("h2b", [128, D], BF16)
    h2T = sb("h2T", [128, 8, 128], BF16)
    ssq5 = sb("ssq5", [128, 8])
    junk2 = junk if peer else sb("junk2", [128, D], BF16)
    P2 = T(pall.t[:, 0:2, :], "P2"); P2.b.excl = True
    P4 = T(pall.t[:, 4:6, :], "P4"); P4.b.excl = True
    PV = T(pall.t[:, 6:8, :], "PV"); PV.b.excl = True
    b2, b3 = banks[2], banks[3]

    def rms(src, dst_f32, wbt, k0):
        S.op("act", "activation", reads=[src], writes=[junk2, ssq5], out=junk2[:], in_=src[:], func=AF.Square,
             accum_out=ssq5[:, k0:k0 + 1])
        S.op("act", "activation", reads=[ssq5], writes=[ssq5], out=ssq5[:, k0 + 1:k0 + 2], in_=ssq5[:, k0:k0 + 1], func=AF.Sqrt,
             scale=1.0 / D, bias=epsc[:, 0:1])
        S.op("dve", "reciprocal", reads=[ssq5], writes=[ssq5], out=ssq5[:, k0 + 2:k0 + 3], in_=ssq5[:, k0 + 1:k0 + 2])
        S.op("dve", "scalar_tensor_tensor", reads=[src, ssq5, wbt], writes=[dst_f32], out=dst_f32[:], in0=src[:],
             scalar=ssq5[:, k0 + 2:k0 + 3], in1=wbt[:], op0=ALU.mult, op1=ALU.mult)

    x1s = [x1, sb("x1b", [128, D])]
    h2s = [h2, h2bb] if peer else [h2, h2]
    gates = [gate, gateb] if peer else None
    eids = [eid, sb("eidb", [128, 128], U32)] if peer else None

    def partA(i):
        tsl = slice(i * 128, (i + 1) * 128)
        x1 = x1s[i % 2]
        eid = eids[i % 2] if peer else None
        h2 = h2s[i % 2]
        gate = gates[i % 2] if peer else None
        S.dma("sp", ot[:], OB[tsl, :], writes=[ot])
        S.dma("act", vt5[:], VTOK[tsl, :], writes=[vt5])
        S.dma("sp", ypT[:], YP[:, tsl].rearrange("(g p) t -> p g t", p=128), writes=[ypT])
        S.dma("act", xt5[:], x[tsl, :], writes=[xt5])
        o3 = ot[:].rearrange("p (h n) -> p h n", h=8)
        t3 = tm[:].rearrange("p (h n) -> p h n", h=8)
        S.op("dve", "tensor_reduce", reads=[ot], writes=[st8], out=st8[:, 0:8], in_=o3, axis=AX.X, op=ALU.add)
        S.op("pool", "tensor_tensor", reads=[ot], writes=[tm], out=tm[:], in0=ot[:], in1=ot[:], op=ALU.mult)
        S.op("dve", "tensor_reduce", reads=[tm], writes=[st8], out=st8[:, 8:16], in_=t3, axis=AX.X, op=ALU.add)
        S.op("dve", "tensor_scalar", reads=[st8], writes=[st8], out=st8[:, 16:24], in0=st8[:, 0:8], scalar1=1.0 / 64, scalar2=None, op0=ALU.mult)
        S.op("dve", "tensor_tensor", reads=[st8], writes=[st8], out=st8[:, 24:32], in0=st8[:, 16:24], in1=st8[:, 16:24], op=ALU.mult)
        S.op("dve", "scalar_tensor_tensor", reads=[st8], writes=[st8], out=st8[:, 32:40], in0=st8[:, 8:16], scalar=1.0 / 64, in1=st8[:, 24:32],
             op0=ALU.mult, op1=ALU.subtract)
        S.op("act", "activation", reads=[st8], writes=[st8], out=st8[:, 40:48], in_=st8[:, 32:40], func=AF.Sqrt, bias=epsc[:, 1:2])
        S.op("dve", "reciprocal", reads=[st8], writes=[st8], out=st8[:, 48:56], in_=st8[:, 40:48])
        S.op("dve", "tensor_tensor", reads=[ot, st8], writes=[tm], out=t3, in0=o3, in1=st8[:, 16:24].unsqueeze(2).to_broadcast([128, 8, 64]),
             op=ALU.subtract)
        S.op("dve", "tensor_tensor", reads=[tm, st8], writes=[tm], out=t3, in0=t3, in1=st8[:, 48:56].unsqueeze(2).to_broadcast([128, 8, 64]),
             op=ALU.mult)
        S.op("pool", "tensor_tensor", reads=[tm, lnw_b], writes=[tm], out=tm[:], in0=tm[:], in1=lnw_b[:], op=ALU.mult)
        S.op("pool", "tensor_tensor", reads=[tm, lnb_b], writes=[tm], out=tm[:], in0=tm[:], in1=lnb_b[:], op=ALU.add)
        S.op("dve", "tensor_tensor", reads=[vt5, coef], writes=[vt5], out=vt5[:].rearrange("p (h n) -> p h n", h=8),
             in0=vt5[:].rearrange("p (h n) -> p h n", h=8), in1=coef[:, i * 8:(i + 1) * 8].unsqueeze(2).to_broadcast([128, 8, 64]), op=ALU.mult)
        S.op("dve", "tensor_tensor", reads=[tm, vt5], writes=[tm], out=tm[:], in0=tm[:], in1=vt5[:], op=ALU.add)
        S.op("pe", "matmul", reads=[sgA, gupA], writes=[b2], out=b2[:, :], lhsT=sgA[:, tsl], rhs=gupA[:], start=True, stop=False)
        S.op("pe", "matmul", reads=[sgB, gupB], writes=[b2], out=b2[:, :], lhsT=sgB[:, tsl], rhs=gupB[:], start=False, stop=True)
        S.op("dve", "tensor_tensor", reads=[tm, b2], writes=[ybf], out=ybf[:], in0=tm[:], in1=b2[:, :], op=ALU.mult)
        b3b = b3.t.bitcast(BF16)
        for q in range(4):
            S.op("pe", "transpose", reads=[ybf, identb], writes=[b3], out=b3b[:, q * 128:(q + 1) * 128], in_=ybf[:, q * 128:(q + 1) * 128],
                 identity=identb[:])
        S.op("act", "activation", reads=[b3], writes=[yT], out=yT[:], in_=b3b[:, 0:512].rearrange("p (q t) -> p q t", q=4), func=AF.Copy)
        for hf in range(2):
            for kc in range(8):
                lt = yT[:, kc, :] if kc < 4 else ypT[:, kc - 4, :]
                S.op("pe", "matmul", reads=[yT, ypT, woutb], writes=[P2], out=P2[:, hf, :], lhsT=lt, rhs=woutb[:, kc, hf * 512:(hf + 1) * 512],
                     start=(kc == 0), stop=(kc == 7))
        S.op("dve", "tensor_tensor", reads=[P2, xt5], writes=[x1], out=x1[:].rearrange("p (a n) -> p a n", a=2), in0=P2[:, :, :],
             in1=xt5[:].rearrange("p (a n) -> p a n", a=2), op=ALU.add)
        if "X1" in debug:
            S.dma("sp", X1[tsl, :], x1[:], reads=[x1])
        if not peer:
            rms(x1, h2, wfb, 0)
            S.dma("sp", out[tsl, :], h2[:], reads=[h2])
            return
        rms(x1, h2, wb2, 0)
        S.op("pool", "tensor_copy", reads=[h2], writes=[h2b], out=h2b[:], in_=h2[:])
        for kc in range(8):
            S.op("pe", "transpose", reads=[h2b, identb], writes=[b3], out=b3b[:, kc * 128:(kc + 1) * 128], in_=h2b[:, kc * 128:(kc + 1) * 128],
                 identity=identb[:])
        S.op("act", "activation", reads=[b3], writes=[h2T], out=h2T[:], in_=b3b[:, :].rearrange("p (k t) -> p k t", k=8), func=AF.Copy)
        for c4 in range(4):
            bk = b2 if c4 % 2 == 0 else b3
            for cq in range(4):
                ch = c4 * 4 + cq
                for kc in range(8):
                    S.op("pe", "matmul", reads=[wqb, h2T], writes=[bk], out=bk[:, cq * 128:(cq + 1) * 128], lhsT=wqb[:, kc, ch * 128:(ch + 1) * 128],
                         rhs=h2T[:, kc, :], start=(kc == 0), stop=(kc == 7))
            S.op("act" if c4 % 2 == 0 else "dve", "activation" if c4 % 2 == 0 else "tensor_copy", reads=[bk], writes=[qT],
                 out=qT[:, c4 * 4:(c4 + 1) * 4, :], in_=bk[:, :].rearrange("p (c t) -> p c t", c=4), **({"func": AF.Copy} if c4 % 2 == 0 else {}))
        for half in range(2):
            for c8 in range(8):
                ch = half * 8 + c8
                S.op("pe", "matmul", reads=[qT, keyb], writes=[P4], out=P4[:, c8 // 4, (c8 % 4) * 128:(c8 % 4 + 1) * 128], lhsT=qT[:, ch, :],
                     rhs=keyb[:, ch, :], start=True, stop=True)
            S.op("dve", "tensor_copy", reads=[P4], writes=[stg], out=stg[:, half * 1024:(half + 1) * 1024].rearrange("p (a n) -> p a n", a=2),
                 in_=P4[:, :, :])
        scv = stg[:].rearrange("p (c n) -> p c n", c=16)
        sc2v = sc2[:].rearrange("p (c n) -> p c n", c=16)
        m1v = m1[:].rearrange("p (c n) -> p c n", c=16)
        i1v = i1[:].rearrange("p (c n) -> p c n", c=16)
        for ch in range(16):
            S.op("dve", "max", reads=[stg], writes=[m1c[ch]], out=m1v[:, ch, 0:8], in_=scv[:, ch, :])
        for ch in range(16):
            S.op("dve", "max_index", reads=[stg, m1c[ch]], writes=[i1c[ch]], out=i1v[:, ch, 0:8], in_max=m1v[:, ch, 0:8], in_values=scv[:, ch, :])
        for ch in range(16):
            S.op("dve", "match_replace", reads=[stg, m1c[ch]], writes=[sc2c[ch]], out=sc2v[:, ch, :], in_to_replace=m1v[:, ch, 0:8],
                 in_values=scv[:, ch, :], imm_value=-1e30)
        for ch in range(16):
            S.op("dve", "max", reads=[sc2c[ch]], writes=[m1c[ch]], out=m1v[:, ch, 8:16], in_=sc2v[:, ch, :])
        for ch in range(16):
            S.op("dve", "max_index", reads=[sc2c[ch], m1c[ch]], writes=[i1c[ch]], out=i1v[:, ch, 8:16], in_max=m1v[:, ch, 8:16],
                 in_values=sc2v[:, ch, :])
        S.op("dve", "tensor_copy", reads=i1c, writes=[idxf], out=idxf[:], in_=i1[:])
        S.op("dve", "tensor_scalar", reads=[idxf], writes=[idx128], out=idx128[:], in0=idxf[:], scalar1=128.0, scalar2=None, op0=ALU.mult)
        m4 = m1[:].rearrange("p (h a n) -> p h a n", h=8, a=2)
        x4 = idxf[:].rearrange("p (h a n) -> p h a n", h=8, a=2)
        y4 = idx128[:].rearrange("p (h a n) -> p h a n", h=8, a=2)
        c4v = cand[:].rearrange("p (h i j) -> p h i j", h=8, i=16)
        S.op("dve", "tensor_tensor", reads=m1c, writes=[cand], out=c4v, in0=m4[:, :, 0, :].unsqueeze(3).to_broadcast([128, 8, 16, 16]),
             in1=m4[:, :, 1, :].unsqueeze(2).to_broadcast([128, 8, 16, 16]), op=ALU.add)
        cv = cand[:].rearrange("p (h n) -> p h n", h=8)
        c2v = sc2[:].rearrange("p (h n) -> p h n", h=8)
        s16 = sc16[:].rearrange("p (h n) -> p h n", h=8)
        p16 = pos[:].rearrange("p (h n) -> p h n", h=8)
        for hh in range(8):
            S.op("dve", "max", reads=[cand], writes=[s16c[hh]], out=s16[:, hh, 0:8], in_=cv[:, hh, :])
        for hh in range(8):
            S.op("dve", "max_index", reads=[cand, s16c[hh]], writes=[posc[hh]], out=p16[:, hh, 0:8], in_max=s16[:, hh, 0:8], in_values=cv[:, hh, :])
        for hh in range(8):
            S.op("dve", "match_replace", reads=[cand, s16c[hh]], writes=[sc2c[2 * hh], sc2c[2 * hh + 1]], out=c2v[:, hh, :],
                 in_to_replace=s16[:, hh, 0:8], in_values=cv[:, hh, :], imm_value=-1e30)
        for hh in range(8):
            S.op("dve", "max", reads=[sc2c[2 * hh], sc2c[2 * hh + 1]], writes=[s16c[hh]], out=s16[:, hh, 8:16], in_=c2v[:, hh, :])
        for hh in range(8):
            S.op("dve", "max_index", reads=[sc2c[2 * hh], sc2c[2 * hh + 1], s16c[hh]], writes=[posc[hh]], out=p16[:, hh, 8:16],
                 in_max=s16[:, hh, 8:16], in_values=c2v[:, hh, :])
        S.op("dve", "tensor_scalar", reads=posc, writes=[r1u], out=r1u[:], in0=pos[:], scalar1=4, scalar2=None, op0=ALU.logical_shift_right)
        S.op("dve", "tensor_scalar", reads=posc, writes=[r2u], out=r2u[:], in0=pos[:], scalar1=15, scalar2=None, op0=ALU.bitwise_and)
        S.op("dve", "tensor_copy", reads=[r1u], writes=[posf], out=posf[:], in_=r1u[:])
        S.op("dve", "tensor_copy", reads=[r2u], writes=[r2f], out=r2f[:], in_=r2u[:])
        io4 = iota_f[:, 0:16].unsqueeze(1).unsqueeze(1).to_broadcast([128, 8, 16, 16])
        for (rf_, src4, dstv) in ((posf, y4[:, :, 0, :], e1v), (r2f, x4[:, :, 1, :], e2v)):
            S.op("dve", "tensor_tensor", reads=[iota_f, rf_], writes=[cand], out=c4v, in0=io4,
                 in1=rf_[:].rearrange("p (h j) -> p h j", h=8).unsqueeze(3).to_broadcast([128, 8, 16, 16]), op=ALU.is_equal)
            S.op("dve", "tensor_tensor", reads=[cand, idxf, idx128], writes=[cand], out=c4v, in0=c4v,
                 in1=src4.unsqueeze(2).to_broadcast([128, 8, 16, 16]), op=ALU.mult)
            S.op("dve", "tensor_reduce", reads=[cand], writes=[dstv], out=dstv[:], in_=cand[:].rearrange("p (q i) -> p q i", i=16), axis=AX.X, op=ALU.add)
        S.op("dve", "tensor_tensor", reads=[e1v, e2v], writes=eidc, out=eidf[:], in0=e1v[:], in1=e2v[:], op=ALU.add)
        S.op("dve", "tensor_scalar", reads=eidc, writes=eidc, out=eidf[:], in0=eidf[:], scalar1=0.0, scalar2=16383.0, op0=ALU.max, op1=ALU.min)
        S.op("dve", "tensor_copy", reads=eidc, writes=[eid], out=eid[:], in_=eidf[:])
        g3 = gate[:].rearrange("p (h n) -> p h n", h=8)
        S.op("dve", "tensor_tensor", reads=s16c, writes=[gate], out=g3, in0=s16, in1=s16[:, :, 0:1].to_broadcast([128, 8, 16]), op=ALU.subtract)
        S.op("act", "activation", reads=[gate], writes=[gate], out=gate[:], in_=gate[:], func=AF.Exp)
        S.op("dve", "tensor_reduce", reads=[gate], writes=[gsm], out=gsm[:, 0:8], in_=g3, axis=AX.X, op=ALU.add)
        S.op("dve", "reciprocal", reads=[gsm], writes=[gsm], out=gsm[:, 8:16], in_=gsm[:, 0:8])
        S.op("dve", "tensor_tensor", reads=[gate, gsm], writes=[gate], out=g3, in0=g3, in1=gsm[:, 8:16].unsqueeze(2).to_broadcast([128, 8, 16]),
             op=ALU.mult)

    LAG = 5

    def partUV(i, interleave):
        tsl = slice(i * 128, (i + 1) * 128)
        eid = eids[i % 2]
        h2 = h2s[i % 2]
        gate = gates[i % 2]
        per = (len(S.pending) + 127) // 128 if interleave else 0

        def tail(hj):
            uv_ = uvg[hj % NG]
            d_ = dg[hj % NG]
            S.op("pool", "tensor_scalar", reads=[identb, gelc[hj], gate], writes=[d_], out=d_[:], in0=identb[:], scalar1=gel[:, hj:hj + 1],
                 scalar2=gate[:, hj:hj + 1], op0=ALU.mult, op1=ALU.mult)
            for hf in range(2):
                S.op("pe", "matmul", reads=[d_, uv_], writes=[PV], out=PV[:, hf, :], lhsT=d_[:], rhs=uv_[:, D + hf * 512:D + (hf + 1) * 512],
                     start=(hj == 0), stop=(hj == 127))

        for hj in range(128):
            uv_ = uvg[hj % NG]
            S.dma("pool", uv_[:], UVB[:, :], reads=[eid], writes=[uv_], indirect=bass.IndirectOffsetOnAxis(ap=eid[:, hj:hj + 1], axis=0))
            S.op("dve", "scalar_tensor_tensor", reads=[uv_, h2], writes=[aactc[hj], accb], out=junk[:], in0=uv_[:, 0:D], scalar=1.0, in1=h2[:],
                 op0=ALU.mult, op1=ALU.mult, accum_out=aact[:, hj:hj + 1])
            S.op("act", "activation", reads=[aactc[hj]], writes=[gelc[hj]], out=gel[:, hj:hj + 1], in_=aact[:, hj:hj + 1], func=AF.Gelu)
            if hj >= LAG:
                tail(hj - LAG)
            if per:
                S.flush(per)
        for hj in range(128 - LAG, 128):
            tail(hj)
        S.flush()

    def partF(i):
        tsl = slice(i * 128, (i + 1) * 128)
        x1 = x1s[i % 2]
        S.op("dve", "tensor_tensor", reads=[PV, x1], writes=[x1], out=x1[:].rearrange("p (a n) -> p a n", a=2), in0=PV[:, :, :],
             in1=x1[:].rearrange("p (a n) -> p a n", a=2), op=ALU.add)
        rms(x1, x1, wfb, 4)
        S.dma("sp", out[tsl, :], x1[:], reads=[x1])

    if not peer:
        for i in range(NT):
            partA(i)
    else:
        partA(0)
        for i in range(NT):
            if i + 1 < NT:
                S.rec = True
                partA(i + 1)
                S.rec = False
            partUV(i, True)
            partF(i)

    S.barrier()
    print("ops", S.nops, "waits", S.nwait, "dmas", S.dcount)
    return nc


def make_consts():
    idx = np.arange(128)
    ident = np.eye(128, dtype=np.float32)
    mus = (idx[None, :] > idx[:, None]).astype(np.float32)
    mui = (idx[None, :] >= idx[:, None]).astype(np.float32)
    mls = (idx[None, :] < idx[:, None]).astype(np.float32)
    mli = (idx[None, :] <= idx[:, None]).astype(np.float32)
    masks = np.stack([mus, mui, mls, mli], axis=1)
    bones = (idx[None, :] // 64 == idx[:, None] // 64).astype(np.float32)
    sel = (idx[:, None] // 64 == np.arange(2)[None, :]).astype(np.float32)
    reset = np.ones((128, 1024), np.float32)
    reset[:, ::128] = 0.0
    invcnt = np.zeros((128, 4, 16), np.float32)
    for gi, win in enumerate((2, 4, 8, 16)):
        half = win // 2
        for t in range(half):
            invcnt[:, gi, t] = 1.0 / (t + half)
        for q in range(half - 1):
            t = S_LEN - (half - 1) + q
            invcnt[:, gi, 8 + q] = 1.0 / (S_LEN - t + half)
    return dict(c_ident=ident, c_masks=masks, c_bones=bones, c_sel=sel, c_reset=reset, c_invcnt=invcnt)


def make_in_maps(inp, peer=True):
    f = lambda a: np.ascontiguousarray(np.asarray(a, dtype=np.float32))
    shared = dict(
        w_in=f(inp["w_in"][0].reshape(8, 128, D_IN).transpose(1, 0, 2)),
        shift_mu=f(inp["shift_mu"][0]), norm1_w=f(inp["norm1_w"][0]),
        w0=f(inp["w0"][0]), a0=f(inp["a0"][0]),
        w_up=f(inp["w_up"][0].reshape(128, 512)), a_up=f(inp["a_up"][0].reshape(128, 512)),
        g_up=f(inp["g_up"][0]), k_k=f(inp["k_k"][0]), k_a=f(inp["k_a"][0]), r_k=f(inp["r_k"][0].reshape(512)),
        ln_x_w=f(inp["ln_x_w"][0]), ln_x_b=f(inp["ln_x_b"][0]),
        pool_w=f(inp["pool_w"][0].transpose(1, 0, 2)), pool_scale=f(inp["pool_scale"][0]),
        w_out=f(inp["w_out"][0].reshape(8, 128, D).transpose(1, 0, 2)),
        norm2_w=f(inp["norm2_w"][0]), norm_f_w=f(inp["norm_f_w"]),
        wq=f(inp["peer_wq"][0].reshape(8, 128, 2048).transpose(1, 0, 2)),
        keysT=f(inp["peer_keys"][0].transpose(3, 0, 1, 2).reshape(128, 16, 128)),
        peer_u=f(inp["peer_u"][0]), peer_v=f(inp["peer_v"][0]),
    )
    shared.update(make_consts())
    if not peer:
        del shared["peer_u"], shared["peer_v"]
    xs = np.asarray(inp["x"], dtype=np.float32)
    return [dict(shared, x=np.ascontiguousarray(xs[c])) for c in range(8)]


def kernel(**inputs):
    nc = build()
    in_maps = make_in_maps(inputs)
    res = run_bass_kernel_spmd(nc, in_maps, core_ids=list(range(8)))
    return np.stack([np.asarray(r["out"], dtype=np.float32) for r in res.results], axis=0)
```
